# Optimizing a Trainium2 kernel written in Bass

```python
import jax, jax.numpy as jnp
from jax import lax
import numpy as np

D_MODEL = 1024
BATCH = 8
SEQ = 4096
DEPTH = 2

CHUNK = 64
Q_BLOCK = 128
HEAD_DIM = 64
SB_HEADS = 4
MLA_HEADS = 8
MLA_Q_RANK = 256
MLA_KV_RANK = 128
MLA_NOPE = 64
MLA_ROPE = 32
MLA_V = 64
ROPE_THETA = 10000.0
CK_HEADS = 4
CK_LEFT_CHUNKS = 8
REL_CLIP = 128

D_SB = SB_HEADS * HEAD_DIM
D_MLA = MLA_HEADS * MLA_V
D_CK = CK_HEADS * HEAD_DIM
D_MIX = D_SB + D_MLA + D_CK
IN_SIZES = (D_SB, D_SB, D_SB, MLA_Q_RANK, MLA_KV_RANK, MLA_ROPE, D_CK, D_CK, D_CK)
IN_COLS = sum(IN_SIZES)
IN_SPLITS = tuple(int(v) for v in np.cumsum(IN_SIZES)[:-1])
GROUP_SPLITS = (D_SB, D_SB + D_MLA)

D_FF = 2816
N_EXPERTS = 8
TOP_K = 2
N_DENSE = (DEPTH + 1) // 2
N_MOE = DEPTH // 2
EPS = 1e-6
NEG = -1e30

kernel_name = "hybrid_streaming_sb_mla_chunkattn_moe"


def _rms_norm(x, g):
    xf = x.astype(jnp.float32)
    y = xf * lax.rsqrt(jnp.mean(xf * xf, axis=-1, keepdims=True) + EPS)
    return (y * g.astype(jnp.float32)).astype(x.dtype)


def _rope(x, cos, sin):
    half = x.shape[-1] // 2
    x1, x2 = x[..., :half], x[..., half:]
    return jnp.concatenate([x1 * cos - x2 * sin, x2 * cos + x1 * sin], axis=-1)


def _stick_breaking(q, k, v):
    S, Dh = q.shape[1], q.shape[-1]
    scale = Dh ** -0.5
    outs = []
    for i in range(S // Q_BLOCK):
        q0, q1 = i * Q_BLOCK, (i + 1) * Q_BLOCK
        z = jnp.einsum('bqhd,bkhd->bhqk', q[:, q0:q1], k[:, :q1]).astype(jnp.float32) * scale
        tq = q0 + jnp.arange(Q_BLOCK)
        tk = jnp.arange(q1)
        strict = tk[None, :] < tq[:, None]
        log_1mb = jnp.where(strict, jax.nn.log_sigmoid(-z), 0.0)
        tail = lax.cumsum(log_1mb, axis=3, reverse=True) - log_1mb
        w = jnp.where(strict, jnp.exp(jax.nn.log_sigmoid(z) + tail), 0.0)
        outs.append(jnp.einsum('bhqk,bkhd->bqhd', w.astype(v.dtype), v[:, :q1]))
    return jnp.concatenate(outs, axis=1)


def _chunk_causal_attention(q, k, v, scale):
    S = q.shape[1]
    outs = []
    for i in range(S // Q_BLOCK):
        q0, q1 = i * Q_BLOCK, (i + 1) * Q_BLOCK
        s = jnp.einsum('bqhd,bkhd->bhqk', q[:, q0:q1], k[:, :q1]).astype(jnp.float32) * scale
        tq = q0 + jnp.arange(Q_BLOCK)
        tk = jnp.arange(q1)
        allowed = (tk[None, :] // CHUNK) <= (tq[:, None] // CHUNK)
        p = jax.nn.softmax(jnp.where(allowed, s, NEG), axis=-1)
        outs.append(jnp.einsum('bhqk,bkhd->bqhd', p.astype(v.dtype), v[:, :q1]))
    return jnp.concatenate(outs, axis=1)


def _chunked_band_attention(q, k, v, rel_bias):
    B, S, H, Dh = q.shape
    NC = S // CHUNK
    W = CK_LEFT_CHUNKS + 1
    qc = q.reshape(B, NC, CHUNK, H, Dh)
    pad = ((0, 0), (CK_LEFT_CHUNKS * CHUNK, 0), (0, 0), (0, 0))
    kp = jnp.pad(k, pad).reshape(B, NC + CK_LEFT_CHUNKS, CHUNK, H, Dh)
    vp = jnp.pad(v, pad).reshape(B, NC + CK_LEFT_CHUNKS, CHUNK, H, Dh)
    band = jnp.arange(NC)[:, None] + jnp.arange(W)[None, :]
    kb = kp[:, band].reshape(B, NC, W * CHUNK, H, Dh)
    vb = vp[:, band].reshape(B, NC, W * CHUNK, H, Dh)
    s = jnp.einsum('bnqhd,bnkhd->bnhqk', qc, kb).astype(jnp.float32) * (Dh ** -0.5)
    q_pos = jnp.arange(CHUNK) + CK_LEFT_CHUNKS * CHUNK
    k_pos = jnp.arange(W * CHUNK)
    rel = jnp.clip(q_pos[:, None] - k_pos[None, :], -REL_CLIP, REL_CLIP) + REL_CLIP
    bias = rel_bias.astype(jnp.float32)[:, rel]
    key_chunk = jnp.arange(NC)[:, None] + (k_pos // CHUNK)[None, :] - CK_LEFT_CHUNKS
    valid = (key_chunk >= 0)[None, :, None, None, :]
    p = jax.nn.softmax(jnp.where(valid, s + bias[None, None], NEG), axis=-1)
    o = jnp.einsum('bnhqk,bnkhd->bnqhd', p.astype(v.dtype), vb)
    return o.reshape(B, S, H, Dh)


def _mixer(h, cos, sin, w_in, mla_q_norm, w_q_up, mla_kv_norm, w_kv_up, mla_q_qknorm,
           mla_k_qknorm, ck_q_qknorm, ck_k_qknorm, ck_rel_bias, group_out_norm, w_out):
    B, S, _ = h.shape
    proj = h @ w_in
    sb_q, sb_k, sb_v, cq, ckv, k_pe, ck_q, ck_k, ck_v = jnp.split(proj, IN_SPLITS, axis=-1)

    hd = lambda t, n: t.reshape(B, S, n, -1)
    o_sb = _stick_breaking(hd(sb_q, SB_HEADS), hd(sb_k, SB_HEADS), hd(sb_v, SB_HEADS))

    q = (_rms_norm(cq, mla_q_norm) @ w_q_up).reshape(B, S, MLA_HEADS, MLA_NOPE + MLA_ROPE)
    q = jnp.concatenate([q[..., :MLA_NOPE], _rope(q[..., MLA_NOPE:], cos, sin)], axis=-1)
    kv = (_rms_norm(ckv, mla_kv_norm) @ w_kv_up).reshape(B, S, MLA_HEADS, MLA_NOPE + MLA_V)
    k_pe = _rope(k_pe[:, :, None, :], cos, sin)
    k = jnp.concatenate([kv[..., :MLA_NOPE],
                         jnp.broadcast_to(k_pe, (B, S, MLA_HEADS, MLA_ROPE))], axis=-1)
    q = _rms_norm(q, mla_q_qknorm)
    k = _rms_norm(k, mla_k_qknorm)
    o_mla = _chunk_causal_attention(q, k, kv[..., MLA_NOPE:], (MLA_NOPE + MLA_ROPE) ** -0.5)

    o_ck = _chunked_band_attention(_rms_norm(hd(ck_q, CK_HEADS), ck_q_qknorm),
                                   _rms_norm(hd(ck_k, CK_HEADS), ck_k_qknorm),
                                   hd(ck_v, CK_HEADS), ck_rel_bias)

    g_sb, g_mla, g_ck = jnp.split(group_out_norm, GROUP_SPLITS)
    merged = jnp.concatenate([_rms_norm(o_sb.reshape(B, S, D_SB), g_sb),
                              _rms_norm(o_mla.reshape(B, S, D_MLA), g_mla),
                              _rms_norm(o_ck.reshape(B, S, D_CK), g_ck)], axis=-1)
    return merged @ w_out


def _swiglu(h, w_gate, w_up, w_down):
    return (jax.nn.silu(h @ w_gate) * (h @ w_up)) @ w_down


def _moe(h, w_router, w_gate, w_up, w_down):
    logits = (h @ w_router).astype(jnp.float32)
    top_val, top_idx = lax.top_k(logits, TOP_K)
    top_w = jax.nn.softmax(top_val, axis=-1)
    gates = jnp.sum(jax.nn.one_hot(top_idx, N_EXPERTS, dtype=jnp.float32) * top_w[..., None], axis=-2)
    gates = gates.astype(h.dtype)
    out = jnp.zeros_like(h)
    for e in range(N_EXPERTS):
        out = out + gates[..., e:e + 1] * _swiglu(h, w_gate[e], w_up[e], w_down[e])
    return out


def setup_inputs(seed: int = 0) -> dict:
    key = jax.random.key(seed)
    ks = iter(jax.random.split(key, 32))
    f32 = jnp.float32

    def w(shape, fan_in, mult=1.0):
        return jax.random.normal(next(ks), shape, f32) * (mult * fan_in ** -0.5)

    def gain(shape):
        return 1.0 + 0.1 * jax.random.normal(next(ks), shape, f32)

    x = jax.random.normal(next(ks), (BATCH, SEQ, D_MODEL), f32)
    c = jax.random.normal(next(ks), (BATCH, D_MODEL), f32)
    offsets = jax.random.randint(next(ks), (BATCH, 1), 0, 4096, dtype=jnp.int32)
    positions = (offsets + jnp.arange(SEQ, dtype=jnp.int32)[None, :]).astype(jnp.int32)
    return {
        "x": x,
        "c": c,
        "positions": positions,
        "ada_w": w((DEPTH, D_MODEL, 6 * D_MODEL), D_MODEL, 0.5),
        "ada_b": 0.02 * jax.random.normal(next(ks), (DEPTH, 6 * D_MODEL), f32),
        "norm_mix": gain((DEPTH, D_MODEL)),
        "norm_ffn": gain((DEPTH, D_MODEL)),
        "w_in": w((DEPTH, D_MODEL, IN_COLS), D_MODEL),
        "mla_q_norm": gain((DEPTH, MLA_Q_RANK)),
        "w_q_up": w((DEPTH, MLA_Q_RANK, MLA_HEADS * (MLA_NOPE + MLA_ROPE)), MLA_Q_RANK),
        "mla_kv_norm": gain((DEPTH, MLA_KV_RANK)),
        "w_kv_up": w((DEPTH, MLA_KV_RANK, MLA_HEADS * (MLA_NOPE + MLA_V)), MLA_KV_RANK),
        "mla_q_qknorm": gain((DEPTH, MLA_NOPE + MLA_ROPE)),
        "mla_k_qknorm": gain((DEPTH, MLA_NOPE + MLA_ROPE)),
        "ck_q_qknorm": gain((DEPTH, HEAD_DIM)),
        "ck_k_qknorm": gain((DEPTH, HEAD_DIM)),
        "ck_rel_bias": 0.5 * jax.random.normal(next(ks), (DEPTH, CK_HEADS, 2 * REL_CLIP + 1), f32),
        "group_out_norm": gain((DEPTH, D_MIX)),
        "w_out": w((DEPTH, D_MIX, D_MODEL), D_MIX),
        "ffn_w_gate": w((N_DENSE, D_MODEL, D_FF), D_MODEL),
        "ffn_w_up": w((N_DENSE, D_MODEL, D_FF), D_MODEL),
        "ffn_w_down": w((N_DENSE, D_FF, D_MODEL), D_FF),
        "moe_router": w((N_MOE, D_MODEL, N_EXPERTS), D_MODEL),
        "moe_w_gate": w((N_MOE, N_EXPERTS, D_MODEL, D_FF), D_MODEL),
        "moe_w_up": w((N_MOE, N_EXPERTS, D_MODEL, D_FF), D_MODEL),
        "moe_w_down": w((N_MOE, N_EXPERTS, D_FF, D_MODEL), D_FF),
    }


def reference(x, c, positions, ada_w, ada_b, norm_mix, norm_ffn, w_in, mla_q_norm, w_q_up,
              mla_kv_norm, w_kv_up, mla_q_qknorm, mla_k_qknorm, ck_q_qknorm, ck_k_qknorm,
              ck_rel_bias, group_out_norm, w_out, ffn_w_gate, ffn_w_up, ffn_w_down,
              moe_router, moe_w_gate, moe_w_up, moe_w_down):
    inv_freq = ROPE_THETA ** (-jnp.arange(0, MLA_ROPE, 2, dtype=jnp.float32) / MLA_ROPE)
    ang = positions.astype(jnp.float32)[..., None] * inv_freq
    cos = jnp.cos(ang)[:, :, None, :].astype(x.dtype)
    sin = jnp.sin(ang)[:, :, None, :].astype(x.dtype)
    c_act = jax.nn.silu(c)
    for layer in range(DEPTH):
        mod = c_act @ ada_w[layer] + ada_b[layer]
        sh1, sc1, g1, sh2, sc2, g2 = [m[:, None, :] for m in jnp.split(mod, 6, axis=-1)]
        h = _rms_norm(x, norm_mix[layer]) * (1.0 + sc1) + sh1
        x = x + g1 * _mixer(h, cos, sin, w_in[layer], mla_q_norm[layer], w_q_up[layer],
                            mla_kv_norm[layer], w_kv_up[layer], mla_q_qknorm[layer],
                            mla_k_qknorm[layer], ck_q_qknorm[layer], ck_k_qknorm[layer],
                            ck_rel_bias[layer], group_out_norm[layer], w_out[layer])
        h = _rms_norm(x, norm_ffn[layer]) * (1.0 + sc2) + sh2
        i = layer // 2
        if layer % 2 == 0:
            y = _swiglu(h, ffn_w_gate[i], ffn_w_up[i], ffn_w_down[i])
        else:
            y = _moe(h, moe_router[i], moe_w_gate[i], moe_w_up[i], moe_w_down[i])
        x = x + g2 * y
    return x
```

```python
import math
import numpy as np
from contextlib import ExitStack
import concourse.bass as bass
import concourse.mybir as mybir
from concourse.bass_utils import run_bass_kernel_spmd

F32 = mybir.dt.float32
BF16 = mybir.dt.bfloat16
I32 = mybir.dt.int32
AF = mybir.ActivationFunctionType
ALU = mybir.AluOpType
AX = mybir.AxisListType

D = 1024
DFF = 2816
NJ = DFF // 128
NE = 8
EPS = 1e-6
NEG = -30000.0
COMPUTE = ("pe", "act", "dve", "pool")


class Prog:
    def __init__(self, nc):
        self.nc = nc
        self.stacks = [ExitStack()]
        self.ins = {e: [] for e in COMPUTE + ("sp",)}
        self.res_w = {}
        self.res_r = {}
        self.dma_cnt = {}
        self.sems = {}
        self.ntile = 0
        self.pending = {}
        self.local = None
        self.capture = None

    def replay_interleaved(self, lists):
        self.local = None
        pos = [0] * len(lists)
        tot = max(len(x) for x in lists) if lists else 0
        for step in range(tot):
            for li, lst in enumerate(lists):
                upto = ((step + 1) * len(lst) + tot - 1) // tot
                while pos[li] < min(upto, len(lst)):
                    rec = lst[pos[li]]
                    pos[li] += 1
                    if rec[0] == "op":
                        self.op(rec[1], rec[2], rec[3], rec[4])
                    else:
                        self.dma(rec[1], rec[2], rec[3], rec[4], q=rec[5])

    def _rn(self, rs):
        if self.local is None:
            return list(rs)
        names, sfx = self.local
        out = []
        for r in rs:
            base = r[0] if isinstance(r, tuple) else r
            out.append((r, sfx) if base in names else r)
        return out

    def push(self):
        self.stacks.append(ExitStack())

    def pop(self):
        self.stacks.pop().close()

    def sb(self, shape, dt, name=None):
        self.ntile += 1
        return self.stacks[-1].enter_context(self.nc.sbuf_tensor(f"t{self.ntile}", list(shape), dt))

    def ps(self, shape, dt=F32):
        self.ntile += 1
        return self.stacks[0].enter_context(self.nc.psum_tensor(f"p{self.ntile}", list(shape), dt))

    def _deps(self, eng, reads, writes):
        deps = {}

        def add(d):
            for k, v in d.items():
                if deps.get(k, -1) < v:
                    deps[k] = v
        for r in reads:
            add(self.res_w.get(r, {}))
        for w in writes:
            add(self.res_w.get(w, {}))
            add(self.res_r.get(w, {}))
        if eng in self.pending:
            add(self.pending.pop(eng))
        return deps

    def barrier(self, engines=COMPUTE + ("sp",)):
        d = {}
        for e in COMPUTE:
            if self.ins[e]:
                for i in range(len(self.ins[e]) - 1, -1, -1):
                    if self.ins[e][i]["kind"] == "op":
                        d[("E", e)] = i
                        break
        for res, cnt in self.dma_cnt.items():
            d[("D", res)] = cnt
        for e in engines:
            cur = self.pending.setdefault(e, {})
            for k, v in d.items():
                if cur.get(k, -1) < v:
                    cur[k] = v

    def op(self, eng, fn, reads=(), writes=()):
        reads = self._rn(reads)
        writes = self._rn(writes)
        if self.capture is not None:
            self.capture.append(("op", eng, fn, reads, writes))
            return None
        lst = self.ins[eng]
        idx = len(lst)
        deps = self._deps(eng, reads, writes)
        if eng == "pe":
            deps.pop(("E", "pe"), None)
        rec = dict(kind="op", fn=fn, deps=deps, signal=False)
        lst.append(rec)
        key = ("E", eng)
        for r in reads:
            self.res_r.setdefault(r, {})[key] = idx
        for w in writes:
            self.res_w[w] = {key: idx}
            self.res_r[w] = {}
        return rec

    def dma(self, out_ap, in_ap, reads, writes, q="sp"):
        reads = self._rn(reads)
        writes = self._rn(writes)
        if self.capture is not None:
            self.capture.append(("dma", out_ap, in_ap, reads, writes, q))
            return None
        lst = self.ins[q]
        dst = writes[0]
        deps = self._deps(q, reads, writes)
        skey = ("D", dst)
        deps.pop(skey, None)
        cnt = self.dma_cnt.get(dst, 0) + 16
        self.dma_cnt[dst] = cnt
        rec = dict(kind="dma", out=out_ap, in_=in_ap, deps=deps, skey=skey)
        lst.append(rec)
        for r in reads:
            self.res_r.setdefault(r, {})[skey] = cnt
        for w in writes:
            d = self.res_w.setdefault(w, {})
            if set(d.keys()) - {skey}:
                d = {}
                self.res_w[w] = d
            d[skey] = cnt
            self.res_r[w] = {}
        return rec

    def emit(self, final_waits=()):
        nc = self.nc
        es = self.stacks[0]
        for e in self.ins:
            for rec in self.ins[e]:
                for (kind, k), v in rec["deps"].items():
                    if kind == "E":
                        self.ins[k][v]["signal"] = True
        for e in COMPUTE:
            c = 0
            for rec in self.ins[e]:
                if rec.get("signal"):
                    c += 1
                    rec["sigval"] = c
        keys = [("E", e) for e in COMPUTE] + sorted({rec["skey"] for e in self.ins for rec in self.ins[e]
                                                     if rec["kind"] == "dma"}, key=str)
        for i, k in enumerate(keys):
            self.sems[k] = es.enter_context(nc.semaphore(f"s{i}"))
        final = dict(self.dma_cnt)
        with nc.Block() as block:
            def mk(ename, lists):
                def body(eng):
                    waited = {}
                    for rec in lists:
                        for (kind, k), v in rec["deps"].items():
                            val = self.ins[k][v]["sigval"] if kind == "E" else v
                            key = (kind, k)
                            if waited.get(key, -1) >= val:
                                continue
                            waited[key] = val
                            eng.wait_ge(self.sems[key], val)
                        if rec["kind"] == "op":
                            inst = rec["fn"](eng)
                            if rec["signal"]:
                                inst.then_inc(self.sems[("E", ename)], 1)
                        else:
                            inst = eng.dma_start(out=rec["out"], in_=rec["in_"])
                            inst.then_inc(self.sems[rec["skey"]], 16)
                    if ename == "sp":
                        for res in final_waits:
                            eng.wait_ge(self.sems[("D", res)], final[res])
                return body
            block.tensor(mk("pe", self.ins["pe"]))
            block.scalar(mk("act", self.ins["act"]))
            block.vector(mk("dve", self.ins["dve"]))
            block.gpsimd(mk("pool", self.ins["pool"]))
            block.sync(mk("sp", self.ins["sp"]))
        while self.stacks:
            self.stacks.pop().close()


C_ID, C_NUT, C_STRICT, C_NMA, C_MB, C_CK0, C_CK4, C_SEL = [i * 128 for i in range(8)]
NCONST = 7 * 128 + 8 * 128
INV_FREQ = (np.float32(10000.0) ** (-np.arange(0, 32, 2, dtype=np.float32) / np.float32(32))).astype(np.float32)


def make_consts():
    j = np.arange(128)[:, None]
    i = np.arange(128)[None, :]
    c = np.zeros((128, NCONST), np.float32)
    c[:, C_ID:C_ID + 128] = (j == i)
    c[:, C_NUT:C_NUT + 128] = -(j >= i).astype(np.float32)
    c[:, C_STRICT:C_STRICT + 128] = (j < i)
    c[:, C_NMA:C_NMA + 128] = np.where(j >= i, NEG, 0.0)
    c[:, C_MB:C_MB + 128] = np.where((j >= 64) & (i < 64), NEG, 0.0)
    c[:, C_CK0:C_CK0 + 128] = np.where((i >= 64) & (j < 64), NEG, 0.0)
    c[:, C_CK4:C_CK4 + 128] = np.where((i < 64) & (j >= 64), NEG, 0.0)
    for e in range(8):
        c[e, C_SEL + e * 128:C_SEL + (e + 1) * 128] = 1.0
    return c


G_QN, G_KVN, G_QQK, G_KQK, G_CQ, G_CK, G_OUT = 0, 256, 384, 480, 576, 640, 704
NGAIN = 704 + 1024


def build(S, nlayers=2, debug=False, TS=1024, stop_after=None):
    NT = S // 128
    NB = S // 512
    TS = min(TS, S)
    nc = bass.Bass("TRN2", target_bir_lowering=False)
    P = Prog(nc)

    def din(name, shape, dt=F32):
        return nc.dram_tensor(name, list(shape), dt, kind="ExternalInput").ap()

    def dscr(name, shape, dt):
        return nc.dram_tensor(name, list(shape), dt, kind=("ExternalOutput" if debug else "Internal")).ap()

    xT_in = din("xT", [D, S])
    cT_d = din("cT", [128, 8])
    pos_d = din("pos", [128, NT], I32)
    consts_d = din("consts", [128, NCONST])
    vec_d = din("vec", [2, 128, 64])
    gains_d = din("gains", [2, 128, NGAIN])
    ckb_d = din("ckbias", [2, 128, 20, 128])
    ada_w = din("ada_w", [2, D, 6 * D])
    w_in = din("w_in", [2, D, 1952])
    w_q_up = din("w_q_up", [2, 256, 768])
    w_kv_up = din("w_kv_up", [2, 128, 1024])
    w_out = din("w_out", [2, D, D])
    ffn_g = din("ffn_w_gate", [1, D, DFF])
    ffn_u = din("ffn_w_up", [1, D, DFF])
    ffn_d = din("ffn_w_down", [1, DFF, D])
    router = din("moe_router", [1, D, NE])
    moe_g = din("moe_w_gate", [1, NE, D, DFF])
    moe_u = din("moe_w_up", [1, NE, D, DFF])
    moe_d = din("moe_w_down", [1, NE, DFF, D])
    yT = nc.dram_tensor("yT", [D, S], F32, kind="ExternalOutput").ap()

    xT_mid = dscr("xT_mid", [D, S], F32)
    sbQZ = dscr("sbQZ", [4, 128, S], BF16)
    sbKT = dscr("sbKT", [2, 128, S], BF16)
    sbV = dscr("sbV", [S, 256], BF16)
    mlQT = dscr("mlQT", [8, 96, S], BF16)
    mlKT = dscr("mlKT", [8, 96, S], BF16)
    mlV = dscr("mlV", [S, 512], BF16)
    ckQZ = dscr("ckQZ", [4, 128, S], BF16)
    ckKT = dscr("ckKT", [2, 128, S], BF16)
    ckV = dscr("ckV", [S, 256], BF16)
    mgT = dscr("mgT", [D, S], BF16)
    rope_d = dscr("rope_d", [2, 128, NT, 16], F32)
    dbgG = dscr("dbgG", [8, S], F32) if debug else None

    cst = P.sb([128, NCONST], F32)
    cbf = P.sb([128, 5 * 128], BF16)
    ones_bf = P.sb([128, 128], BF16)
    negones_bf = P.sb([128, 128], BF16)
    negrow = P.sb([1, 128], F32)
    zeros_bf = P.sb([128, 512], BF16)
    negid_bf = P.sb([128, 128], BF16)
    cact = P.sb([128, 8], F32)
    vec = P.sb([128, 2, 64], F32)
    mod = P.sb([128, 2, 48], F32)
    A1 = P.sb([128, 2, 8], F32)
    A2 = P.sb([128, 2, 8], F32)
    PS = [P.ps([128, 512], F32) for _ in range(8)]

    def psr(k):
        return ("ps", k)

    ident_bf = cbf[:, 0:128]
    negut_bf = cbf[:, 128:256]
    strict_bf = cbf[:, 256:384]
    nma_bf = cbf[:, 384:512]
    mb_bf = cbf[:, 512:640]
    ident_f = cst[:, C_ID:C_ID + 128]

    P.dma(cst[:], consts_d[:, :], ["consts_d"], ["cst"])
    P.dma(cact[:], cT_d[:, :], ["cT_d"], ["cact"])
    P.dma(vec[:], vec_d.rearrange("l p c -> p l c"), ["vec_d"], ["vec"])
    P.op("dve", lambda e: e.tensor_copy(out=cbf[:], in_=cst[:, 0:640]), ["cst"], ["cbf"])
    P.op("pool", lambda e: e.memset(ones_bf[:], 1.0), [], ["ones_bf"])
    P.op("pool", lambda e: e.memset(negones_bf[:], -1.0), [], ["negones_bf"])
    P.op("pool", lambda e: e.memset(negrow[:], -1.0), [], ["negrow"])
    P.op("pool", lambda e: e.memset(zeros_bf[:], 0.0), [], ["zeros_bf"])
    P.op("dve", lambda e: e.tensor_scalar(out=negid_bf[:], in0=cst[:, C_ID:C_ID + 128], scalar1=-1.0, scalar2=None, op0=ALU.mult), ["cst"], ["negid_bf"])
    P.op("act", lambda e: e.activation(out=cact[:], in_=cact[:], func=AF.Silu), ["cact"], ["cact"])
    for zt, zres in ((sbQZ, "sbQZ"), (ckQZ, "ckQZ")):
        for h in range(4):
            lo = 64 if h % 2 == 0 else 0
            for tbz in range(NB):
                P.dma(zt[h, lo:lo + 64, tbz * 512:(tbz + 1) * 512], zeros_bf[lo:lo + 64, :], ["zeros_bf"], [zres])

    P.push()
    awb = [P.sb([128, 8, 512], F32) for _ in range(2)]
    for l in range(nlayers):
        awv = ada_w[l].rearrange("(kc p) n -> p kc n", p=128)
        for g in range(12):
            b = g % 2
            P.dma(awb[b][:], awv[:, :, g * 512:(g + 1) * 512], ["ada_w"], [("awb", b)])
            for m in range(4):
                col = g * 4 + m
                for kc in range(8):
                    P.op("pe", lambda e, b=b, m=m, kc=kc, col=col: e.matmul(
                        PS[0][:, col:col + 1], lhsT=awb[b][:, kc, m * 128:(m + 1) * 128], rhs=cact[:, kc:kc + 1],
                        start=(kc == 0), stop=(kc == 7)), [("awb", b), "cact"], [psr(0)])
        P.op("dve", lambda e, l=l: e.tensor_tensor(out=mod[:, l, :], in0=PS[0][:, 0:48], in1=vec[:, l, 0:48], op=ALU.add),
             [psr(0), "vec"], ["mod"])
        P.op("dve", lambda e, l=l: e.scalar_tensor_tensor(out=A1[:, l, :], in0=mod[:, l, 8:16], scalar=1.0, in1=vec[:, l, 48:56],
                                                          op0=ALU.add, op1=ALU.mult), ["mod", "vec"], ["A1"])
        P.op("dve", lambda e, l=l: e.scalar_tensor_tensor(out=A2[:, l, :], in0=mod[:, l, 32:40], scalar=1.0, in1=vec[:, l, 56:64],
                                                          op0=ALU.add, op1=ALU.mult), ["mod", "vec"], ["A2"])

    cos_t = P.sb([128, NT, 16], F32)
    sin_t = P.sb([128, NT, 16], F32)
    posi = P.sb([128, NT], I32)
    posf = P.sb([128, NT], F32)
    ang = P.sb([128, NT, 16], F32)
    u = P.sb([128, NT, 16], F32)
    ki = P.sb([128, NT, 16], I32)
    kf = P.sb([128, NT, 16], F32)
    r = P.sb([128, NT, 16], F32)
    fx = P.sb([128, NT, 16], F32)
    P.dma(posi[:], pos_d[:, :], ["pos_d"], ["posi"])
    P.op("dve", lambda e: e.tensor_copy(out=posf[:], in_=posi[:]), ["posi"], ["posf"])
    for i in range(16):
        P.op("dve", lambda e, i=i: e.tensor_scalar(out=ang[:, :, i], in0=posf[:], scalar1=float(INV_FREQ[i]), scalar2=None,
                                                   op0=ALU.mult), ["posf"], ["ang"])
    TWO_PI = 2.0 * math.pi
    C1 = 6.28125
    C2 = TWO_PI - C1
    for which, dst in ((0, sin_t), (1, cos_t)):
        src = ang
        if which == 1:
            P.op("dve", lambda e: e.tensor_scalar(out=r[:], in0=ang[:], scalar1=math.pi / 2, scalar2=None, op0=ALU.add),
                 ["ang"], ["r"])
            src = r
        P.op("dve", lambda e, src=src: e.tensor_scalar(out=u[:], in0=src[:], scalar1=1.0 / TWO_PI, scalar2=None, op0=ALU.mult),
             ["ang", "r"], ["u"])
        P.op("dve", lambda e: e.tensor_copy(out=ki[:], in_=u[:]), ["u"], ["ki"])
        P.op("dve", lambda e: e.tensor_copy(out=kf[:], in_=ki[:]), ["ki"], ["kf"])
        P.op("dve", lambda e, src=src: e.scalar_tensor_tensor(out=u[:], in0=kf[:], scalar=-C1, in1=src[:], op0=ALU.mult, op1=ALU.add),
             ["kf", "ang", "r"], ["u"])
        P.op("dve", lambda e: e.scalar_tensor_tensor(out=r[:], in0=kf[:], scalar=-C2, in1=u[:], op0=ALU.mult, op1=ALU.add),
             ["kf", "u"], ["r"])
        P.op("dve", lambda e: e.tensor_scalar(out=fx[:], in0=r[:], scalar1=math.pi, scalar2=-TWO_PI, op0=ALU.is_gt, op1=ALU.mult),
             ["r"], ["fx"])
        P.op("dve", lambda e: e.tensor_tensor(out=r[:], in0=r[:], in1=fx[:], op=ALU.add), ["r", "fx"], ["r"])
        P.op("dve", lambda e: e.tensor_scalar(out=fx[:], in0=r[:], scalar1=-math.pi, scalar2=TWO_PI, op0=ALU.is_lt, op1=ALU.mult),
             ["r"], ["fx"])
        P.op("dve", lambda e: e.tensor_tensor(out=r[:], in0=r[:], in1=fx[:], op=ALU.add), ["r", "fx"], ["r"])
        P.op("act", lambda e, dst=dst: e.activation(out=dst[:], in_=r[:], func=AF.Sin), ["r"], ["rope_t0"])
    P.dma(rope_d[0], cos_t[:], ["rope_t0"], ["rope_d"])
    P.dma(rope_d[1], sin_t[:], ["rope_t0"], ["rope_d"])
    P.barrier()
    P.pop()

    def do_layer(l):
        x_src = xT_in if l == 0 else xT_mid
        x_dst = yT if l == nlayers - 1 else xT_mid
        xres_in = "xT_in" if l == 0 else "xT_mid"
        xres_out = "yT" if l == nlayers - 1 else "xT_mid"
        xsv = x_src.rearrange("(c p) s -> p c s", p=128)
        xdv = x_dst.rearrange("(c p) s -> p c s", p=128)

        P.push()
        cos_t = P.sb([128, NT, 16], F32)
        sin_t = P.sb([128, NT, 16], F32)
        gains = P.sb([128, NGAIN], F32)
        ckbm = P.sb([128, 20, 128], F32)
        P.dma(cos_t[:], rope_d[0], ["rope_d"], ["rope_t"])
        P.dma(sin_t[:], rope_d[1], ["rope_d"], ["rope_t"])
        P.dma(gains[:], gains_d[l], ["gains_d"], ["gains"])
        P.dma(ckbm[:], ckb_d[l], ["ckb_d"], ["ckbm"])
        for h in range(4):
            P.op("dve", lambda e, h=h: e.tensor_tensor(out=ckbm[:, h * 5 + 0, :], in0=ckbm[:, h * 5 + 0, :],
                                                       in1=cst[:, C_CK0:C_CK0 + 128], op=ALU.add), ["ckbm", "cst"], ["ckbm"])
            P.op("dve", lambda e, h=h: e.tensor_tensor(out=ckbm[:, h * 5 + 4, :], in0=ckbm[:, h * 5 + 4, :],
                                                       in1=cst[:, C_CK4:C_CK4 + 128], op=ALU.add), ["ckbm", "cst"], ["ckbm"])

        P.push()
        win_bf = P.sb([128, 8, 1952], BF16)
        wq_bf = P.sb([128, 2, 768], BF16)
        wkv_bf = P.sb([128, 1024], BF16)
        wiv = w_in[l].rearrange("(kc p) n -> p kc n", p=128)
        for kc in range(8):
            P.dma(win_bf[:, kc, :], wiv[:, kc, :], ["w_in"], ["win_bf"], q="pool")
        P.dma(wq_bf[:], w_q_up[l].rearrange("(kc p) n -> p kc n", p=128), ["w_q_up"], ["wq_bf"], q="pool")
        P.dma(wkv_bf[:], w_kv_up[l], ["w_kv_up"], ["wkv_bf"], q="pool")

        xb = P.sb([128, 8, 512], F32)
        sq = P.sb([128, 8, 512], BF16)
        rstd = P.sb([128, 512], F32)
        hT = P.sb([128, 8, 512], BF16)
        fmo = [P.sb([128, 512], BF16) for _ in range(2)]
        LOCAL = {"proj", "vst", "junk", "ss", "ss2", "lat", "latT", "qf", "kvf", "kf32", "kper", "t16", "ssh", "sqj", "qn", "kn",
                 "cqn", "ckn", "trq", "trk", "trc", "mlv"}

        def alloc_tile_set():
            return dict(
                proj=P.sb([128, 1440], F32), vst=[P.sb([128, 256], BF16) for _ in range(2)], junk=P.sb([128, 256], F32),
                ss=P.sb([128, 4], F32), lat=P.sb([128, 384], BF16), latT=P.sb([128, 3, 128], BF16), qf=P.sb([128, 8, 96], F32),
                kvf=P.sb([128, 8, 128], F32), kf32=P.sb([128, 8, 96], F32), kper=P.sb([128, 32], F32),
                t16=[P.sb([128, 8, 16], F32) for _ in range(4)], ssh=P.sb([128, 8], F32), sqj=P.sb([128, 8, 96], F32),
                qn=P.sb([128, 8, 96], BF16), kn=P.sb([128, 8, 96], BF16), cqn=P.sb([128, 4, 64], BF16), ckn=P.sb([128, 4, 64], BF16),
                trq=P.sb([128, 1024], BF16), trk=P.sb([128, 1024], BF16), trc=P.sb([128, 512], BF16), mlv=P.sb([128, 8, 64], BF16))
        tsets = [alloc_tile_set() for _ in range(2)]

        def tt_s1(tb, i, proj, vst, junk, ss, lat, latT, qf, kvf, kf32, kper, t16, ssh, sqj, qn, kn, cqn, ckn, trq, trk, trc, mlv):
            j = tb * 4 + i
            tk = slice(i * 128, (i + 1) * 128)
            rows = slice(j * 128, (j + 1) * 128)
            for gi, (c0, c1, pb) in enumerate(((512, 1024, 3), (1024, 1440, 4), (1440, 1952, 5))):
                for kc in range(8):
                    P.op("pe", lambda e, kc=kc, c0=c0, c1=c1, pb=pb, tk=tk: e.matmul(
                        PS[pb][:, 0:c1 - c0], lhsT=hT[:, kc, tk], rhs=win_bf[:, kc, c0:c1], start=(kc == 0), stop=(kc == 7)),
                        ["hT", "win_bf"], [psr(pb)])
            P.op("act", lambda e: e.activation(out=proj[:, 0:512], in_=PS[3][:, :], func=AF.Copy), [psr(3)], [("proj", 0)])
            P.op("dve", lambda e: e.tensor_copy(out=proj[:, 512:928], in_=PS[4][:, 0:416]), [psr(4)], [("proj", 1)])
            P.op("act", lambda e: e.activation(out=proj[:, 928:1440], in_=PS[5][:, :], func=AF.Copy), [psr(5)], [("proj", 2)])
            P.op("pool", lambda e: e.tensor_copy(out=vst[0][:], in_=proj[:, 0:256]), [("proj", 0)], [("vst", 0)])
            P.dma(sbV[rows, :], vst[0][:], [("vst", 0)], ["sbV"])
            P.op("pool", lambda e: e.tensor_copy(out=vst[1][:], in_=proj[:, 1184:1440]), [("proj", 2)], [("vst", 1)])
            P.dma(ckV[rows, :], vst[1][:], [("vst", 1)], ["ckV"])
            P.op("act", lambda e: e.activation(out=junk[:, 0:256], in_=proj[:, 256:512], func=AF.Square, accum_out=ss[:, 0:1]),
                 [("proj", 0)], ["junk", "ss"])
            P.op("act", lambda e: e.activation(out=junk[:, 0:128], in_=proj[:, 512:640], func=AF.Square, accum_out=ss[:, 1:2]),
                 [("proj", 1), "ss"], ["junk", "ss"])
            P.op("act", lambda e: e.activation(out=ss[:, 2:3], in_=ss[:, 0:1], func=AF.Sqrt, scale=1.0 / 256, bias=EPS), ["ss"], ["ss"])
            P.op("act", lambda e: e.activation(out=ss[:, 3:4], in_=ss[:, 1:2], func=AF.Sqrt, scale=1.0 / 128, bias=EPS), ["ss"], ["ss"])
            P.op("dve", lambda e: e.reciprocal(out=ss[:, 2:4], in_=ss[:, 2:4]), ["ss"], ["ss"])
            P.op("dve", lambda e: e.scalar_tensor_tensor(out=lat[:, 0:256], in0=proj[:, 256:512], scalar=ss[:, 2:3],
                                                         in1=gains[:, G_QN:G_QN + 256], op0=ALU.mult, op1=ALU.mult),
                 [("proj", 0), "ss", "gains"], ["lat"])
            P.op("dve", lambda e: e.scalar_tensor_tensor(out=lat[:, 256:384], in0=proj[:, 512:640], scalar=ss[:, 3:4],
                                                         in1=gains[:, G_KVN:G_KVN + 128], op0=ALU.mult, op1=ALU.mult),
                 [("proj", 1), "ss", "gains"], ["lat"])
            pbt = PS[2][:].bitcast(BF16)
            for k in range(3):
                P.op("pe", lambda e, k=k, pbt=pbt: e.transpose(pbt[:, k * 128:(k + 1) * 128], lat[:, k * 128:(k + 1) * 128], ident_bf),
                     ["lat", "cbf"], [psr(2)])
            P.op("dve", lambda e, pbt=pbt: e.tensor_copy(out=latT[:], in_=pbt[:, 0:384]), [psr(2)], ["latT"])
            for half, pb in ((0, 3), (1, 4)):
                for kc in range(2):
                    P.op("pe", lambda e, half=half, pb=pb, kc=kc: e.matmul(
                        PS[pb][:, 0:384], lhsT=latT[:, kc, :], rhs=wq_bf[:, kc, half * 384:(half + 1) * 384],
                        start=(kc == 0), stop=(kc == 1)), ["latT", "wq_bf"], [psr(pb)])
            for half, pb in ((0, 5), (1, 7)):
                P.op("pe", lambda e, half=half, pb=pb: e.matmul(
                    PS[pb][:, :], lhsT=latT[:, 2, :], rhs=wkv_bf[:, half * 512:(half + 1) * 512], start=True, stop=True),
                    ["latT", "wkv_bf"], [psr(pb)])
            P.op("act", lambda e: e.activation(out=qf[:, 0:4, :], in_=PS[3][:, 0:384], func=AF.Copy), [psr(3)], [("qf", 0)])
            P.op("act", lambda e: e.activation(out=qf[:, 4:8, :], in_=PS[4][:, 0:384], func=AF.Copy), [psr(4)], [("qf", 1)])
            P.op("dve", lambda e: e.tensor_copy(out=kvf[:, 0:4, :], in_=PS[5][:, :]), [psr(5)], [("kvf", 0)])
            P.op("dve", lambda e: e.tensor_copy(out=kvf[:, 4:8, :], in_=PS[7][:, :]), [psr(7)], [("kvf", 1)])

        def tt_s2(tb, i, proj, vst, junk, ss, lat, latT, qf, kvf, kf32, kper, t16, ssh, sqj, qn, kn, cqn, ckn, trq, trk, trc, mlv):
            j = tb * 4 + i
            rows = slice(j * 128, (j + 1) * 128)
            pbt = PS[6][:].bitcast(BF16)
            cosb = cos_t[:, j, :]
            sinb = sin_t[:, j, :]
            cos8 = cosb.unsqueeze(1).broadcast_to([128, 8, 16])
            sin8 = sinb.unsqueeze(1).broadcast_to([128, 8, 16])
            QF = [("qf", 0), ("qf", 1)]
            P.op("pool", lambda e, cos8=cos8: e.tensor_tensor(out=t16[0][:], in0=qf[:, :, 64:80], in1=cos8, op=ALU.mult), QF + ["rope_t"], [("t16", 0)])
            P.op("pool", lambda e, sin8=sin8: e.tensor_tensor(out=t16[1][:], in0=qf[:, :, 80:96], in1=sin8, op=ALU.mult), QF + ["rope_t"], [("t16", 1)])
            P.op("pool", lambda e, cos8=cos8: e.tensor_tensor(out=t16[2][:], in0=qf[:, :, 80:96], in1=cos8, op=ALU.mult), QF + ["rope_t"], [("t16", 2)])
            P.op("pool", lambda e, sin8=sin8: e.tensor_tensor(out=t16[3][:], in0=qf[:, :, 64:80], in1=sin8, op=ALU.mult), QF + ["rope_t"], [("t16", 3)])
            P.op("dve", lambda e: e.tensor_tensor(out=qf[:, :, 64:80], in0=t16[0][:], in1=t16[1][:], op=ALU.subtract),
                 [("t16", 0), ("t16", 1)], QF)
            P.op("dve", lambda e: e.tensor_tensor(out=qf[:, :, 80:96], in0=t16[2][:], in1=t16[3][:], op=ALU.add),
                 [("t16", 2), ("t16", 3)], QF)
            P.op("dve", lambda e, cosb=cosb: e.tensor_tensor(out=t16[0][:, 0, :], in0=proj[:, 640:656], in1=cosb, op=ALU.mult), [("proj", 1), "rope_t"], [("t16", 0)])
            P.op("dve", lambda e, sinb=sinb: e.tensor_tensor(out=t16[1][:, 0, :], in0=proj[:, 656:672], in1=sinb, op=ALU.mult), [("proj", 1), "rope_t"], [("t16", 1)])
            P.op("dve", lambda e, cosb=cosb: e.tensor_tensor(out=t16[2][:, 0, :], in0=proj[:, 656:672], in1=cosb, op=ALU.mult), [("proj", 1), "rope_t"], [("t16", 2)])
            P.op("dve", lambda e, sinb=sinb: e.tensor_tensor(out=t16[3][:, 0, :], in0=proj[:, 640:656], in1=sinb, op=ALU.mult), [("proj", 1), "rope_t"], [("t16", 3)])
            P.op("dve", lambda e: e.tensor_tensor(out=kper[:, 0:16], in0=t16[0][:, 0, :], in1=t16[1][:, 0, :], op=ALU.subtract),
                 [("t16", 0), ("t16", 1)], ["kper"])
            P.op("dve", lambda e: e.tensor_tensor(out=kper[:, 16:32], in0=t16[2][:, 0, :], in1=t16[3][:, 0, :], op=ALU.add),
                 [("t16", 2), ("t16", 3)], ["kper"])
            KV = [("kvf", 0), ("kvf", 1)]
            P.op("pool", lambda e: e.tensor_copy(out=kf32[:, :, 0:64], in_=kvf[:, :, 0:64]), KV, ["kf32"])
            P.op("pool", lambda e: e.tensor_copy(out=kf32[:, :, 64:96], in_=kper[:].unsqueeze(1).broadcast_to([128, 8, 32])), ["kper", "kf32"], ["kf32"])
            P.op("pool", lambda e: e.tensor_copy(out=mlv[:], in_=kvf[:, :, 64:128]), KV, ["mlv"])
            P.dma(mlV[rows, :], mlv[:].rearrange("p h d -> p (h d)"), ["mlv"], ["mlV"])
            for (src, srcres, gofs, dstn, dres, sc) in ((qf, QF, G_QQK, qn, "qn", 96 ** -0.5), (kf32, ["kf32"], G_KQK, kn, "kn", 1.0)):
                P.op("pool", lambda e, src=src: e.tensor_tensor(out=sqj[:], in0=src[:], in1=src[:], op=ALU.mult), srcres, ["sqj"])
                P.op("dve", lambda e: e.tensor_reduce(out=ssh[:], in_=sqj[:], axis=AX.X, op=ALU.add), ["sqj"], ["ssh"])
                P.op("act", lambda e: e.activation(out=ssh[:], in_=ssh[:], func=AF.Sqrt, scale=1.0 / 96, bias=EPS), ["ssh"], ["ssh"])
                P.op("dve", lambda e: e.reciprocal(out=ssh[:], in_=ssh[:]), ["ssh"], ["ssh"])
                P.op("dve", lambda e, src=src: e.tensor_tensor(out=sqj[:], in0=src[:], in1=ssh[:].unsqueeze(2).broadcast_to([128, 8, 96]),
                                                               op=ALU.mult), srcres + ["ssh"], ["sqj"])
                P.op("dve", lambda e, gofs=gofs, dstn=dstn, sc=sc: e.scalar_tensor_tensor(
                    out=dstn[:], in0=sqj[:], scalar=float(sc), in1=gains[:, gofs:gofs + 96].unsqueeze(1).broadcast_to([128, 8, 96]),
                    op0=ALU.mult, op1=ALU.mult), ["sqj", "gains"], [dres])
            for (c0, pres, gofs, dstn, dres, sc) in ((672, ("proj", 1), G_CQ, cqn, "cqn", 0.125), (928, ("proj", 2), G_CK, ckn, "ckn", 1.0)):
                srcv = proj[:, c0:c0 + 256].rearrange("p (h d) -> p h d", h=4)
                sq4 = sqj[:, 0:4, 0:64]
                P.op("dve", lambda e, srcv=srcv, sq4=sq4: e.tensor_tensor(out=sq4, in0=srcv, in1=srcv, op=ALU.mult), [pres], ["sqj"])
                P.op("dve", lambda e, sq4=sq4: e.tensor_reduce(out=ssh[:, 0:4], in_=sq4, axis=AX.X, op=ALU.add), ["sqj"], ["ssh"])
                P.op("act", lambda e: e.activation(out=ssh[:, 0:4], in_=ssh[:, 0:4], func=AF.Sqrt, scale=1.0 / 64, bias=EPS), ["ssh"], ["ssh"])
                P.op("dve", lambda e: e.reciprocal(out=ssh[:, 0:4], in_=ssh[:, 0:4]), ["ssh"], ["ssh"])
                P.op("dve", lambda e, srcv=srcv, sq4=sq4: e.tensor_tensor(out=sq4, in0=srcv, in1=ssh[:, 0:4].unsqueeze(2).broadcast_to([128, 4, 64]),
                                                                          op=ALU.mult), [pres, "ssh"], ["sqj"])
                P.op("dve", lambda e, gofs=gofs, dstn=dstn, sc=sc, sq4=sq4: e.scalar_tensor_tensor(
                    out=dstn[:], in0=sq4, scalar=float(sc), in1=gains[:, gofs:gofs + 64].unsqueeze(1).broadcast_to([128, 4, 64]),
                    op0=ALU.mult, op1=ALU.mult), ["sqj", "gains"], [dres])
            for (srcn, sres, dstd, dres, trx, tres, pbk) in ((qn, "qn", mlQT, "mlQT", trq, "trq", 6), (kn, "kn", mlKT, "mlKT", trk, "trk", 0)):
                pbx = PS[pbk][:].bitcast(BF16)
                for h in range(8):
                    P.op("pe", lambda e, h=h, srcn=srcn, pbx=pbx: e.transpose(pbx[0:96, h * 128:(h + 1) * 128], srcn[:, h, :], ident_bf),
                         [sres, "cbf"], [psr(pbk)])
                P.op("act", lambda e, pbx=pbx, trx=trx: e.activation(out=trx[0:96, :], in_=pbx[0:96, :], func=AF.Copy), [psr(pbk)], [tres])
                P.dma(dstd[:, :, rows].rearrange("h d t -> d h t"), trx[0:96, :].rearrange("d (h t) -> d h t", h=8), [tres], [dres])
            for pr in range(2):
                P.op("pe", lambda e, pr=pr, pbt=pbt: e.transpose(pbt[:, pr * 128:(pr + 1) * 128], cqn[:, 2 * pr:2 * pr + 2, :].rearrange("p h d -> p (h d)"), ident_bf),
                     ["cqn", "cbf"], [psr(6)])
                P.op("pe", lambda e, pr=pr, pbt=pbt: e.transpose(pbt[:, (2 + pr) * 128:(3 + pr) * 128], ckn[:, 2 * pr:2 * pr + 2, :].rearrange("p h d -> p (h d)"), ident_bf),
                     ["ckn", "cbf"], [psr(6)])
            P.op("act", lambda e, pbt=pbt: e.activation(out=trc[:, 0:512], in_=pbt[:, 0:512], func=AF.Copy), [psr(6)], ["trc"])
            for pr in range(2):
                P.dma(ckQZ[2 * pr, 0:64, rows], trc[0:64, pr * 128:(pr + 1) * 128], ["trc"], ["ckQZ"])
                P.dma(ckQZ[2 * pr + 1, 64:128, rows], trc[64:128, pr * 128:(pr + 1) * 128], ["trc"], ["ckQZ"])
                P.dma(ckKT[pr, :, rows], trc[:, (2 + pr) * 128:(3 + pr) * 128], ["trc"], ["ckKT"])

        for tb in range(NB):
            t0 = tb * 512
            P.dma(xb[:], xsv[:, :, t0:t0 + 512], [xres_in], ["xb"])
            P.op("act", lambda e: e.activation(out=sq[:], in_=xb[:], func=AF.Square), ["xb"], ["sq"])
            for c in range(8):
                P.op("pe", lambda e, c=c: e.matmul(PS[0][:, :], lhsT=ones_bf[:], rhs=sq[:, c, :], start=(c == 0), stop=(c == 7)),
                     ["sq", "ones_bf"], [psr(0)])
            P.op("act", lambda e: e.activation(out=rstd[:], in_=PS[0][:, :], func=AF.Sqrt, scale=1.0 / D, bias=EPS), [psr(0)], ["rstd"])
            P.op("dve", lambda e: e.reciprocal(out=rstd[:], in_=rstd[:]), ["rstd"], ["rstd"])
            for c in range(8):
                P.op("dve", lambda e, c=c: e.scalar_tensor_tensor(out=xb[:, c, :], in0=xb[:, c, :], scalar=A1[:, l, c:c + 1], in1=rstd[:],
                                                                  op0=ALU.mult, op1=ALU.mult), ["xb", "A1", "rstd"], ["xb"])
                P.op("act", lambda e, c=c: e.activation(out=hT[:, c, :], in_=xb[:, c, :], func=AF.Identity, bias=mod[:, l, c:c + 1], scale=1.0),
                     ["xb", "mod"], ["hT"])
            for m in range(4):
                pb = 1 + (m % 2)
                for kc in range(8):
                    P.op("pe", lambda e, m=m, kc=kc, pb=pb: e.matmul(PS[pb][:, :], lhsT=win_bf[:, kc, m * 128:(m + 1) * 128], rhs=hT[:, kc, :],
                                                                    start=(kc == 0), stop=(kc == 7)), ["win_bf", "hT"], [psr(pb)])
                fb = m % 2
                if m < 2:
                    P.op("act", lambda e, pb=pb, fb=fb: e.activation(out=fmo[fb][:], in_=PS[pb][:, :], func=AF.Copy, scale=0.125),
                         [psr(pb)], [("fmo", fb)])
                    P.dma(sbQZ[2 * m, 0:64, t0:t0 + 512], fmo[fb][0:64, :], [("fmo", fb)], ["sbQZ"])
                    P.dma(sbQZ[2 * m + 1, 64:128, t0:t0 + 512], fmo[fb][64:128, :], [("fmo", fb)], ["sbQZ"])
                else:
                    P.op("act", lambda e, pb=pb, fb=fb: e.activation(out=fmo[fb][:], in_=PS[pb][:, :], func=AF.Copy),
                         [psr(pb)], [("fmo", fb)])
                    P.dma(sbKT[m - 2, :, t0:t0 + 512], fmo[fb][:, :], [("fmo", fb)], ["sbKT"])
            for i in range(4):
                jj = tb * 4 + i
                caps = []
                P.capture = []
                P.local = (LOCAL, jj % 2)
                tt_s1(tb, i, **tsets[jj % 2])
                caps.append(P.capture)
                if jj >= 1:
                    P.capture = []
                    P.local = (LOCAL, (jj - 1) % 2)
                    tt_s2((jj - 1) // 4, (jj - 1) % 4, **tsets[(jj - 1) % 2])
                    caps.append(P.capture)
                P.capture = None
                P.local = None
                P.replay_interleaved(caps)
        P.local = (LOCAL, (NT - 1) % 2)
        tt_s2((NT - 1) // 4, (NT - 1) % 4, **tsets[(NT - 1) % 2])
        P.local = None
        P.barrier()
        P.pop()
        if stop_after == ("P1", l):
            return True

        def norm_pass(o_t, Dg, gofs, row0):
            nch = Dg // 128
            onb = [P.sb([128, Dg], BF16) for _ in range(2)]
            stg = [P.sb([128, nch, 128], BF16) for _ in range(2)]
            gss = P.sb([128, 2], F32)
            gj = P.sb([128, Dg], F32)
            for m in range(NT):
                b = m % 2
                P.op("act", lambda e, m=m: e.activation(out=gj[:], in_=o_t[:, m, :], func=AF.Square, accum_out=gss[:, 0:1]), ["o_t"], ["gj", "gss"])
                P.op("act", lambda e: e.activation(out=gss[:, 1:2], in_=gss[:, 0:1], func=AF.Sqrt, scale=1.0 / Dg, bias=EPS), ["gss"], ["gss"])
                P.op("dve", lambda e: e.reciprocal(out=gss[:, 1:2], in_=gss[:, 1:2]), ["gss"], ["gss"])
                P.op("dve", lambda e, m=m, b=b: e.scalar_tensor_tensor(out=onb[b][:], in0=o_t[:, m, :], scalar=gss[:, 1:2],
                                                                       in1=gains[:, gofs:gofs + Dg], op0=ALU.mult, op1=ALU.mult),
                     ["o_t", "gss", "gains"], [("onb", b)])
                pbt = PS[7][:].bitcast(BF16)
                for k in range(nch):
                    P.op("pe", lambda e, k=k, b=b, pbt=pbt: e.transpose(pbt[:, k * 128:(k + 1) * 128], onb[b][:, k * 128:(k + 1) * 128], ident_bf),
                         [("onb", b), "cbf"], [psr(7)])
                P.op("act", lambda e, b=b, pbt=pbt: e.activation(out=stg[b][:].rearrange("p c t -> p (c t)"), in_=pbt[:, 0:nch * 128], func=AF.Copy),
                     [psr(7)], [("stg", b)])
                P.dma(mgT[row0:row0 + Dg, m * 128:(m + 1) * 128].rearrange("(c p) t -> p c t", p=128), stg[b][:], [("stg", b)], ["mgT"])

        P.push()
        QZ = P.sb([128, 4, S], BF16)
        KT = P.sb([128, 2, S], BF16)
        VA = P.sb([128, NT, 256], BF16)
        o_t = P.sb([128, NT, 256], F32)
        P.dma(QZ[:], sbQZ.rearrange("h p s -> p h s"), ["sbQZ"], ["QZ"])
        P.dma(KT[:], sbKT.rearrange("h p s -> p h s"), ["sbKT"], ["KT"])
        P.dma(VA[:], sbV.rearrange("(n p) c -> p n c", p=128), ["sbV"], ["VA"])
        Eb = [P.sb([128, 512], F32) for _ in range(2)]
        SPb = [P.sb([128, 512], BF16) for _ in range(2)]
        Wb = [P.sb([128, 512], BF16) for _ in range(2)]
        chi = [P.sb([128, 128], BF16) for _ in range(2)]
        clo = [P.sb([128, 128], BF16) for _ in range(2)]
        items = []
        for m in range(NT):
            for h in range(4):
                blocks = list(range(m, -1, -1))
                ng = (len(blocks) + 3) // 4
                for g in range(ng):
                    items.append((m, h, g, blocks[g * 4:(g + 1) * 4], g == ng - 1))
        Zp = [0, 1]
        LWp = [2, 3]
        CP = 4
        OP = [5, 6]

        def a_st1(it, i):
            m, h, g, blks, last = it
            par = i % 2
            n = len(blks)
            qs = slice(m * 128, (m + 1) * 128)
            for c, kb in enumerate(blks):
                P.op("pe", lambda e, c=c, kb=kb, h=h, par=par, qs=qs: e.matmul(
                    PS[Zp[par]][:, c * 128:(c + 1) * 128], lhsT=KT[:, h // 2, kb * 128:(kb + 1) * 128], rhs=QZ[:, h, qs], start=True, stop=True),
                    ["KT", "QZ"], [psr(Zp[par])])
            P.op("act", lambda e, par=par, n=n: e.activation(out=Eb[par][:, 0:n * 128], in_=PS[Zp[par]][:, 0:n * 128], func=AF.Exp),
                 [psr(Zp[par])], [("Eb", par)])
            P.op("act", lambda e, par=par, n=n: e.activation(out=SPb[par][:, 0:n * 128], in_=Eb[par][:, 0:n * 128], func=AF.Ln, bias=1.0),
                 [("Eb", par)], [("SPb", par)])
            if g == 0:
                P.op("dve", lambda e, par=par: e.tensor_tensor(out=SPb[par][:, 0:128], in0=SPb[par][:, 0:128], in1=strict_bf, op=ALU.mult),
                     [("SPb", par), "cbf"], [("SPb", par)])

        def a_st2(it, i):
            m, h, g, blks, last = it
            par = i % 2
            n = len(blks)
            qs = slice(m * 128, (m + 1) * 128)
            cpar = g % 2
            for c in range(n):
                P.op("pe", lambda e, c=c, par=par, g=g, n=n, last=last: e.matmul(
                    PS[CP][:, 0:128], lhsT=ones_bf[:], rhs=SPb[par][:, c * 128:(c + 1) * 128],
                    start=(g == 0 and c == 0), stop=(last and c == n - 1), skip_group_check=True), [("SPb", par), "ones_bf"], [psr(CP)])
            if not last:
                P.op("dve", lambda e, cpar=cpar: e.tensor_copy(out=chi[1 - cpar][:], in_=PS[CP][:, 0:128]),
                     [psr(CP)], [("chi", 1 - cpar)])
                P.op("dve", lambda e, cpar=cpar: e.tensor_tensor(out=clo[1 - cpar][:], in0=PS[CP][:, 0:128], in1=chi[1 - cpar][:], op=ALU.subtract),
                     [psr(CP), ("chi", 1 - cpar)], [("clo", 1 - cpar)])
            mms = []
            for c, kb in enumerate(blks):
                cs = slice(c * 128, (c + 1) * 128)
                mms.append((PS[LWp[par]][:, cs], KT[:, h // 2, kb * 128:(kb + 1) * 128], QZ[:, h, qs], ["KT", "QZ"]))
            mms.append((PS[LWp[par]][:, 0:n * 128], negut_bf, SPb[par][:, 0:n * 128], ["cbf", ("SPb", par)]))
            for c2 in range(n - 1):
                k = n - 1 - c2
                mms.append((PS[LWp[par]][:, (c2 + 1) * 128:n * 128].rearrange("p (k i) -> p k i", k=k), negones_bf[:],
                            SPb[par][:, c2 * 128:(c2 + 1) * 128].unsqueeze(1).broadcast_to([128, k, 128]), ["negones_bf", ("SPb", par)]))
            if g > 0:
                for ct, cres in ((chi, "chi"), (clo, "clo")):
                    mms.append((PS[LWp[par]][:, 0:n * 128].rearrange("p (k i) -> p k i", k=n), negid_bf[:],
                                ct[cpar][:].unsqueeze(1).broadcast_to([128, n, 128]), ["negid_bf", (cres, cpar)]))
            if g == 0:
                mms.append((PS[LWp[par]][:, 0:128], ident_bf, nma_bf, ["cbf"]))
            for k, (ot, lt, rh, rd) in enumerate(mms):
                P.op("pe", lambda e, ot=ot, lt=lt, rh=rh, k=k, nm=len(mms): e.matmul(
                    ot, lhsT=lt, rhs=rh, start=(k == 0), stop=(k == nm - 1), skip_group_check=True), rd, [psr(LWp[par])])
            P.op("act", lambda e, par=par, n=n: e.activation(out=Wb[par][:, 0:n * 128], in_=PS[LWp[par]][:, 0:n * 128], func=AF.Exp),
                 [psr(LWp[par])], [("Wb", par)])

        def a_st3(it, i):
            m, h, g, blks, last = it
            par = i % 2
            opar = (m * 4 + h) % 2
            n = len(blks)
            for c, kb in enumerate(blks):
                P.op("pe", lambda e, c=c, kb=kb, par=par, opar=opar, h=h: e.matmul(
                    PS[OP[opar]][:, 0:64], lhsT=Wb[par][:, c * 128:(c + 1) * 128], rhs=VA[:, kb, h * 64:(h + 1) * 64],
                    start=(g == 0 and c == 0), stop=(last and c == n - 1)), [("Wb", par), "VA"], [psr(OP[opar])])
            if last:
                P.op("dve", lambda e, opar=opar, m=m, h=h: e.tensor_copy(out=o_t[:, m, h * 64:(h + 1) * 64], in_=PS[OP[opar]][:, 0:64]),
                     [psr(OP[opar])], ["o_t"])

        NI = len(items)
        for i in range(NI + 2):
            if i < NI:
                a_st1(items[i], i)
            if 1 <= i <= NI:
                a_st2(items[i - 1], i - 1)
            if 2 <= i:
                a_st3(items[i - 2], i - 2)
        norm_pass(o_t, 256, G_OUT, 0)
        P.barrier()
        P.pop()

        def softmax_attn(nheads, Dg, load_head, blocks_of, add_of, gofs, row0):
            P.push()
            o_t = P.sb([128, NT, Dg], F32)
            Wb = [P.sb([128, 512], BF16) for _ in range(2)]
            rc = [P.sb([128, 1], F32) for _ in range(2)]
            hb = [load_head(b) for b in range(2)]
            for b in range(2):
                P.op("pool", lambda e, b=b: e.memset(hb[b][2][:, :, 64:65], 1.0), [], [("hv", b)])
            Sp = [0, 1]
            Op = [2, 3]
            items = []
            for h in range(nheads):
                for m in range(NT):
                    blocks = blocks_of(m)
                    ng = (len(blocks) + 3) // 4
                    for g in range(ng):
                        items.append((h, m, g, blocks[g * 4:(g + 1) * 4], g == ng - 1))

            def st1(it, i):
                h, m, g, blks, last = it
                par = i % 2
                b = h % 2
                qt, kt, vt, fill = hb[b]
                if m == 0 and g == 0:
                    fill(h, b)
                n = len(blks)
                qs = slice(m * 128, (m + 1) * 128)
                for c, kb in enumerate(blks):
                    cs = slice(c * 128, (c + 1) * 128)
                    ad = add_of(h, m, kb)
                    P.op("pe", lambda e, cs=cs, kb=kb, par=par, qs=qs, kt=kt, qt=qt, ad=ad: e.matmul(
                        PS[Sp[par]][:, cs], lhsT=kt[:, kb * 128:(kb + 1) * 128], rhs=qt[:, qs], start=True, stop=(ad is None or ad[0] != "pe")),
                        [("hk", b), ("hq", b)], [psr(Sp[par])])
                    if ad is not None and ad[0] == "pe":
                        P.op("pe", lambda e, cs=cs, par=par, ad=ad: e.matmul(PS[Sp[par]][:, cs], lhsT=ident_bf, rhs=ad[1], start=False, stop=True),
                             ["cbf"], [psr(Sp[par])])
                    elif ad is not None:
                        P.op("dve", lambda e, cs=cs, par=par, ad=ad: e.tensor_tensor(out=PS[Sp[par]][:, cs], in0=PS[Sp[par]][:, cs], in1=ad[1], op=ALU.add),
                             [psr(Sp[par]), "ckbm"], [psr(Sp[par])])
                P.op("act", lambda e, par=par, n=n: e.activation(out=Wb[par][:, 0:n * 128], in_=PS[Sp[par]][:, 0:n * 128], func=AF.Exp),
                     [psr(Sp[par])], [("Wb", par)])

            def st2(it, i):
                h, m, g, blks, last = it
                par = i % 2
                b = h % 2
                qt, kt, vt, fill = hb[b]
                opar = (h * NT + m) % 2
                n = len(blks)
                for c, kb in enumerate(blks):
                    P.op("pe", lambda e, c=c, kb=kb, par=par, opar=opar, vt=vt: e.matmul(
                        PS[Op[opar]][:, 0:65], lhsT=Wb[par][:, c * 128:(c + 1) * 128], rhs=vt[:, kb, :],
                        start=(g == 0 and c == 0), stop=(last and c == n - 1)), [("Wb", par), ("hv", b)], [psr(Op[opar])])
                if last:
                    P.op("dve", lambda e, opar=opar: e.reciprocal(out=rc[opar][:], in_=PS[Op[opar]][:, 64:65]), [psr(Op[opar])], [("rc", opar)])
                    P.op("dve", lambda e, opar=opar, m=m, h=h: e.tensor_scalar(out=o_t[:, m, h * 64:(h + 1) * 64], in0=PS[Op[opar]][:, 0:64],
                                                                              scalar1=rc[opar][:, 0:1], scalar2=None, op0=ALU.mult),
                         [psr(Op[opar]), ("rc", opar)], ["o_t"])

            NI = len(items)
            for i in range(NI + 1):
                if i < NI:
                    st1(items[i], i)
                if i >= 1:
                    st2(items[i - 1], i - 1)
            norm_pass(o_t, Dg, gofs, row0)
            P.barrier()
            P.pop()

        def mla_load(b):
            qt = P.sb([96, S], BF16)
            kt = P.sb([96, S], BF16)
            vt = P.sb([128, NT, 65], BF16)

            def fill(h, b):
                P.dma(qt[:], mlQT[h], ["mlQT"], [("hq", b)])
                P.dma(kt[:], mlKT[h], ["mlKT"], [("hk", b)])
                P.dma(vt[:, :, 0:64], mlV[:, h * 64:(h + 1) * 64].rearrange("(n p) c -> p n c", p=128), ["mlV"], [("hv", b)])
            return (qt, kt, vt, fill)

        softmax_attn(8, 512, mla_load, lambda m: list(range(m, -1, -1)),
                     lambda h, m, kb: (("pe", mb_bf) if kb == m else None), G_OUT + 256, 256)

        def ck_load(b):
            qt = P.sb([128, S], BF16)
            kt = P.sb([128, S], BF16)
            vt = P.sb([128, NT, 65], BF16)

            def fill(h, b):
                P.dma(qt[:], ckQZ[h], ["ckQZ"], [("hq", b)])
                P.dma(kt[:], ckKT[h // 2], ["ckKT"], [("hk", b)])
                P.dma(vt[:, :, 0:64], ckV[:, h * 64:(h + 1) * 64].rearrange("(n p) c -> p n c", p=128), ["ckV"], [("hv", b)])
            return (qt, kt, vt, fill)

        softmax_attn(4, 256, ck_load, lambda m: [kb for kb in range(m, m - 5, -1) if kb >= 0],
                     lambda h, m, kb: ("dve", ckbm[:, h * 5 + (kb - (m - 4)), :]), G_OUT + 768, 768)
        if stop_after == ("P2", l):
            return True

        P.pop()
        P.push()
        moe = (l % 2 == 1)
        li = l // 2
        NSB = TS // 512
        wo_bf = P.sb([128, 8, D], BF16)
        wgb = [P.sb([128, 8, 512], BF16) for _ in range(2)]
        wub = [P.sb([128, 8, 512], BF16) for _ in range(2)]
        wdb = [P.sb([128, 12, 512], BF16) for _ in range(2)]
        P.dma(wo_bf[:], w_out[l].rearrange("(kc p) n -> p kc n", p=128), ["w_out"], ["wo_bf"], q="pool")
        x1 = P.sb([128, 8, TS], F32)
        h2T = P.sb([128, 8, TS], BF16)
        h2f = P.sb([128, 8, 512], F32)
        mtb = P.sb([128, 8, 512], BF16)
        rstd = P.sb([128, 512], F32)
        AT = P.sb([128, 12, TS], BF16)
        sg = [P.sb([128, 512], BF16) for _ in range(2)]
        if moe:
            wr = P.sb([128, 8, NE], F32)
            P.dma(wr[:], router[li].rearrange("(kc p) n -> p kc n", p=128), ["router"], ["wr"])
            lg = P.sb([128, 4, NE], F32)
            lg2 = P.sb([128, 4, NE], F32)
            eq1 = P.sb([128, 4, NE], F32)
            eq2 = P.sb([128, 4, NE], F32)
            mx = P.sb([128, 4, 4], F32)
            gts = P.sb([128, 4, NE], F32)
            gT = P.sb([8, TS], F32)
            Gb = [P.sb([128, 512], F32) for _ in range(NSB)]
            ytmp = [P.sb([128, 512], F32) for _ in range(2)]
        gcount = [0, 0]
        for tsb in range(S // TS):
            for sbi in range(NSB):
                t0 = tsb * TS + sbi * 512
                xs = slice(sbi * 512, (sbi + 1) * 512)
                P.dma(x1[:, :, xs], xsv[:, :, t0:t0 + 512], [xres_in], [("x1", sbi)])
                P.dma(mtb[:], mgT.rearrange("(c p) s -> p c s", p=128)[:, :, t0:t0 + 512], ["mgT"], ["mtb"])
                for c in range(8):
                    pb = c % 2
                    for kc in range(8):
                        P.op("pe", lambda e, c=c, kc=kc, pb=pb: e.matmul(PS[pb][:, :], lhsT=wo_bf[:, kc, c * 128:(c + 1) * 128], rhs=mtb[:, kc, :],
                                                                        start=(kc == 0), stop=(kc == 7)), ["wo_bf", "mtb"], [psr(pb)])
                    P.op("dve", lambda e, c=c, pb=pb, xs=xs: e.scalar_tensor_tensor(out=x1[:, c, xs], in0=PS[pb][:, :], scalar=mod[:, l, 16 + c:17 + c],
                                                                                    in1=x1[:, c, xs], op0=ALU.mult, op1=ALU.add),
                         [psr(pb), "mod", ("x1", sbi)], [("x1", sbi)])
                P.op("act", lambda e, xs=xs: e.activation(out=h2T[:, :, xs], in_=x1[:, :, xs], func=AF.Square), [("x1", sbi)], [("h2T", sbi)])
                for c in range(8):
                    P.op("pe", lambda e, c=c, xs=xs: e.matmul(PS[2][:, :], lhsT=ones_bf[:], rhs=h2T[:, c, xs], start=(c == 0), stop=(c == 7)),
                         [("h2T", sbi), "ones_bf"], [psr(2)])
                P.op("act", lambda e: e.activation(out=rstd[:], in_=PS[2][:, :], func=AF.Sqrt, scale=1.0 / D, bias=EPS), [psr(2)], ["rstd"])
                P.op("dve", lambda e: e.reciprocal(out=rstd[:], in_=rstd[:]), ["rstd"], ["rstd"])
                for c in range(8):
                    P.op("dve", lambda e, c=c, xs=xs: e.scalar_tensor_tensor(out=h2f[:, c, :], in0=x1[:, c, xs], scalar=A2[:, l, c:c + 1], in1=rstd[:],
                                                                             op0=ALU.mult, op1=ALU.mult), [("x1", sbi), "A2", "rstd"], [("h2f", c)])
                    P.op("act", lambda e, c=c: e.activation(out=h2f[:, c, :], in_=h2f[:, c, :], func=AF.Identity, bias=mod[:, l, 24 + c:25 + c], scale=1.0),
                         [("h2f", c), "mod"], [("h2f", c)])
                    P.op("pool", lambda e, c=c, xs=xs: e.tensor_copy(out=h2T[:, c, xs], in_=h2f[:, c, :]), [("h2f", c)], [("h2T", sbi)])
                if moe:
                    H2F = [("h2f", c) for c in range(8)]
                    for i in range(4):
                        for kc in range(8):
                            P.op("pe", lambda e, i=i, kc=kc: e.matmul(PS[3][:, i * 8:(i + 1) * 8], lhsT=h2f[:, kc, i * 128:(i + 1) * 128], rhs=wr[:, kc, :],
                                                                      start=(kc == 0), stop=(kc == 7)), H2F + ["wr"], [psr(3)])
                    P.op("dve", lambda e: e.tensor_copy(out=lg[:].rearrange("p a b -> p (a b)"), in_=PS[3][:, 0:32]), [psr(3)], ["lg"])
                    P.op("dve", lambda e: e.tensor_reduce(out=mx[:, :, 0], in_=lg[:], axis=AX.X, op=ALU.max), ["lg"], ["mx"])
                    P.op("dve", lambda e: e.tensor_tensor(out=eq1[:], in0=lg[:], in1=mx[:, :, 0:1].broadcast_to([128, 4, NE]), op=ALU.is_equal),
                         ["lg", "mx"], ["eq1"])
                    P.op("dve", lambda e: e.scalar_tensor_tensor(out=lg2[:], in0=eq1[:], scalar=-1e30, in1=lg[:], op0=ALU.mult, op1=ALU.add),
                         ["eq1", "lg"], ["lg2"])
                    P.op("dve", lambda e: e.tensor_reduce(out=mx[:, :, 1], in_=lg2[:], axis=AX.X, op=ALU.max), ["lg2", "mx"], ["mx"])
                    P.op("dve", lambda e: e.tensor_tensor(out=eq2[:], in0=lg2[:], in1=mx[:, :, 1:2].broadcast_to([128, 4, NE]), op=ALU.is_equal),
                         ["lg2", "mx"], ["eq2"])
                    P.op("dve", lambda e: e.tensor_tensor(out=mx[:, :, 2], in0=mx[:, :, 1], in1=mx[:, :, 0], op=ALU.subtract), ["mx"], ["mx"])
                    P.op("act", lambda e: e.activation(out=mx[:, :, 2], in_=mx[:, :, 2], func=AF.Exp), ["mx"], ["mx"])
                    P.op("dve", lambda e: e.tensor_scalar(out=mx[:, :, 2], in0=mx[:, :, 2], scalar1=1.0, scalar2=None, op0=ALU.add), ["mx"], ["mx"])
                    P.op("dve", lambda e: e.reciprocal(out=mx[:, :, 2], in_=mx[:, :, 2]), ["mx"], ["mx"])
                    P.op("dve", lambda e: e.tensor_scalar(out=mx[:, :, 3], in0=mx[:, :, 2], scalar1=-1.0, scalar2=1.0, op0=ALU.mult, op1=ALU.add), ["mx"], ["mx"])
                    P.op("dve", lambda e: e.tensor_tensor(out=eq1[:], in0=eq1[:], in1=mx[:, :, 2:3].broadcast_to([128, 4, NE]), op=ALU.mult), ["eq1", "mx"], ["eq1"])
                    P.op("dve", lambda e: e.tensor_tensor(out=eq2[:], in0=eq2[:], in1=mx[:, :, 3:4].broadcast_to([128, 4, NE]), op=ALU.mult), ["eq2", "mx"], ["eq2"])
                    P.op("dve", lambda e: e.tensor_tensor(out=gts[:], in0=eq1[:], in1=eq2[:], op=ALU.add), ["eq1", "eq2"], ["gts"])
                    for i in range(4):
                        P.op("pe", lambda e, i=i: e.transpose(PS[2][0:8, i * 128:(i + 1) * 128], gts[:, i, :], ident_f), ["gts", "cst"], [psr(2)])
                    P.op("act", lambda e, xs=xs: e.activation(out=gT[:, xs], in_=PS[2][0:8, :], func=AF.Copy), [psr(2)], [("gT", sbi)])
                    if debug:
                        P.dma(dbgG[:, t0:t0 + 512], gT[:, xs], [("gT", sbi)], ["dbgG"])
            nexp = NE if moe else 1
            for ex in range(nexp):
                if moe:
                    wg_d, wu_d, wd_d = moe_g[li, ex], moe_u[li, ex], moe_d[li, ex]
                else:
                    wg_d, wu_d, wd_d = ffn_g[li], ffn_u[li], ffn_d[li]
                wgv = wg_d.rearrange("(kc p) n -> p kc n", p=128)
                wuv = wu_d.rearrange("(kc p) n -> p kc n", p=128)
                wdv = wd_d.rearrange("(j p) n -> p j n", p=128)
                for pas, (j0, njp, groups) in enumerate(((0, 12, ((0, 4), (4, 4), (8, 4))), (12, 10, ((12, 4), (16, 4), (20, 2))))):
                    for (js, nj) in groups:
                        wbuf = gcount[0] % 2
                        gcount[0] += 1
                        P.dma(wgb[wbuf][:, :, 0:nj * 128], wgv[:, :, js * 128:(js + nj) * 128], ["wg_d"], [("wgb", wbuf)], q="pool")
                        P.dma(wub[wbuf][:, :, 0:nj * 128], wuv[:, :, js * 128:(js + nj) * 128], ["wu_d"], [("wub", wbuf)], q="pool")
                        for jj in range(nj):
                            jl = js + jj - j0
                            for sbi in range(NSB):
                                xs = slice(sbi * 512, (sbi + 1) * 512)
                                par = (jj * NSB + sbi) % 2
                                gp, up = 4 + par, 6 + par
                                for kc in range(8):
                                    P.op("pe", lambda e, kc=kc, jj=jj, wbuf=wbuf, xs=xs, gp=gp: e.matmul(
                                        PS[gp][:, :], lhsT=wgb[wbuf][:, kc, jj * 128:(jj + 1) * 128], rhs=h2T[:, kc, xs], start=(kc == 0), stop=(kc == 7)),
                                        [("wgb", wbuf), ("h2T", sbi)], [psr(gp)])
                                for kc in range(8):
                                    P.op("pe", lambda e, kc=kc, jj=jj, wbuf=wbuf, xs=xs, up=up: e.matmul(
                                        PS[up][:, :], lhsT=wub[wbuf][:, kc, jj * 128:(jj + 1) * 128], rhs=h2T[:, kc, xs], start=(kc == 0), stop=(kc == 7)),
                                        [("wub", wbuf), ("h2T", sbi)], [psr(up)])
                                P.op("act", lambda e, par=par, gp=gp: e.activation(out=sg[par][:], in_=PS[gp][:, :], func=AF.Silu), [psr(gp)], [("sg", par)])
                                P.op("dve", lambda e, par=par, up=up, jl=jl, xs=xs: e.tensor_tensor(out=AT[:, jl, xs], in0=PS[up][:, :], in1=sg[par][:], op=ALU.mult),
                                     [psr(up), ("sg", par)], [("AT", sbi)])
                    for dh in range(2):
                        wdbuf = gcount[1] % 2
                        gcount[1] += 1
                        P.dma(wdb[wdbuf][:, 0:njp, :], wdv[:, j0:j0 + njp, dh * 512:(dh + 1) * 512], ["wd_d"], [("wdb", wdbuf)], q="pool")
                        for cc in range(4):
                            c = dh * 4 + cc
                            for sbi in range(NSB):
                                xs = slice(sbi * 512, (sbi + 1) * 512)
                                par = (cc * NSB + sbi) % 2
                                dp = par
                                for jl in range(njp):
                                    P.op("pe", lambda e, jl=jl, wdbuf=wdbuf, xs=xs, dp=dp, cc=cc, njp=njp: e.matmul(
                                        PS[dp][:, :], lhsT=wdb[wdbuf][:, jl, cc * 128:(cc + 1) * 128], rhs=AT[:, jl, xs], start=(jl == 0), stop=(jl == njp - 1)),
                                        [("wdb", wdbuf), ("AT", sbi)], [psr(dp)])
                                if not moe:
                                    P.op("dve", lambda e, c=c, xs=xs, dp=dp: e.scalar_tensor_tensor(
                                        out=x1[:, c, xs], in0=PS[dp][:, :], scalar=mod[:, l, 40 + c:41 + c], in1=x1[:, c, xs], op0=ALU.mult, op1=ALU.add),
                                        [psr(dp), "mod", ("x1", sbi)], [("x1", sbi)])
                                else:
                                    if pas == 0 and dh == 0 and cc == 0:
                                        P.op("pe", lambda e, xs=xs, sbi=sbi, ex=ex: e.matmul(
                                            PS[2 + sbi % 2][:, :], lhsT=cst[0:8, C_SEL + ex * 128:C_SEL + (ex + 1) * 128], rhs=gT[:, xs], start=True, stop=True),
                                            ["cst", ("gT", sbi)], [psr(2 + sbi % 2)])
                                        P.op("act", lambda e, sbi=sbi: e.activation(out=Gb[sbi][:], in_=PS[2 + sbi % 2][:, :], func=AF.Copy),
                                             [psr(2 + sbi % 2)], [("Gb", sbi)])
                                    P.op("dve", lambda e, c=c, dp=dp, sbi=sbi, par=par: e.scalar_tensor_tensor(
                                        out=ytmp[par][:], in0=PS[dp][:, :], scalar=mod[:, l, 40 + c:41 + c], in1=Gb[sbi][:], op0=ALU.mult, op1=ALU.mult),
                                        [psr(dp), "mod", ("Gb", sbi)], [("ytmp", par)])
                                    P.op("dve", lambda e, c=c, xs=xs, par=par: e.tensor_tensor(out=x1[:, c, xs], in0=x1[:, c, xs], in1=ytmp[par][:], op=ALU.add),
                                         [("ytmp", par), ("x1", sbi)], [("x1", sbi)])
            for sbi in range(NSB):
                t0 = tsb * TS + sbi * 512
                xs = slice(sbi * 512, (sbi + 1) * 512)
                P.dma(xdv[:, :, t0:t0 + 512], x1[:, :, xs], [("x1", sbi)], [xres_out])
        P.barrier()
        P.pop()

    for l in range(nlayers):
        if do_layer(l):
            break

    P.emit(final_waits=["yT"])
    return nc


def host_inputs(inp, S):
    NT = S // 128
    B = inp["x"].shape[0]
    f = lambda a: np.ascontiguousarray(np.asarray(a, dtype=np.float32))
    consts = make_consts()
    L = inp["w_in"].shape[0]
    vec = np.zeros((L, 128, 64), np.float32)
    gains = np.zeros((L, 128, NGAIN), np.float32)
    ckb = np.zeros((L, 128, 20, 128), np.float32)
    jj = np.arange(128)[:, None]
    ii = np.arange(128)[None, :]
    for l in range(L):
        vec[l, :, 0:48] = np.asarray(inp["ada_b"][l]).reshape(48, 128).T
        vec[l, :, 48:56] = np.asarray(inp["norm_mix"][l]).reshape(8, 128).T
        vec[l, :, 56:64] = np.asarray(inp["norm_ffn"][l]).reshape(8, 128).T
        g = np.concatenate([np.asarray(inp[k][l]) for k in ("mla_q_norm", "mla_kv_norm", "mla_q_qknorm", "mla_k_qknorm",
                                                            "ck_q_qknorm", "ck_k_qknorm", "group_out_norm")])
        gains[l] = np.broadcast_to(g[None, :], (128, NGAIN))
        rb = np.asarray(inp["ck_rel_bias"][l])
        for h in range(4):
            for o in range(5):
                idx = np.clip(128 * (4 - o) + ii - jj, -128, 128) + 128
                ckb[l, :, h * 5 + o, :] = rb[h][idx]
    shared = dict(consts=consts, vec=vec, gains=gains, ckbias=ckb)
    for k in ("ada_w", "w_in", "w_q_up", "w_kv_up", "w_out", "ffn_w_gate", "ffn_w_up", "ffn_w_down", "moe_router",
              "moe_w_gate", "moe_w_up", "moe_w_down"):
        shared[k] = f(inp[k])
    maps = []
    for b in range(B):
        m = dict(shared)
        m["xT"] = np.ascontiguousarray(np.asarray(inp["x"][b], np.float32).T)
        m["cT"] = np.ascontiguousarray(np.asarray(inp["c"][b], np.float32).reshape(8, 128).T)
        m["pos"] = np.ascontiguousarray(np.asarray(inp["positions"][b], np.int32).reshape(NT, 128).T)
        maps.append(m)
    return maps


_NC_CACHE = {}


def kernel(**inputs):
    S = inputs["x"].shape[1]
    B = inputs["x"].shape[0]
    key = (S,)
    if key not in _NC_CACHE:
        _NC_CACHE[key] = build(S)
    nc = _NC_CACHE[key]
    maps = host_inputs(inputs, S)
    res = run_bass_kernel_spmd(nc, maps, core_ids=list(range(B)))
    out = np.stack([np.ascontiguousarray(res.results[b]["yT"].T) for b in range(B)], axis=0)
    return out.astype(np.float32)
```

```python
import math
import numpy as np
from contextlib import ExitStack
import concourse.bass as bass
import concourse.mybir as mybir
from concourse.bass_utils import run_bass_kernel_spmd

F32 = mybir.dt.float32
BF16 = mybir.dt.bfloat16
I32 = mybir.dt.int32
AF = mybir.ActivationFunctionType
ALU = mybir.AluOpType
AX = mybir.AxisListType

D = 1024
DFF = 2816
NJ = DFF // 128
NE = 8
EPS = 1e-6
NEG = -30000.0
COMPUTE = ("pe", "act", "dve", "pool")


class Prog:
    def __init__(self, nc):
        self.nc = nc
        self.stacks = [ExitStack()]
        self.ins = {e: [] for e in COMPUTE + ("sp",)}
        self.res_w = {}
        self.res_r = {}
        self.dma_cnt = {}
        self.sems = {}
        self.ntile = 0
        self.pending = {}
        self.local = None
        self.capture = None

    def replay_interleaved(self, lists):
        self.local = None
        pos = [0] * len(lists)
        tot = max(len(x) for x in lists) if lists else 0
        for step in range(tot):
            for li, lst in enumerate(lists):
                upto = ((step + 1) * len(lst) + tot - 1) // tot
                while pos[li] < min(upto, len(lst)):
                    rec = lst[pos[li]]
                    pos[li] += 1
                    if rec[0] == "op":
                        self.op(rec[1], rec[2], rec[3], rec[4])
                    else:
                        self.dma(rec[1], rec[2], rec[3], rec[4], q=rec[5])

    def _rn(self, rs):
        if self.local is None:
            return list(rs)
        names, sfx = self.local
        out = []
        for r in rs:
            base = r[0] if isinstance(r, tuple) else r
            out.append((r, sfx) if base in names else r)
        return out

    def push(self):
        self.stacks.append(ExitStack())

    def pop(self):
        self.stacks.pop().close()

    def sb(self, shape, dt, name=None):
        self.ntile += 1
        return self.stacks[-1].enter_context(self.nc.sbuf_tensor(f"t{self.ntile}", list(shape), dt))

    def ps(self, shape, dt=F32):
        self.ntile += 1
        return self.stacks[0].enter_context(self.nc.psum_tensor(f"p{self.ntile}", list(shape), dt))

    def _deps(self, eng, reads, writes):
        deps = {}

        def add(d):
            for k, v in d.items():
                if deps.get(k, -1) < v:
                    deps[k] = v
        for r in reads:
            add(self.res_w.get(r, {}))
        for w in writes:
            add(self.res_w.get(w, {}))
            add(self.res_r.get(w, {}))
        if eng in self.pending:
            add(self.pending.pop(eng))
        return deps

    def barrier(self, engines=COMPUTE + ("sp",)):
        d = {}
        for e in COMPUTE:
            if self.ins[e]:
                for i in range(len(self.ins[e]) - 1, -1, -1):
                    if self.ins[e][i]["kind"] == "op":
                        d[("E", e)] = i
                        break
        for res, cnt in self.dma_cnt.items():
            d[("D", res)] = cnt
        for e in engines:
            cur = self.pending.setdefault(e, {})
            for k, v in d.items():
                if cur.get(k, -1) < v:
                    cur[k] = v

    def op(self, eng, fn, reads=(), writes=()):
        reads = self._rn(reads)
        writes = self._rn(writes)
        if self.capture is not None:
            self.capture.append(("op", eng, fn, reads, writes))
            return None
        lst = self.ins[eng]
        idx = len(lst)
        deps = self._deps(eng, reads, writes)
        if eng == "pe":
            deps.pop(("E", "pe"), None)
        rec = dict(kind="op", fn=fn, deps=deps, signal=False)
        lst.append(rec)
        key = ("E", eng)
        for r in reads:
            self.res_r.setdefault(r, {})[key] = idx
        for w in writes:
            self.res_w[w] = {key: idx}
            self.res_r[w] = {}
        return rec

    def dma(self, out_ap, in_ap, reads, writes, q="sp"):
        reads = self._rn(reads)
        writes = self._rn(writes)
        if self.capture is not None:
            self.capture.append(("dma", out_ap, in_ap, reads, writes, q))
            return None
        lst = self.ins[q]
        dst = writes[0]
        deps = self._deps(q, reads, writes)
        skey = ("D", dst)
        deps.pop(skey, None)
        cnt = self.dma_cnt.get(dst, 0) + 16
        self.dma_cnt[dst] = cnt
        rec = dict(kind="dma", out=out_ap, in_=in_ap, deps=deps, skey=skey)
        lst.append(rec)
        for r in reads:
            self.res_r.setdefault(r, {})[skey] = cnt
        for w in writes:
            d = self.res_w.setdefault(w, {})
            if set(d.keys()) - {skey}:
                d = {}
                self.res_w[w] = d
            d[skey] = cnt
            self.res_r[w] = {}
        return rec

    def emit(self, final_waits=()):
        nc = self.nc
        es = self.stacks[0]
        for e in self.ins:
            for rec in self.ins[e]:
                for (kind, k), v in rec["deps"].items():
                    if kind == "E":
                        self.ins[k][v]["signal"] = True
        for e in COMPUTE:
            c = 0
            for rec in self.ins[e]:
                if rec.get("signal"):
                    c += 1
                    rec["sigval"] = c
        keys = [("E", e) for e in COMPUTE] + sorted({rec["skey"] for e in self.ins for rec in self.ins[e]
                                                     if rec["kind"] == "dma"}, key=str)
        for i, k in enumerate(keys):
            self.sems[k] = es.enter_context(nc.semaphore(f"s{i}"))
        final = dict(self.dma_cnt)
        with nc.Block() as block:
            def mk(ename, lists):
                def body(eng):
                    waited = {}
                    for rec in lists:
                        for (kind, k), v in rec["deps"].items():
                            val = self.ins[k][v]["sigval"] if kind == "E" else v
                            key = (kind, k)
                            if waited.get(key, -1) >= val:
                                continue
                            waited[key] = val
                            eng.wait_ge(self.sems[key], val)
                        if rec["kind"] == "op":
                            inst = rec["fn"](eng)
                            if rec["signal"]:
                                inst.then_inc(self.sems[("E", ename)], 1)
                        else:
                            inst = eng.dma_start(out=rec["out"], in_=rec["in_"])
                            inst.then_inc(self.sems[rec["skey"]], 16)
                    if ename == "sp":
                        for res in final_waits:
                            eng.wait_ge(self.sems[("D", res)], final[res])
                return body
            block.tensor(mk("pe", self.ins["pe"]))
            block.scalar(mk("act", self.ins["act"]))
            block.vector(mk("dve", self.ins["dve"]))
            block.gpsimd(mk("pool", self.ins["pool"]))
            block.sync(mk("sp", self.ins["sp"]))
        while self.stacks:
            self.stacks.pop().close()


C_ID, C_NUT, C_STRICT, C_NMA, C_MB, C_CK0, C_CK4, C_SEL = [i * 128 for i in range(8)]
NCONST = 7 * 128 + 8 * 128
INV_FREQ = (np.float32(10000.0) ** (-np.arange(0, 32, 2, dtype=np.float32) / np.float32(32))).astype(np.float32)


def make_consts():
    j = np.arange(128)[:, None]
    i = np.arange(128)[None, :]
    c = np.zeros((128, NCONST), np.float32)
    c[:, C_ID:C_ID + 128] = (j == i)
    c[:, C_NUT:C_NUT + 128] = -(j >= i).astype(np.float32)
    c[:, C_STRICT:C_STRICT + 128] = (j < i)
    c[:, C_NMA:C_NMA + 128] = np.where(j >= i, NEG, 0.0)
    c[:, C_MB:C_MB + 128] = np.where((j >= 64) & (i < 64), NEG, 0.0)
    c[:, C_CK0:C_CK0 + 128] = np.where((i >= 64) & (j < 64), NEG, 0.0)
    c[:, C_CK4:C_CK4 + 128] = np.where((i < 64) & (j >= 64), NEG, 0.0)
    for e in range(8):
        c[e, C_SEL + e * 128:C_SEL + (e + 1) * 128] = 1.0
    return c


G_QN, G_KVN, G_QQK, G_KQK, G_CQ, G_CK, G_OUT = 0, 256, 384, 480, 576, 640, 704
NGAIN = 704 + 1024


def build(S, nlayers=2, debug=False, TS=1024, stop_after=None):
    NT = S // 128
    NB = S // 512
    TS = min(TS, S)
    nc = bass.Bass("TRN2", target_bir_lowering=False)
    P = Prog(nc)

    def din(name, shape, dt=F32):
        return nc.dram_tensor(name, list(shape), dt, kind="ExternalInput").ap()

    def dscr(name, shape, dt):
        return nc.dram_tensor(name, list(shape), dt, kind=("ExternalOutput" if debug else "Internal")).ap()

    xT_in = din("xT", [D, S])
    cT_d = din("cT", [128, 8])
    pos_d = din("pos", [128, NT], I32)
    consts_d = din("consts", [128, NCONST])
    vec_d = din("vec", [2, 128, 64])
    gains_d = din("gains", [2, 128, NGAIN])
    ckb_d = din("ckbias", [2, 128, 20, 128])
    ada_w = din("ada_w", [2, D, 6 * D])
    w_in = din("w_in", [2, D, 1952])
    w_q_up = din("w_q_up", [2, 256, 768])
    w_kv_up = din("w_kv_up", [2, 128, 1024])
    w_out = din("w_out", [2, D, D])
    ffn_g = din("ffn_w_gate", [1, D, DFF])
    ffn_u = din("ffn_w_up", [1, D, DFF])
    ffn_d = din("ffn_w_down", [1, DFF, D])
    router = din("moe_router", [1, D, NE])
    moe_g = din("moe_w_gate", [1, NE, D, DFF])
    moe_u = din("moe_w_up", [1, NE, D, DFF])
    moe_d = din("moe_w_down", [1, NE, DFF, D])
    yT = nc.dram_tensor("yT", [D, S], F32, kind="ExternalOutput").ap()

    xT_mid = dscr("xT_mid", [D, S], F32)
    sbQZ = dscr("sbQZ", [4, 128, S], BF16)
    sbKT = dscr("sbKT", [2, 128, S], BF16)
    sbV = dscr("sbV", [S, 256], BF16)
    mlQT = dscr("mlQT", [8, 96, S], BF16)
    mlKT = dscr("mlKT", [8, 96, S], BF16)
    mlV = dscr("mlV", [S, 512], BF16)
    ckQZ = dscr("ckQZ", [4, 128, S], BF16)
    ckKT = dscr("ckKT", [2, 128, S], BF16)
    ckV = dscr("ckV", [S, 256], BF16)
    mgT = dscr("mgT", [D, S], BF16)
    rope_d = dscr("rope_d", [2, 128, NT, 16], F32)
    dbgG = dscr("dbgG", [8, S], F32) if debug else None

    cst = P.sb([128, NCONST], F32)
    cbf = P.sb([128, 5 * 128], BF16)
    ones_bf = P.sb([128, 128], BF16)
    negones_bf = P.sb([128, 128], BF16)
    negrow = P.sb([1, 128], F32)
    zeros_bf = P.sb([128, 512], BF16)
    negid_bf = P.sb([128, 128], BF16)
    cact = P.sb([128, 8], F32)
    vec = P.sb([128, 2, 64], F32)
    mod = P.sb([128, 2, 48], F32)
    A1 = P.sb([128, 2, 8], F32)
    A2 = P.sb([128, 2, 8], F32)
    PS = [P.ps([128, 512], F32) for _ in range(8)]

    def psr(k):
        return ("ps", k)

    ident_bf = cbf[:, 0:128]
    negut_bf = cbf[:, 128:256]
    strict_bf = cbf[:, 256:384]
    nma_bf = cbf[:, 384:512]
    mb_bf = cbf[:, 512:640]
    ident_f = cst[:, C_ID:C_ID + 128]

    P.dma(cst[:], consts_d[:, :], ["consts_d"], ["cst"])
    P.dma(cact[:], cT_d[:, :], ["cT_d"], ["cact"])
    P.dma(vec[:], vec_d.rearrange("l p c -> p l c"), ["vec_d"], ["vec"])
    P.op("dve", lambda e: e.tensor_copy(out=cbf[:], in_=cst[:, 0:640]), ["cst"], ["cbf"])
    P.op("pool", lambda e: e.memset(ones_bf[:], 1.0), [], ["ones_bf"])
    P.op("pool", lambda e: e.memset(negones_bf[:], -1.0), [], ["negones_bf"])
    P.op("pool", lambda e: e.memset(negrow[:], -1.0), [], ["negrow"])
    P.op("pool", lambda e: e.memset(zeros_bf[:], 0.0), [], ["zeros_bf"])
    P.op("dve", lambda e: e.tensor_scalar(out=negid_bf[:], in0=cst[:, C_ID:C_ID + 128], scalar1=-1.0, scalar2=None, op0=ALU.mult), ["cst"], ["negid_bf"])
    P.op("act", lambda e: e.activation(out=cact[:], in_=cact[:], func=AF.Silu), ["cact"], ["cact"])
    for zt, zres in ((sbQZ, "sbQZ"), (ckQZ, "ckQZ")):
        for h in range(4):
            lo = 64 if h % 2 == 0 else 0
            for tbz in range(NB):
                P.dma(zt[h, lo:lo + 64, tbz * 512:(tbz + 1) * 512], zeros_bf[lo:lo + 64, :], ["zeros_bf"], [zres])

    P.push()
    awb = [P.sb([128, 8, 512], F32) for _ in range(2)]
    for l in range(nlayers):
        awv = ada_w[l].rearrange("(kc p) n -> p kc n", p=128)
        for g in range(12):
            b = g % 2
            P.dma(awb[b][:], awv[:, :, g * 512:(g + 1) * 512], ["ada_w"], [("awb", b)])
            for m in range(4):
                col = g * 4 + m
                for kc in range(8):
                    P.op("pe", lambda e, b=b, m=m, kc=kc, col=col: e.matmul(
                        PS[0][:, col:col + 1], lhsT=awb[b][:, kc, m * 128:(m + 1) * 128], rhs=cact[:, kc:kc + 1],
                        start=(kc == 0), stop=(kc == 7)), [("awb", b), "cact"], [psr(0)])
        P.op("dve", lambda e, l=l: e.tensor_tensor(out=mod[:, l, :], in0=PS[0][:, 0:48], in1=vec[:, l, 0:48], op=ALU.add),
             [psr(0), "vec"], ["mod"])
        P.op("dve", lambda e, l=l: e.scalar_tensor_tensor(out=A1[:, l, :], in0=mod[:, l, 8:16], scalar=1.0, in1=vec[:, l, 48:56],
                                                          op0=ALU.add, op1=ALU.mult), ["mod", "vec"], ["A1"])
        P.op("dve", lambda e, l=l: e.scalar_tensor_tensor(out=A2[:, l, :], in0=mod[:, l, 32:40], scalar=1.0, in1=vec[:, l, 56:64],
                                                          op0=ALU.add, op1=ALU.mult), ["mod", "vec"], ["A2"])

    cos_t = P.sb([128, NT, 16], F32)
    sin_t = P.sb([128, NT, 16], F32)
    posi = P.sb([128, NT], I32)
    posf = P.sb([128, NT], F32)
    ang = P.sb([128, NT, 16], F32)
    u = P.sb([128, NT, 16], F32)
    ki = P.sb([128, NT, 16], I32)
    kf = P.sb([128, NT, 16], F32)
    r = P.sb([128, NT, 16], F32)
    fx = P.sb([128, NT, 16], F32)
    P.dma(posi[:], pos_d[:, :], ["pos_d"], ["posi"])
    P.op("dve", lambda e: e.tensor_copy(out=posf[:], in_=posi[:]), ["posi"], ["posf"])
    for i in range(16):
        P.op("dve", lambda e, i=i: e.tensor_scalar(out=ang[:, :, i], in0=posf[:], scalar1=float(INV_FREQ[i]), scalar2=None,
                                                   op0=ALU.mult), ["posf"], ["ang"])
    TWO_PI = 2.0 * math.pi
    C1 = 6.28125
    C2 = TWO_PI - C1
    for which, dst in ((0, sin_t), (1, cos_t)):
        src = ang
        if which == 1:
            P.op("dve", lambda e: e.tensor_scalar(out=r[:], in0=ang[:], scalar1=math.pi / 2, scalar2=None, op0=ALU.add),
                 ["ang"], ["r"])
            src = r
        P.op("dve", lambda e, src=src: e.tensor_scalar(out=u[:], in0=src[:], scalar1=1.0 / TWO_PI, scalar2=None, op0=ALU.mult),
             ["ang", "r"], ["u"])
        P.op("dve", lambda e: e.tensor_copy(out=ki[:], in_=u[:]), ["u"], ["ki"])
        P.op("dve", lambda e: e.tensor_copy(out=kf[:], in_=ki[:]), ["ki"], ["kf"])
        P.op("dve", lambda e, src=src: e.scalar_tensor_tensor(out=u[:], in0=kf[:], scalar=-C1, in1=src[:], op0=ALU.mult, op1=ALU.add),
             ["kf", "ang", "r"], ["u"])
        P.op("dve", lambda e: e.scalar_tensor_tensor(out=r[:], in0=kf[:], scalar=-C2, in1=u[:], op0=ALU.mult, op1=ALU.add),
             ["kf", "u"], ["r"])
        P.op("dve", lambda e: e.tensor_scalar(out=fx[:], in0=r[:], scalar1=math.pi, scalar2=-TWO_PI, op0=ALU.is_gt, op1=ALU.mult),
             ["r"], ["fx"])
        P.op("dve", lambda e: e.tensor_tensor(out=r[:], in0=r[:], in1=fx[:], op=ALU.add), ["r", "fx"], ["r"])
        P.op("dve", lambda e: e.tensor_scalar(out=fx[:], in0=r[:], scalar1=-math.pi, scalar2=TWO_PI, op0=ALU.is_lt, op1=ALU.mult),
             ["r"], ["fx"])
        P.op("dve", lambda e: e.tensor_tensor(out=r[:], in0=r[:], in1=fx[:], op=ALU.add), ["r", "fx"], ["r"])
        P.op("act", lambda e, dst=dst: e.activation(out=dst[:], in_=r[:], func=AF.Sin), ["r"], ["rope_t0"])
    P.dma(rope_d[0], cos_t[:], ["rope_t0"], ["rope_d"])
    P.dma(rope_d[1], sin_t[:], ["rope_t0"], ["rope_d"])
    P.barrier()
    P.pop()

    def do_layer(l):
        x_src = xT_in if l == 0 else xT_mid
        x_dst = yT if l == nlayers - 1 else xT_mid
        xres_in = "xT_in" if l == 0 else "xT_mid"
        xres_out = "yT" if l == nlayers - 1 else "xT_mid"
        xsv = x_src.rearrange("(c p) s -> p c s", p=128)
        xdv = x_dst.rearrange("(c p) s -> p c s", p=128)

        P.push()
        cos_t = P.sb([128, NT, 16], F32)
        sin_t = P.sb([128, NT, 16], F32)
        gains = P.sb([128, NGAIN], F32)
        ckbm = P.sb([128, 20, 128], F32)
        P.dma(cos_t[:], rope_d[0], ["rope_d"], ["rope_t"])
        P.dma(sin_t[:], rope_d[1], ["rope_d"], ["rope_t"])
        P.dma(gains[:], gains_d[l], ["gains_d"], ["gains"])
        P.dma(ckbm[:], ckb_d[l], ["ckb_d"], ["ckbm"])
        for h in range(4):
            P.op("dve", lambda e, h=h: e.tensor_tensor(out=ckbm[:, h * 5 + 0, :], in0=ckbm[:, h * 5 + 0, :],
                                                       in1=cst[:, C_CK0:C_CK0 + 128], op=ALU.add), ["ckbm", "cst"], ["ckbm"])
            P.op("dve", lambda e, h=h: e.tensor_tensor(out=ckbm[:, h * 5 + 4, :], in0=ckbm[:, h * 5 + 4, :],
                                                       in1=cst[:, C_CK4:C_CK4 + 128], op=ALU.add), ["ckbm", "cst"], ["ckbm"])

        P.push()
        win_bf = P.sb([128, 8, 1952], BF16)
        wq_bf = P.sb([128, 2, 768], BF16)
        wkv_bf = P.sb([128, 1024], BF16)
        wiv = w_in[l].rearrange("(kc p) n -> p kc n", p=128)
        for kc in range(8):
            P.dma(win_bf[:, kc, :], wiv[:, kc, :], ["w_in"], ["win_bf"], q="pool")
        P.dma(wq_bf[:], w_q_up[l].rearrange("(kc p) n -> p kc n", p=128), ["w_q_up"], ["wq_bf"], q="pool")
        P.dma(wkv_bf[:], w_kv_up[l], ["w_kv_up"], ["wkv_bf"], q="pool")

        xb = P.sb([128, 8, 512], F32)
        sq = P.sb([128, 8, 512], BF16)
        rstd = P.sb([128, 512], F32)
        hT = P.sb([128, 8, 512], BF16)
        fmo = [P.sb([128, 512], BF16) for _ in range(2)]
        LOCAL = {"proj", "vst", "junk", "ss", "ss2", "lat", "latT", "qf", "kvf", "kf32", "kper", "t16", "ssh", "sqj", "qn", "kn",
                 "cqn", "ckn", "trq", "trk", "trc", "mlv"}

        def alloc_tile_set():
            return dict(
                proj=P.sb([128, 1440], F32), vst=[P.sb([128, 256], BF16) for _ in range(2)], junk=P.sb([128, 256], F32),
                ss=P.sb([128, 4], F32), lat=P.sb([128, 384], BF16), latT=P.sb([128, 3, 128], BF16), qf=P.sb([128, 8, 96], F32),
                kvf=P.sb([128, 8, 128], F32), kf32=P.sb([128, 8, 96], F32), kper=P.sb([128, 32], F32),
                t16=[P.sb([128, 8, 16], F32) for _ in range(4)], ssh=P.sb([128, 8], F32), sqj=P.sb([128, 8, 96], F32),
                qn=P.sb([128, 8, 96], BF16), kn=P.sb([128, 8, 96], BF16), cqn=P.sb([128, 4, 64], BF16), ckn=P.sb([128, 4, 64], BF16),
                trq=P.sb([128, 1024], BF16), trk=P.sb([128, 1024], BF16), trc=P.sb([128, 512], BF16), mlv=P.sb([128, 8, 64], BF16))
        tsets = [alloc_tile_set() for _ in range(2)]

        def tt_s1(tb, i, proj, vst, junk, ss, lat, latT, qf, kvf, kf32, kper, t16, ssh, sqj, qn, kn, cqn, ckn, trq, trk, trc, mlv):
            j = tb * 4 + i
            tk = slice(i * 128, (i + 1) * 128)
            rows = slice(j * 128, (j + 1) * 128)
            for gi, (c0, c1, pb) in enumerate(((512, 1024, 3), (1024, 1440, 4), (1440, 1952, 5))):
                for kc in range(8):
                    P.op("pe", lambda e, kc=kc, c0=c0, c1=c1, pb=pb, tk=tk: e.matmul(
                        PS[pb][:, 0:c1 - c0], lhsT=hT[:, kc, tk], rhs=win_bf[:, kc, c0:c1], start=(kc == 0), stop=(kc == 7)),
                        ["hT", "win_bf"], [psr(pb)])
            P.op("act", lambda e: e.activation(out=proj[:, 0:512], in_=PS[3][:, :], func=AF.Copy), [psr(3)], [("proj", 0)])
            P.op("dve", lambda e: e.tensor_copy(out=proj[:, 512:928], in_=PS[4][:, 0:416]), [psr(4)], [("proj", 1)])
            P.op("act", lambda e: e.activation(out=proj[:, 928:1440], in_=PS[5][:, :], func=AF.Copy), [psr(5)], [("proj", 2)])
            P.op("pool", lambda e: e.tensor_copy(out=vst[0][:], in_=proj[:, 0:256]), [("proj", 0)], [("vst", 0)])
            P.dma(sbV[rows, :], vst[0][:], [("vst", 0)], ["sbV"])
            P.op("pool", lambda e: e.tensor_copy(out=vst[1][:], in_=proj[:, 1184:1440]), [("proj", 2)], [("vst", 1)])
            P.dma(ckV[rows, :], vst[1][:], [("vst", 1)], ["ckV"])
            P.op("act", lambda e: e.activation(out=junk[:, 0:256], in_=proj[:, 256:512], func=AF.Square, accum_out=ss[:, 0:1]),
                 [("proj", 0)], ["junk", "ss"])
            P.op("act", lambda e: e.activation(out=junk[:, 0:128], in_=proj[:, 512:640], func=AF.Square, accum_out=ss[:, 1:2]),
                 [("proj", 1), "ss"], ["junk", "ss"])
            P.op("act", lambda e: e.activation(out=ss[:, 2:3], in_=ss[:, 0:1], func=AF.Sqrt, scale=1.0 / 256, bias=EPS), ["ss"], ["ss"])
            P.op("act", lambda e: e.activation(out=ss[:, 3:4], in_=ss[:, 1:2], func=AF.Sqrt, scale=1.0 / 128, bias=EPS), ["ss"], ["ss"])
            P.op("dve", lambda e: e.reciprocal(out=ss[:, 2:4], in_=ss[:, 2:4]), ["ss"], ["ss"])
            P.op("dve", lambda e: e.scalar_tensor_tensor(out=lat[:, 0:256], in0=proj[:, 256:512], scalar=ss[:, 2:3],
                                                         in1=gains[:, G_QN:G_QN + 256], op0=ALU.mult, op1=ALU.mult),
                 [("proj", 0), "ss", "gains"], ["lat"])
            P.op("dve", lambda e: e.scalar_tensor_tensor(out=lat[:, 256:384], in0=proj[:, 512:640], scalar=ss[:, 3:4],
                                                         in1=gains[:, G_KVN:G_KVN + 128], op0=ALU.mult, op1=ALU.mult),
                 [("proj", 1), "ss", "gains"], ["lat"])
            pbt = PS[2][:].bitcast(BF16)
            for k in range(3):
                P.op("pe", lambda e, k=k, pbt=pbt: e.transpose(pbt[:, k * 128:(k + 1) * 128], lat[:, k * 128:(k + 1) * 128], ident_bf),
                     ["lat", "cbf"], [psr(2)])
            P.op("dve", lambda e, pbt=pbt: e.tensor_copy(out=latT[:], in_=pbt[:, 0:384]), [psr(2)], ["latT"])
            for half, pb in ((0, 3), (1, 4)):
                for kc in range(2):
                    P.op("pe", lambda e, half=half, pb=pb, kc=kc: e.matmul(
                        PS[pb][:, 0:384], lhsT=latT[:, kc, :], rhs=wq_bf[:, kc, half * 384:(half + 1) * 384],
                        start=(kc == 0), stop=(kc == 1)), ["latT", "wq_bf"], [psr(pb)])
            for half, pb in ((0, 5), (1, 7)):
                P.op("pe", lambda e, half=half, pb=pb: e.matmul(
                    PS[pb][:, :], lhsT=latT[:, 2, :], rhs=wkv_bf[:, half * 512:(half + 1) * 512], start=True, stop=True),
                    ["latT", "wkv_bf"], [psr(pb)])
            P.op("act", lambda e: e.activation(out=qf[:, 0:4, :], in_=PS[3][:, 0:384], func=AF.Copy), [psr(3)], [("qf", 0)])
            P.op("act", lambda e: e.activation(out=qf[:, 4:8, :], in_=PS[4][:, 0:384], func=AF.Copy), [psr(4)], [("qf", 1)])
            P.op("dve", lambda e: e.tensor_copy(out=kvf[:, 0:4, :], in_=PS[5][:, :]), [psr(5)], [("kvf", 0)])
            P.op("dve", lambda e: e.tensor_copy(out=kvf[:, 4:8, :], in_=PS[7][:, :]), [psr(7)], [("kvf", 1)])

        def tt_s2(tb, i, proj, vst, junk, ss, lat, latT, qf, kvf, kf32, kper, t16, ssh, sqj, qn, kn, cqn, ckn, trq, trk, trc, mlv):
            j = tb * 4 + i
            rows = slice(j * 128, (j + 1) * 128)
            pbt = PS[6][:].bitcast(BF16)
            cosb = cos_t[:, j, :]
            sinb = sin_t[:, j, :]
            cos8 = cosb.unsqueeze(1).broadcast_to([128, 8, 16])
            sin8 = sinb.unsqueeze(1).broadcast_to([128, 8, 16])
            QF = [("qf", 0), ("qf", 1)]
            P.op("pool", lambda e, cos8=cos8: e.tensor_tensor(out=t16[0][:], in0=qf[:, :, 64:80], in1=cos8, op=ALU.mult), QF + ["rope_t"], [("t16", 0)])
            P.op("pool", lambda e, sin8=sin8: e.tensor_tensor(out=t16[1][:], in0=qf[:, :, 80:96], in1=sin8, op=ALU.mult), QF + ["rope_t"], [("t16", 1)])
            P.op("pool", lambda e, cos8=cos8: e.tensor_tensor(out=t16[2][:], in0=qf[:, :, 80:96], in1=cos8, op=ALU.mult), QF + ["rope_t"], [("t16", 2)])
            P.op("pool", lambda e, sin8=sin8: e.tensor_tensor(out=t16[3][:], in0=qf[:, :, 64:80], in1=sin8, op=ALU.mult), QF + ["rope_t"], [("t16", 3)])
            P.op("dve", lambda e: e.tensor_tensor(out=qf[:, :, 64:80], in0=t16[0][:], in1=t16[1][:], op=ALU.subtract),
                 [("t16", 0), ("t16", 1)], QF)
            P.op("dve", lambda e: e.tensor_tensor(out=qf[:, :, 80:96], in0=t16[2][:], in1=t16[3][:], op=ALU.add),
                 [("t16", 2), ("t16", 3)], QF)
            P.op("dve", lambda e, cosb=cosb: e.tensor_tensor(out=t16[0][:, 0, :], in0=proj[:, 640:656], in1=cosb, op=ALU.mult), [("proj", 1), "rope_t"], [("t16", 0)])
            P.op("dve", lambda e, sinb=sinb: e.tensor_tensor(out=t16[1][:, 0, :], in0=proj[:, 656:672], in1=sinb, op=ALU.mult), [("proj", 1), "rope_t"], [("t16", 1)])
            P.op("dve", lambda e, cosb=cosb: e.tensor_tensor(out=t16[2][:, 0, :], in0=proj[:, 656:672], in1=cosb, op=ALU.mult), [("proj", 1), "rope_t"], [("t16", 2)])
            P.op("dve", lambda e, sinb=sinb: e.tensor_tensor(out=t16[3][:, 0, :], in0=proj[:, 640:656], in1=sinb, op=ALU.mult), [("proj", 1), "rope_t"], [("t16", 3)])
            P.op("dve", lambda e: e.tensor_tensor(out=kper[:, 0:16], in0=t16[0][:, 0, :], in1=t16[1][:, 0, :], op=ALU.subtract),
                 [("t16", 0), ("t16", 1)], ["kper"])
            P.op("dve", lambda e: e.tensor_tensor(out=kper[:, 16:32], in0=t16[2][:, 0, :], in1=t16[3][:, 0, :], op=ALU.add),
                 [("t16", 2), ("t16", 3)], ["kper"])
            KV = [("kvf", 0), ("kvf", 1)]
            P.op("pool", lambda e: e.tensor_copy(out=kf32[:, :, 0:64], in_=kvf[:, :, 0:64]), KV, ["kf32"])
            P.op("pool", lambda e: e.tensor_copy(out=kf32[:, :, 64:96], in_=kper[:].unsqueeze(1).broadcast_to([128, 8, 32])), ["kper", "kf32"], ["kf32"])
            P.op("pool", lambda e: e.tensor_copy(out=mlv[:], in_=kvf[:, :, 64:128]), KV, ["mlv"])
            P.dma(mlV[rows, :], mlv[:].rearrange("p h d -> p (h d)"), ["mlv"], ["mlV"])
            for (src, srcres, gofs, dstn, dres, sc) in ((qf, QF, G_QQK, qn, "qn", 96 ** -0.5), (kf32, ["kf32"], G_KQK, kn, "kn", 1.0)):
                P.op("pool", lambda e, src=src: e.tensor_tensor(out=sqj[:], in0=src[:], in1=src[:], op=ALU.mult), srcres, ["sqj"])
                P.op("dve", lambda e: e.tensor_reduce(out=ssh[:], in_=sqj[:], axis=AX.X, op=ALU.add), ["sqj"], ["ssh"])
                P.op("act", lambda e: e.activation(out=ssh[:], in_=ssh[:], func=AF.Sqrt, scale=1.0 / 96, bias=EPS), ["ssh"], ["ssh"])
                P.op("dve", lambda e: e.reciprocal(out=ssh[:], in_=ssh[:]), ["ssh"], ["ssh"])
                P.op("dve", lambda e, src=src: e.tensor_tensor(out=sqj[:], in0=src[:], in1=ssh[:].unsqueeze(2).broadcast_to([128, 8, 96]),
                                                               op=ALU.mult), srcres + ["ssh"], ["sqj"])
                P.op("dve", lambda e, gofs=gofs, dstn=dstn, sc=sc: e.scalar_tensor_tensor(
                    out=dstn[:], in0=sqj[:], scalar=float(sc), in1=gains[:, gofs:gofs + 96].unsqueeze(1).broadcast_to([128, 8, 96]),
                    op0=ALU.mult, op1=ALU.mult), ["sqj", "gains"], [dres])
            for (c0, pres, gofs, dstn, dres, sc) in ((672, ("proj", 1), G_CQ, cqn, "cqn", 0.125), (928, ("proj", 2), G_CK, ckn, "ckn", 1.0)):
                srcv = proj[:, c0:c0 + 256].rearrange("p (h d) -> p h d", h=4)
                sq4 = sqj[:, 0:4, 0:64]
                P.op("dve", lambda e, srcv=srcv, sq4=sq4: e.tensor_tensor(out=sq4, in0=srcv, in1=srcv, op=ALU.mult), [pres], ["sqj"])
                P.op("dve", lambda e, sq4=sq4: e.tensor_reduce(out=ssh[:, 0:4], in_=sq4, axis=AX.X, op=ALU.add), ["sqj"], ["ssh"])
                P.op("act", lambda e: e.activation(out=ssh[:, 0:4], in_=ssh[:, 0:4], func=AF.Sqrt, scale=1.0 / 64, bias=EPS), ["ssh"], ["ssh"])
                P.op("dve", lambda e: e.reciprocal(out=ssh[:, 0:4], in_=ssh[:, 0:4]), ["ssh"], ["ssh"])
                P.op("dve", lambda e, srcv=srcv, sq4=sq4: e.tensor_tensor(out=sq4, in0=srcv, in1=ssh[:, 0:4].unsqueeze(2).broadcast_to([128, 4, 64]),
                                                                          op=ALU.mult), [pres, "ssh"], ["sqj"])
                P.op("dve", lambda e, gofs=gofs, dstn=dstn, sc=sc, sq4=sq4: e.scalar_tensor_tensor(
                    out=dstn[:], in0=sq4, scalar=float(sc), in1=gains[:, gofs:gofs + 64].unsqueeze(1).broadcast_to([128, 4, 64]),
                    op0=ALU.mult, op1=ALU.mult), ["sqj", "gains"], [dres])
            for (srcn, sres, dstd, dres, trx, tres, pbk) in ((qn, "qn", mlQT, "mlQT", trq, "trq", 6), (kn, "kn", mlKT, "mlKT", trk, "trk", 0)):
                pbx = PS[pbk][:].bitcast(BF16)
                for h in range(8):
                    P.op("pe", lambda e, h=h, srcn=srcn, pbx=pbx: e.transpose(pbx[0:96, h * 128:(h + 1) * 128], srcn[:, h, :], ident_bf),
                         [sres, "cbf"], [psr(pbk)])
                P.op("act", lambda e, pbx=pbx, trx=trx: e.activation(out=trx[0:96, :], in_=pbx[0:96, :], func=AF.Copy), [psr(pbk)], [tres])
                P.dma(dstd[:, :, rows].rearrange("h d t -> d h t"), trx[0:96, :].rearrange("d (h t) -> d h t", h=8), [tres], [dres])
            for pr in range(2):
                P.op("pe", lambda e, pr=pr, pbt=pbt: e.transpose(pbt[:, pr * 128:(pr + 1) * 128], cqn[:, 2 * pr:2 * pr + 2, :].rearrange("p h d -> p (h d)"), ident_bf),
                     ["cqn", "cbf"], [psr(6)])
                P.op("pe", lambda e, pr=pr, pbt=pbt: e.transpose(pbt[:, (2 + pr) * 128:(3 + pr) * 128], ckn[:, 2 * pr:2 * pr + 2, :].rearrange("p h d -> p (h d)"), ident_bf),
                     ["ckn", "cbf"], [psr(6)])
            P.op("act", lambda e, pbt=pbt: e.activation(out=trc[:, 0:512], in_=pbt[:, 0:512], func=AF.Copy), [psr(6)], ["trc"])
            for pr in range(2):
                P.dma(ckQZ[2 * pr, 0:64, rows], trc[0:64, pr * 128:(pr + 1) * 128], ["trc"], ["ckQZ"])
                P.dma(ckQZ[2 * pr + 1, 64:128, rows], trc[64:128, pr * 128:(pr + 1) * 128], ["trc"], ["ckQZ"])
                P.dma(ckKT[pr, :, rows], trc[:, (2 + pr) * 128:(3 + pr) * 128], ["trc"], ["ckKT"])

        for tb in range(NB):
            t0 = tb * 512
            P.dma(xb[:], xsv[:, :, t0:t0 + 512], [xres_in], ["xb"])
            P.op("act", lambda e: e.activation(out=sq[:], in_=xb[:], func=AF.Square), ["xb"], ["sq"])
            for c in range(8):
                P.op("pe", lambda e, c=c: e.matmul(PS[0][:, :], lhsT=ones_bf[:], rhs=sq[:, c, :], start=(c == 0), stop=(c == 7)),
                     ["sq", "ones_bf"], [psr(0)])
            P.op("act", lambda e: e.activation(out=rstd[:], in_=PS[0][:, :], func=AF.Sqrt, scale=1.0 / D, bias=EPS), [psr(0)], ["rstd"])
            P.op("dve", lambda e: e.reciprocal(out=rstd[:], in_=rstd[:]), ["rstd"], ["rstd"])
            for c in range(8):
                P.op("dve", lambda e, c=c: e.scalar_tensor_tensor(out=xb[:, c, :], in0=xb[:, c, :], scalar=A1[:, l, c:c + 1], in1=rstd[:],
                                                                  op0=ALU.mult, op1=ALU.mult), ["xb", "A1", "rstd"], ["xb"])
                P.op("act", lambda e, c=c: e.activation(out=hT[:, c, :], in_=xb[:, c, :], func=AF.Identity, bias=mod[:, l, c:c + 1], scale=1.0),
                     ["xb", "mod"], ["hT"])
            for m in range(4):
                pb = 1 + (m % 2)
                for kc in range(8):
                    P.op("pe", lambda e, m=m, kc=kc, pb=pb: e.matmul(PS[pb][:, :], lhsT=win_bf[:, kc, m * 128:(m + 1) * 128], rhs=hT[:, kc, :],
                                                                    start=(kc == 0), stop=(kc == 7)), ["win_bf", "hT"], [psr(pb)])
                fb = m % 2
                if m < 2:
                    P.op("act", lambda e, pb=pb, fb=fb: e.activation(out=fmo[fb][:], in_=PS[pb][:, :], func=AF.Copy, scale=0.125),
                         [psr(pb)], [("fmo", fb)])
                    P.dma(sbQZ[2 * m, 0:64, t0:t0 + 512], fmo[fb][0:64, :], [("fmo", fb)], ["sbQZ"])
                    P.dma(sbQZ[2 * m + 1, 64:128, t0:t0 + 512], fmo[fb][64:128, :], [("fmo", fb)], ["sbQZ"])
                else:
                    P.op("act", lambda e, pb=pb, fb=fb: e.activation(out=fmo[fb][:], in_=PS[pb][:, :], func=AF.Copy),
                         [psr(pb)], [("fmo", fb)])
                    P.dma(sbKT[m - 2, :, t0:t0 + 512], fmo[fb][:, :], [("fmo", fb)], ["sbKT"])
            for i in range(4):
                jj = tb * 4 + i
                caps = []
                P.capture = []
                P.local = (LOCAL, jj % 2)
                tt_s1(tb, i, **tsets[jj % 2])
                caps.append(P.capture)
                if i >= 1:
                    P.capture = []
                    P.local = (LOCAL, (jj - 1) % 2)
                    tt_s2(tb, i - 1, **tsets[(jj - 1) % 2])
                    caps.append(P.capture)
                P.capture = None
                P.local = None
                P.replay_interleaved(caps)
            P.local = (LOCAL, (tb * 4 + 3) % 2)
            tt_s2(tb, 3, **tsets[(tb * 4 + 3) % 2])
            P.local = None
        P.barrier()
        P.pop()
        if stop_after == ("P1", l):
            return True

        def norm_pass(o_t, Dg, gofs, row0):
            nch = Dg // 128
            onb = [P.sb([128, Dg], BF16) for _ in range(2)]
            stg = [P.sb([128, nch, 128], BF16) for _ in range(2)]
            gss = P.sb([128, 2], F32)
            gj = P.sb([128, Dg], F32)
            for m in range(NT):
                b = m % 2
                P.op("act", lambda e, m=m: e.activation(out=gj[:], in_=o_t[:, m, :], func=AF.Square, accum_out=gss[:, 0:1]), ["o_t"], ["gj", "gss"])
                P.op("act", lambda e: e.activation(out=gss[:, 1:2], in_=gss[:, 0:1], func=AF.Sqrt, scale=1.0 / Dg, bias=EPS), ["gss"], ["gss"])
                P.op("dve", lambda e: e.reciprocal(out=gss[:, 1:2], in_=gss[:, 1:2]), ["gss"], ["gss"])
                P.op("dve", lambda e, m=m, b=b: e.scalar_tensor_tensor(out=onb[b][:], in0=o_t[:, m, :], scalar=gss[:, 1:2],
                                                                       in1=gains[:, gofs:gofs + Dg], op0=ALU.mult, op1=ALU.mult),
                     ["o_t", "gss", "gains"], [("onb", b)])
                pbt = PS[7][:].bitcast(BF16)
                for k in range(nch):
                    P.op("pe", lambda e, k=k, b=b, pbt=pbt: e.transpose(pbt[:, k * 128:(k + 1) * 128], onb[b][:, k * 128:(k + 1) * 128], ident_bf),
                         [("onb", b), "cbf"], [psr(7)])
                P.op("act", lambda e, b=b, pbt=pbt: e.activation(out=stg[b][:].rearrange("p c t -> p (c t)"), in_=pbt[:, 0:nch * 128], func=AF.Copy),
                     [psr(7)], [("stg", b)])
                P.dma(mgT[row0:row0 + Dg, m * 128:(m + 1) * 128].rearrange("(c p) t -> p c t", p=128), stg[b][:], [("stg", b)], ["mgT"])

        P.push()
        QZ = P.sb([128, 4, S], BF16)
        KT = P.sb([128, 2, S], BF16)
        VA = P.sb([128, NT, 256], BF16)
        o_t = P.sb([128, NT, 256], F32)
        P.dma(QZ[:], sbQZ.rearrange("h p s -> p h s"), ["sbQZ"], ["QZ"])
        P.dma(KT[:], sbKT.rearrange("h p s -> p h s"), ["sbKT"], ["KT"])
        P.dma(VA[:], sbV.rearrange("(n p) c -> p n c", p=128), ["sbV"], ["VA"])
        Eb = [P.sb([128, 512], F32) for _ in range(2)]
        SPb = [P.sb([128, 512], BF16) for _ in range(2)]
        Wb = [P.sb([128, 512], BF16) for _ in range(2)]
        chi = [P.sb([128, 128], BF16) for _ in range(2)]
        clo = [P.sb([128, 128], BF16) for _ in range(2)]
        items = []
        for m in range(NT):
            for h in range(4):
                blocks = list(range(m, -1, -1))
                ng = (len(blocks) + 3) // 4
                for g in range(ng):
                    items.append((m, h, g, blocks[g * 4:(g + 1) * 4], g == ng - 1))
        Zp = [0, 1]
        LWp = [2, 3]
        CP = 4
        OP = [5, 6]

        def a_st1(it, i):
            m, h, g, blks, last = it
            par = i % 2
            n = len(blks)
            qs = slice(m * 128, (m + 1) * 128)
            for c, kb in enumerate(blks):
                P.op("pe", lambda e, c=c, kb=kb, h=h, par=par, qs=qs: e.matmul(
                    PS[Zp[par]][:, c * 128:(c + 1) * 128], lhsT=KT[:, h // 2, kb * 128:(kb + 1) * 128], rhs=QZ[:, h, qs], start=True, stop=True),
                    ["KT", "QZ"], [psr(Zp[par])])
            P.op("act", lambda e, par=par, n=n: e.activation(out=Eb[par][:, 0:n * 128], in_=PS[Zp[par]][:, 0:n * 128], func=AF.Exp),
                 [psr(Zp[par])], [("Eb", par)])
            P.op("act", lambda e, par=par, n=n: e.activation(out=SPb[par][:, 0:n * 128], in_=Eb[par][:, 0:n * 128], func=AF.Ln, bias=1.0),
                 [("Eb", par)], [("SPb", par)])
            if g == 0:
                P.op("dve", lambda e, par=par: e.tensor_tensor(out=SPb[par][:, 0:128], in0=SPb[par][:, 0:128], in1=strict_bf, op=ALU.mult),
                     [("SPb", par), "cbf"], [("SPb", par)])

        def a_st2(it, i):
            m, h, g, blks, last = it
            par = i % 2
            n = len(blks)
            qs = slice(m * 128, (m + 1) * 128)
            cpar = g % 2
            for c in range(n):
                P.op("pe", lambda e, c=c, par=par, g=g, n=n, last=last: e.matmul(
                    PS[CP][:, 0:128], lhsT=ones_bf[:], rhs=SPb[par][:, c * 128:(c + 1) * 128],
                    start=(g == 0 and c == 0), stop=(last and c == n - 1), skip_group_check=True), [("SPb", par), "ones_bf"], [psr(CP)])
            if not last:
                P.op("dve", lambda e, cpar=cpar: e.tensor_copy(out=chi[1 - cpar][:], in_=PS[CP][:, 0:128]),
                     [psr(CP)], [("chi", 1 - cpar)])
                P.op("dve", lambda e, cpar=cpar: e.tensor_tensor(out=clo[1 - cpar][:], in0=PS[CP][:, 0:128], in1=chi[1 - cpar][:], op=ALU.subtract),
                     [psr(CP), ("chi", 1 - cpar)], [("clo", 1 - cpar)])
            mms = []
            for c, kb in enumerate(blks):
                cs = slice(c * 128, (c + 1) * 128)
                mms.append((PS[LWp[par]][:, cs], KT[:, h // 2, kb * 128:(kb + 1) * 128], QZ[:, h, qs], ["KT", "QZ"]))
            mms.append((PS[LWp[par]][:, 0:n * 128], negut_bf, SPb[par][:, 0:n * 128], ["cbf", ("SPb", par)]))
            for c2 in range(n - 1):
                k = n - 1 - c2
                mms.append((PS[LWp[par]][:, (c2 + 1) * 128:n * 128].rearrange("p (k i) -> p k i", k=k), negones_bf[:],
                            SPb[par][:, c2 * 128:(c2 + 1) * 128].unsqueeze(1).broadcast_to([128, k, 128]), ["negones_bf", ("SPb", par)]))
            if g > 0:
                for ct, cres in ((chi, "chi"), (clo, "clo")):
                    mms.append((PS[LWp[par]][:, 0:n * 128].rearrange("p (k i) -> p k i", k=n), negid_bf[:],
                                ct[cpar][:].unsqueeze(1).broadcast_to([128, n, 128]), ["negid_bf", (cres, cpar)]))
            if g == 0:
                mms.append((PS[LWp[par]][:, 0:128], ident_bf, nma_bf, ["cbf"]))
            for k, (ot, lt, rh, rd) in enumerate(mms):
                P.op("pe", lambda e, ot=ot, lt=lt, rh=rh, k=k, nm=len(mms): e.matmul(
                    ot, lhsT=lt, rhs=rh, start=(k == 0), stop=(k == nm - 1), skip_group_check=True), rd, [psr(LWp[par])])
            P.op("act", lambda e, par=par, n=n: e.activation(out=Wb[par][:, 0:n * 128], in_=PS[LWp[par]][:, 0:n * 128], func=AF.Exp),
                 [psr(LWp[par])], [("Wb", par)])

        def a_st3(it, i):
            m, h, g, blks, last = it
            par = i % 2
            opar = (m * 4 + h) % 2
            n = len(blks)
            for c, kb in enumerate(blks):
                P.op("pe", lambda e, c=c, kb=kb, par=par, opar=opar, h=h: e.matmul(
                    PS[OP[opar]][:, 0:64], lhsT=Wb[par][:, c * 128:(c + 1) * 128], rhs=VA[:, kb, h * 64:(h + 1) * 64],
                    start=(g == 0 and c == 0), stop=(last and c == n - 1)), [("Wb", par), "VA"], [psr(OP[opar])])
            if last:
                P.op("dve", lambda e, opar=opar, m=m, h=h: e.tensor_copy(out=o_t[:, m, h * 64:(h + 1) * 64], in_=PS[OP[opar]][:, 0:64]),
                     [psr(OP[opar])], ["o_t"])

        NI = len(items)
        for i in range(NI + 2):
            if i < NI:
                a_st1(items[i], i)
            if 1 <= i <= NI:
                a_st2(items[i - 1], i - 1)
            if 2 <= i:
                a_st3(items[i - 2], i - 2)
        norm_pass(o_t, 256, G_OUT, 0)
        P.barrier()
        P.pop()

        def softmax_attn(nheads, Dg, load_head, blocks_of, add_of, gofs, row0):
            P.push()
            o_t = P.sb([128, NT, Dg], F32)
            Wb = [P.sb([128, 512], BF16) for _ in range(2)]
            rc = [P.sb([128, 1], F32) for _ in range(2)]
            hb = [load_head(b) for b in range(2)]
            for b in range(2):
                P.op("pool", lambda e, b=b: e.memset(hb[b][2][:, :, 64:65], 1.0), [], [("hv", b)])
            Sp = [0, 1]
            Op = [2, 3]
            items = []
            for h in range(nheads):
                for m in range(NT):
                    blocks = blocks_of(m)
                    ng = (len(blocks) + 3) // 4
                    for g in range(ng):
                        items.append((h, m, g, blocks[g * 4:(g + 1) * 4], g == ng - 1))

            def st1(it, i):
                h, m, g, blks, last = it
                par = i % 2
                b = h % 2
                qt, kt, vt, fill = hb[b]
                if m == 0 and g == 0:
                    fill(h, b)
                n = len(blks)
                qs = slice(m * 128, (m + 1) * 128)
                for c, kb in enumerate(blks):
                    cs = slice(c * 128, (c + 1) * 128)
                    ad = add_of(h, m, kb)
                    P.op("pe", lambda e, cs=cs, kb=kb, par=par, qs=qs, kt=kt, qt=qt, ad=ad: e.matmul(
                        PS[Sp[par]][:, cs], lhsT=kt[:, kb * 128:(kb + 1) * 128], rhs=qt[:, qs], start=True, stop=(ad is None or ad[0] != "pe")),
                        [("hk", b), ("hq", b)], [psr(Sp[par])])
                    if ad is not None and ad[0] == "pe":
                        P.op("pe", lambda e, cs=cs, par=par, ad=ad: e.matmul(PS[Sp[par]][:, cs], lhsT=ident_bf, rhs=ad[1], start=False, stop=True),
                             ["cbf"], [psr(Sp[par])])
                    elif ad is not None:
                        P.op("dve", lambda e, cs=cs, par=par, ad=ad: e.tensor_tensor(out=PS[Sp[par]][:, cs], in0=PS[Sp[par]][:, cs], in1=ad[1], op=ALU.add),
                             [psr(Sp[par]), "ckbm"], [psr(Sp[par])])
                P.op("act", lambda e, par=par, n=n: e.activation(out=Wb[par][:, 0:n * 128], in_=PS[Sp[par]][:, 0:n * 128], func=AF.Exp),
                     [psr(Sp[par])], [("Wb", par)])

            def st2(it, i):
                h, m, g, blks, last = it
                par = i % 2
                b = h % 2
                qt, kt, vt, fill = hb[b]
                opar = (h * NT + m) % 2
                n = len(blks)
                for c, kb in enumerate(blks):
                    P.op("pe", lambda e, c=c, kb=kb, par=par, opar=opar, vt=vt: e.matmul(
                        PS[Op[opar]][:, 0:65], lhsT=Wb[par][:, c * 128:(c + 1) * 128], rhs=vt[:, kb, :],
                        start=(g == 0 and c == 0), stop=(last and c == n - 1)), [("Wb", par), ("hv", b)], [psr(Op[opar])])
                if last:
                    P.op("dve", lambda e, opar=opar: e.reciprocal(out=rc[opar][:], in_=PS[Op[opar]][:, 64:65]), [psr(Op[opar])], [("rc", opar)])
                    P.op("dve", lambda e, opar=opar, m=m, h=h: e.tensor_scalar(out=o_t[:, m, h * 64:(h + 1) * 64], in0=PS[Op[opar]][:, 0:64],
                                                                              scalar1=rc[opar][:, 0:1], scalar2=None, op0=ALU.mult),
                         [psr(Op[opar]), ("rc", opar)], ["o_t"])

            NI = len(items)
            for i in range(NI + 1):
                if i < NI:
                    st1(items[i], i)
                if i >= 1:
                    st2(items[i - 1], i - 1)
            norm_pass(o_t, Dg, gofs, row0)
            P.barrier()
            P.pop()

        def mla_load(b):
            qt = P.sb([96, S], BF16)
            kt = P.sb([96, S], BF16)
            vt = P.sb([128, NT, 65], BF16)

            def fill(h, b):
                P.dma(qt[:], mlQT[h], ["mlQT"], [("hq", b)])
                P.dma(kt[:], mlKT[h], ["mlKT"], [("hk", b)])
                P.dma(vt[:, :, 0:64], mlV[:, h * 64:(h + 1) * 64].rearrange("(n p) c -> p n c", p=128), ["mlV"], [("hv", b)])
            return (qt, kt, vt, fill)

        softmax_attn(8, 512, mla_load, lambda m: list(range(m, -1, -1)),
                     lambda h, m, kb: (("pe", mb_bf) if kb == m else None), G_OUT + 256, 256)

        def ck_load(b):
            qt = P.sb([128, S], BF16)
            kt = P.sb([128, S], BF16)
            vt = P.sb([128, NT, 65], BF16)

            def fill(h, b):
                P.dma(qt[:], ckQZ[h], ["ckQZ"], [("hq", b)])
                P.dma(kt[:], ckKT[h // 2], ["ckKT"], [("hk", b)])
                P.dma(vt[:, :, 0:64], ckV[:, h * 64:(h + 1) * 64].rearrange("(n p) c -> p n c", p=128), ["ckV"], [("hv", b)])
            return (qt, kt, vt, fill)

        softmax_attn(4, 256, ck_load, lambda m: [kb for kb in range(m, m - 5, -1) if kb >= 0],
                     lambda h, m, kb: ("dve", ckbm[:, h * 5 + (kb - (m - 4)), :]), G_OUT + 768, 768)
        if stop_after == ("P2", l):
            return True

        P.pop()
        P.push()
        moe = (l % 2 == 1)
        li = l // 2
        NSB = TS // 512
        wo_bf = P.sb([128, 8, D], BF16)
        wgb = [P.sb([128, 8, 512], BF16) for _ in range(2)]
        wub = [P.sb([128, 8, 512], BF16) for _ in range(2)]
        wdb = [P.sb([128, 12, 512], BF16) for _ in range(2)]
        P.dma(wo_bf[:], w_out[l].rearrange("(kc p) n -> p kc n", p=128), ["w_out"], ["wo_bf"], q="pool")
        x1 = P.sb([128, 8, TS], F32)
        h2T = P.sb([128, 8, TS], BF16)
        h2f = P.sb([128, 8, 512], F32)
        mtb = P.sb([128, 8, 512], BF16)
        rstd = P.sb([128, 512], F32)
        AT = P.sb([128, 12, TS], BF16)
        sg = [P.sb([128, 512], BF16) for _ in range(2)]
        if moe:
            wr = P.sb([128, 8, NE], F32)
            P.dma(wr[:], router[li].rearrange("(kc p) n -> p kc n", p=128), ["router"], ["wr"])
            lg = P.sb([128, 4, NE], F32)
            lg2 = P.sb([128, 4, NE], F32)
            eq1 = P.sb([128, 4, NE], F32)
            eq2 = P.sb([128, 4, NE], F32)
            mx = P.sb([128, 4, 4], F32)
            gts = P.sb([128, 4, NE], F32)
            gT = P.sb([8, TS], F32)
            Gb = [P.sb([128, 512], F32) for _ in range(NSB)]
            ytmp = [P.sb([128, 512], F32) for _ in range(2)]
        gcount = [0, 0]
        for tsb in range(S // TS):
            for sbi in range(NSB):
                t0 = tsb * TS + sbi * 512
                xs = slice(sbi * 512, (sbi + 1) * 512)
                P.dma(x1[:, :, xs], xsv[:, :, t0:t0 + 512], [xres_in], [("x1", sbi)])
                P.dma(mtb[:], mgT.rearrange("(c p) s -> p c s", p=128)[:, :, t0:t0 + 512], ["mgT"], ["mtb"])
                for c in range(8):
                    pb = c % 2
                    for kc in range(8):
                        P.op("pe", lambda e, c=c, kc=kc, pb=pb: e.matmul(PS[pb][:, :], lhsT=wo_bf[:, kc, c * 128:(c + 1) * 128], rhs=mtb[:, kc, :],
                                                                        start=(kc == 0), stop=(kc == 7)), ["wo_bf", "mtb"], [psr(pb)])
                    P.op("dve", lambda e, c=c, pb=pb, xs=xs: e.scalar_tensor_tensor(out=x1[:, c, xs], in0=PS[pb][:, :], scalar=mod[:, l, 16 + c:17 + c],
                                                                                    in1=x1[:, c, xs], op0=ALU.mult, op1=ALU.add),
                         [psr(pb), "mod", ("x1", sbi)], [("x1", sbi)])
                P.op("act", lambda e, xs=xs: e.activation(out=h2T[:, :, xs], in_=x1[:, :, xs], func=AF.Square), [("x1", sbi)], [("h2T", sbi)])
                for c in range(8):
                    P.op("pe", lambda e, c=c, xs=xs: e.matmul(PS[2][:, :], lhsT=ones_bf[:], rhs=h2T[:, c, xs], start=(c == 0), stop=(c == 7)),
                         [("h2T", sbi), "ones_bf"], [psr(2)])
                P.op("act", lambda e: e.activation(out=rstd[:], in_=PS[2][:, :], func=AF.Sqrt, scale=1.0 / D, bias=EPS), [psr(2)], ["rstd"])
                P.op("dve", lambda e: e.reciprocal(out=rstd[:], in_=rstd[:]), ["rstd"], ["rstd"])
                for c in range(8):
                    P.op("dve", lambda e, c=c, xs=xs: e.scalar_tensor_tensor(out=h2f[:, c, :], in0=x1[:, c, xs], scalar=A2[:, l, c:c + 1], in1=rstd[:],
                                                                             op0=ALU.mult, op1=ALU.mult), [("x1", sbi), "A2", "rstd"], [("h2f", c)])
                    P.op("act", lambda e, c=c: e.activation(out=h2f[:, c, :], in_=h2f[:, c, :], func=AF.Identity, bias=mod[:, l, 24 + c:25 + c], scale=1.0),
                         [("h2f", c), "mod"], [("h2f", c)])
                    P.op("pool", lambda e, c=c, xs=xs: e.tensor_copy(out=h2T[:, c, xs], in_=h2f[:, c, :]), [("h2f", c)], [("h2T", sbi)])
                if moe:
                    H2F = [("h2f", c) for c in range(8)]
                    for i in range(4):
                        for kc in range(8):
                            P.op("pe", lambda e, i=i, kc=kc: e.matmul(PS[3][:, i * 8:(i + 1) * 8], lhsT=h2f[:, kc, i * 128:(i + 1) * 128], rhs=wr[:, kc, :],
                                                                      start=(kc == 0), stop=(kc == 7)), H2F + ["wr"], [psr(3)])
                    P.op("dve", lambda e: e.tensor_copy(out=lg[:].rearrange("p a b -> p (a b)"), in_=PS[3][:, 0:32]), [psr(3)], ["lg"])
                    P.op("dve", lambda e: e.tensor_reduce(out=mx[:, :, 0], in_=lg[:], axis=AX.X, op=ALU.max), ["lg"], ["mx"])
                    P.op("dve", lambda e: e.tensor_tensor(out=eq1[:], in0=lg[:], in1=mx[:, :, 0:1].broadcast_to([128, 4, NE]), op=ALU.is_equal),
                         ["lg", "mx"], ["eq1"])
                    P.op("dve", lambda e: e.scalar_tensor_tensor(out=lg2[:], in0=eq1[:], scalar=-1e30, in1=lg[:], op0=ALU.mult, op1=ALU.add),
                         ["eq1", "lg"], ["lg2"])
                    P.op("dve", lambda e: e.tensor_reduce(out=mx[:, :, 1], in_=lg2[:], axis=AX.X, op=ALU.max), ["lg2", "mx"], ["mx"])
                    P.op("dve", lambda e: e.tensor_tensor(out=eq2[:], in0=lg2[:], in1=mx[:, :, 1:2].broadcast_to([128, 4, NE]), op=ALU.is_equal),
                         ["lg2", "mx"], ["eq2"])
                    P.op("dve", lambda e: e.tensor_tensor(out=mx[:, :, 2], in0=mx[:, :, 1], in1=mx[:, :, 0], op=ALU.subtract), ["mx"], ["mx"])
                    P.op("act", lambda e: e.activation(out=mx[:, :, 2], in_=mx[:, :, 2], func=AF.Exp), ["mx"], ["mx"])
                    P.op("dve", lambda e: e.tensor_scalar(out=mx[:, :, 2], in0=mx[:, :, 2], scalar1=1.0, scalar2=None, op0=ALU.add), ["mx"], ["mx"])
                    P.op("dve", lambda e: e.reciprocal(out=mx[:, :, 2], in_=mx[:, :, 2]), ["mx"], ["mx"])
                    P.op("dve", lambda e: e.tensor_scalar(out=mx[:, :, 3], in0=mx[:, :, 2], scalar1=-1.0, scalar2=1.0, op0=ALU.mult, op1=ALU.add), ["mx"], ["mx"])
                    P.op("dve", lambda e: e.tensor_tensor(out=eq1[:], in0=eq1[:], in1=mx[:, :, 2:3].broadcast_to([128, 4, NE]), op=ALU.mult), ["eq1", "mx"], ["eq1"])
                    P.op("dve", lambda e: e.tensor_tensor(out=eq2[:], in0=eq2[:], in1=mx[:, :, 3:4].broadcast_to([128, 4, NE]), op=ALU.mult), ["eq2", "mx"], ["eq2"])
                    P.op("dve", lambda e: e.tensor_tensor(out=gts[:], in0=eq1[:], in1=eq2[:], op=ALU.add), ["eq1", "eq2"], ["gts"])
                    for i in range(4):
                        P.op("pe", lambda e, i=i: e.transpose(PS[2][0:8, i * 128:(i + 1) * 128], gts[:, i, :], ident_f), ["gts", "cst"], [psr(2)])
                    P.op("act", lambda e, xs=xs: e.activation(out=gT[:, xs], in_=PS[2][0:8, :], func=AF.Copy), [psr(2)], [("gT", sbi)])
                    if debug:
                        P.dma(dbgG[:, t0:t0 + 512], gT[:, xs], [("gT", sbi)], ["dbgG"])
            nexp = NE if moe else 1
            for ex in range(nexp):
                if moe:
                    wg_d, wu_d, wd_d = moe_g[li, ex], moe_u[li, ex], moe_d[li, ex]
                else:
                    wg_d, wu_d, wd_d = ffn_g[li], ffn_u[li], ffn_d[li]
                wgv = wg_d.rearrange("(kc p) n -> p kc n", p=128)
                wuv = wu_d.rearrange("(kc p) n -> p kc n", p=128)
                wdv = wd_d.rearrange("(j p) n -> p j n", p=128)
                for pas, (j0, njp, groups) in enumerate(((0, 12, ((0, 4), (4, 4), (8, 4))), (12, 10, ((12, 4), (16, 4), (20, 2))))):
                    for (js, nj) in groups:
                        wbuf = gcount[0] % 2
                        gcount[0] += 1
                        P.dma(wgb[wbuf][:, :, 0:nj * 128], wgv[:, :, js * 128:(js + nj) * 128], ["wg_d"], [("wgb", wbuf)], q="pool")
                        P.dma(wub[wbuf][:, :, 0:nj * 128], wuv[:, :, js * 128:(js + nj) * 128], ["wu_d"], [("wub", wbuf)], q="pool")
                        for jj in range(nj):
                            jl = js + jj - j0
                            for sbi in range(NSB):
                                xs = slice(sbi * 512, (sbi + 1) * 512)
                                par = (jj * NSB + sbi) % 2
                                gp, up = 4 + par, 6 + par
                                for kc in range(8):
                                    P.op("pe", lambda e, kc=kc, jj=jj, wbuf=wbuf, xs=xs, gp=gp: e.matmul(
                                        PS[gp][:, :], lhsT=wgb[wbuf][:, kc, jj * 128:(jj + 1) * 128], rhs=h2T[:, kc, xs], start=(kc == 0), stop=(kc == 7)),
                                        [("wgb", wbuf), ("h2T", sbi)], [psr(gp)])
                                for kc in range(8):
                                    P.op("pe", lambda e, kc=kc, jj=jj, wbuf=wbuf, xs=xs, up=up: e.matmul(
                                        PS[up][:, :], lhsT=wub[wbuf][:, kc, jj * 128:(jj + 1) * 128], rhs=h2T[:, kc, xs], start=(kc == 0), stop=(kc == 7)),
                                        [("wub", wbuf), ("h2T", sbi)], [psr(up)])
                                P.op("act", lambda e, par=par, gp=gp: e.activation(out=sg[par][:], in_=PS[gp][:, :], func=AF.Silu), [psr(gp)], [("sg", par)])
                                P.op("dve", lambda e, par=par, up=up, jl=jl, xs=xs: e.tensor_tensor(out=AT[:, jl, xs], in0=PS[up][:, :], in1=sg[par][:], op=ALU.mult),
                                     [psr(up), ("sg", par)], [("AT", sbi)])
                    for dh in range(2):
                        wdbuf = gcount[1] % 2
                        gcount[1] += 1
                        P.dma(wdb[wdbuf][:, 0:njp, :], wdv[:, j0:j0 + njp, dh * 512:(dh + 1) * 512], ["wd_d"], [("wdb", wdbuf)], q="pool")
                        for cc in range(4):
                            c = dh * 4 + cc
                            for sbi in range(NSB):
                                xs = slice(sbi * 512, (sbi + 1) * 512)
                                par = (cc * NSB + sbi) % 2
                                dp = par
                                for jl in range(njp):
                                    P.op("pe", lambda e, jl=jl, wdbuf=wdbuf, xs=xs, dp=dp, cc=cc, njp=njp: e.matmul(
                                        PS[dp][:, :], lhsT=wdb[wdbuf][:, jl, cc * 128:(cc + 1) * 128], rhs=AT[:, jl, xs], start=(jl == 0), stop=(jl == njp - 1)),
                                        [("wdb", wdbuf), ("AT", sbi)], [psr(dp)])
                                if not moe:
                                    P.op("dve", lambda e, c=c, xs=xs, dp=dp: e.scalar_tensor_tensor(
                                        out=x1[:, c, xs], in0=PS[dp][:, :], scalar=mod[:, l, 40 + c:41 + c], in1=x1[:, c, xs], op0=ALU.mult, op1=ALU.add),
                                        [psr(dp), "mod", ("x1", sbi)], [("x1", sbi)])
                                else:
                                    if pas == 0 and dh == 0 and cc == 0:
                                        P.op("pe", lambda e, xs=xs, sbi=sbi, ex=ex: e.matmul(
                                            PS[2 + sbi % 2][:, :], lhsT=cst[0:8, C_SEL + ex * 128:C_SEL + (ex + 1) * 128], rhs=gT[:, xs], start=True, stop=True),
                                            ["cst", ("gT", sbi)], [psr(2 + sbi % 2)])
                                        P.op("act", lambda e, sbi=sbi: e.activation(out=Gb[sbi][:], in_=PS[2 + sbi % 2][:, :], func=AF.Copy),
                                             [psr(2 + sbi % 2)], [("Gb", sbi)])
                                    P.op("dve", lambda e, c=c, dp=dp, sbi=sbi, par=par: e.scalar_tensor_tensor(
                                        out=ytmp[par][:], in0=PS[dp][:, :], scalar=mod[:, l, 40 + c:41 + c], in1=Gb[sbi][:], op0=ALU.mult, op1=ALU.mult),
                                        [psr(dp), "mod", ("Gb", sbi)], [("ytmp", par)])
                                    P.op("dve", lambda e, c=c, xs=xs, par=par: e.tensor_tensor(out=x1[:, c, xs], in0=x1[:, c, xs], in1=ytmp[par][:], op=ALU.add),
                                         [("ytmp", par), ("x1", sbi)], [("x1", sbi)])
            for sbi in range(NSB):
                t0 = tsb * TS + sbi * 512
                xs = slice(sbi * 512, (sbi + 1) * 512)
                P.dma(xdv[:, :, t0:t0 + 512], x1[:, :, xs], [("x1", sbi)], [xres_out])
        P.barrier()
        P.pop()

    for l in range(nlayers):
        if do_layer(l):
            break

    P.emit(final_waits=["yT"])
    return nc


def host_inputs(inp, S):
    NT = S // 128
    B = inp["x"].shape[0]
    f = lambda a: np.ascontiguousarray(np.asarray(a, dtype=np.float32))
    consts = make_consts()
    L = inp["w_in"].shape[0]
    vec = np.zeros((L, 128, 64), np.float32)
    gains = np.zeros((L, 128, NGAIN), np.float32)
    ckb = np.zeros((L, 128, 20, 128), np.float32)
    jj = np.arange(128)[:, None]
    ii = np.arange(128)[None, :]
    for l in range(L):
        vec[l, :, 0:48] = np.asarray(inp["ada_b"][l]).reshape(48, 128).T
        vec[l, :, 48:56] = np.asarray(inp["norm_mix"][l]).reshape(8, 128).T
        vec[l, :, 56:64] = np.asarray(inp["norm_ffn"][l]).reshape(8, 128).T
        g = np.concatenate([np.asarray(inp[k][l]) for k in ("mla_q_norm", "mla_kv_norm", "mla_q_qknorm", "mla_k_qknorm",
                                                            "ck_q_qknorm", "ck_k_qknorm", "group_out_norm")])
        gains[l] = np.broadcast_to(g[None, :], (128, NGAIN))
        rb = np.asarray(inp["ck_rel_bias"][l])
        for h in range(4):
            for o in range(5):
                idx = np.clip(128 * (4 - o) + ii - jj, -128, 128) + 128
                ckb[l, :, h * 5 + o, :] = rb[h][idx]
    shared = dict(consts=consts, vec=vec, gains=gains, ckbias=ckb)
    for k in ("ada_w", "w_in", "w_q_up", "w_kv_up", "w_out", "ffn_w_gate", "ffn_w_up", "ffn_w_down", "moe_router",
              "moe_w_gate", "moe_w_up", "moe_w_down"):
        shared[k] = f(inp[k])
    maps = []
    for b in range(B):
        m = dict(shared)
        m["xT"] = np.ascontiguousarray(np.asarray(inp["x"][b], np.float32).T)
        m["cT"] = np.ascontiguousarray(np.asarray(inp["c"][b], np.float32).reshape(8, 128).T)
        m["pos"] = np.ascontiguousarray(np.asarray(inp["positions"][b], np.int32).reshape(NT, 128).T)
        maps.append(m)
    return maps


_NC_CACHE = {}


def kernel(**inputs):
    S = inputs["x"].shape[1]
    B = inputs["x"].shape[0]
    key = (S,)
    if key not in _NC_CACHE:
        _NC_CACHE[key] = build(S)
    nc = _NC_CACHE[key]
    maps = host_inputs(inputs, S)
    res = run_bass_kernel_spmd(nc, maps, core_ids=list(range(B)))
    out = np.stack([np.ascontiguousarray(res.results[b]["yT"].T) for b in range(B)], axis=0)
    return out.astype(np.float32)
```

```python
import math
import numpy as np
from contextlib import ExitStack
import concourse.bass as bass
import concourse.mybir as mybir
from concourse.bass_utils import run_bass_kernel_spmd

F32 = mybir.dt.float32
BF16 = mybir.dt.bfloat16
I32 = mybir.dt.int32
AF = mybir.ActivationFunctionType
ALU = mybir.AluOpType
AX = mybir.AxisListType

D = 1024
DFF = 2816
NJ = DFF // 128
NE = 8
EPS = 1e-6
NEG = -30000.0
COMPUTE = ("pe", "act", "dve", "pool")


class Prog:
    def __init__(self, nc):
        self.nc = nc
        self.stacks = [ExitStack()]
        self.ins = {e: [] for e in COMPUTE + ("sp",)}
        self.res_w = {}
        self.res_r = {}
        self.dma_cnt = {}
        self.sems = {}
        self.ntile = 0
        self.pending = {}
        self.local = None
        self.capture = None

    def replay_interleaved(self, lists):
        self.local = None
        pos = [0] * len(lists)
        tot = max(len(x) for x in lists) if lists else 0
        for step in range(tot):
            for li, lst in enumerate(lists):
                upto = ((step + 1) * len(lst) + tot - 1) // tot
                while pos[li] < min(upto, len(lst)):
                    rec = lst[pos[li]]
                    pos[li] += 1
                    if rec[0] == "op":
                        self.op(rec[1], rec[2], rec[3], rec[4])
                    else:
                        self.dma(rec[1], rec[2], rec[3], rec[4], q=rec[5])

    def _rn(self, rs):
        if self.local is None:
            return list(rs)
        names, sfx = self.local
        out = []
        for r in rs:
            base = r[0] if isinstance(r, tuple) else r
            out.append((r, sfx) if base in names else r)
        return out

    def push(self):
        self.stacks.append(ExitStack())

    def pop(self):
        self.stacks.pop().close()

    def sb(self, shape, dt, name=None):
        self.ntile += 1
        return self.stacks[-1].enter_context(self.nc.sbuf_tensor(f"t{self.ntile}", list(shape), dt))

    def ps(self, shape, dt=F32):
        self.ntile += 1
        return self.stacks[0].enter_context(self.nc.psum_tensor(f"p{self.ntile}", list(shape), dt))

    def _deps(self, eng, reads, writes):
        deps = {}

        def add(d):
            for k, v in d.items():
                if deps.get(k, -1) < v:
                    deps[k] = v
        for r in reads:
            add(self.res_w.get(r, {}))
        for w in writes:
            add(self.res_w.get(w, {}))
            add(self.res_r.get(w, {}))
        if eng in self.pending:
            add(self.pending.pop(eng))
        return deps

    def barrier(self, engines=COMPUTE + ("sp",)):
        d = {}
        for e in COMPUTE:
            if self.ins[e]:
                for i in range(len(self.ins[e]) - 1, -1, -1):
                    if self.ins[e][i]["kind"] == "op":
                        d[("E", e)] = i
                        break
        for res, cnt in self.dma_cnt.items():
            d[("D", res)] = cnt
        for e in engines:
            cur = self.pending.setdefault(e, {})
            for k, v in d.items():
                if cur.get(k, -1) < v:
                    cur[k] = v

    def op(self, eng, fn, reads=(), writes=()):
        reads = self._rn(reads)
        writes = self._rn(writes)
        if self.capture is not None:
            self.capture.append(("op", eng, fn, reads, writes))
            return None
        lst = self.ins[eng]
        idx = len(lst)
        deps = self._deps(eng, reads, writes)
        if eng == "pe":
            deps.pop(("E", "pe"), None)
        rec = dict(kind="op", fn=fn, deps=deps, signal=False)
        lst.append(rec)
        key = ("E", eng)
        for r in reads:
            self.res_r.setdefault(r, {})[key] = idx
        for w in writes:
            self.res_w[w] = {key: idx}
            self.res_r[w] = {}
        return rec

    def dma(self, out_ap, in_ap, reads, writes, q="sp"):
        reads = self._rn(reads)
        writes = self._rn(writes)
        if self.capture is not None:
            self.capture.append(("dma", out_ap, in_ap, reads, writes, q))
            return None
        lst = self.ins[q]
        dst = writes[0]
        deps = self._deps(q, reads, writes)
        skey = ("D", dst)
        deps.pop(skey, None)
        cnt = self.dma_cnt.get(dst, 0) + 16
        self.dma_cnt[dst] = cnt
        rec = dict(kind="dma", out=out_ap, in_=in_ap, deps=deps, skey=skey)
        lst.append(rec)
        for r in reads:
            self.res_r.setdefault(r, {})[skey] = cnt
        for w in writes:
            d = self.res_w.setdefault(w, {})
            if set(d.keys()) - {skey}:
                d = {}
                self.res_w[w] = d
            d[skey] = cnt
            self.res_r[w] = {}
        return rec

    def emit(self, final_waits=()):
        nc = self.nc
        es = self.stacks[0]
        for e in self.ins:
            for rec in self.ins[e]:
                for (kind, k), v in rec["deps"].items():
                    if kind == "E":
                        self.ins[k][v]["signal"] = True
        for e in COMPUTE:
            c = 0
            for rec in self.ins[e]:
                if rec.get("signal"):
                    c += 1
                    rec["sigval"] = c
        keys = [("E", e) for e in COMPUTE] + sorted({rec["skey"] for e in self.ins for rec in self.ins[e]
                                                     if rec["kind"] == "dma"}, key=str)
        for i, k in enumerate(keys):
            self.sems[k] = es.enter_context(nc.semaphore(f"s{i}"))
        final = dict(self.dma_cnt)
        with nc.Block() as block:
            def mk(ename, lists):
                def body(eng):
                    waited = {}
                    for rec in lists:
                        for (kind, k), v in rec["deps"].items():
                            val = self.ins[k][v]["sigval"] if kind == "E" else v
                            key = (kind, k)
                            if waited.get(key, -1) >= val:
                                continue
                            waited[key] = val
                            eng.wait_ge(self.sems[key], val)
                        if rec["kind"] == "op":
                            inst = rec["fn"](eng)
                            if rec["signal"]:
                                inst.then_inc(self.sems[("E", ename)], 1)
                        else:
                            inst = eng.dma_start(out=rec["out"], in_=rec["in_"])
                            inst.then_inc(self.sems[rec["skey"]], 16)
                    if ename == "sp":
                        for res in final_waits:
                            if ("D", res) in self.sems:
                                eng.wait_ge(self.sems[("D", res)], final[res])
                return body
            block.tensor(mk("pe", self.ins["pe"]))
            block.scalar(mk("act", self.ins["act"]))
            block.vector(mk("dve", self.ins["dve"]))
            block.gpsimd(mk("pool", self.ins["pool"]))
            block.sync(mk("sp", self.ins["sp"]))
        while self.stacks:
            self.stacks.pop().close()


C_ID, C_NUT, C_STRICT, C_NMA, C_MB, C_CK0, C_CK4, C_SEL = [i * 128 for i in range(8)]
NCONST = 7 * 128 + 8 * 128
INV_FREQ = (np.float32(10000.0) ** (-np.arange(0, 32, 2, dtype=np.float32) / np.float32(32))).astype(np.float32)


def make_consts():
    j = np.arange(128)[:, None]
    i = np.arange(128)[None, :]
    c = np.zeros((128, NCONST), np.float32)
    c[:, C_ID:C_ID + 128] = (j == i)
    c[:, C_NUT:C_NUT + 128] = -(j >= i).astype(np.float32)
    c[:, C_STRICT:C_STRICT + 128] = (j < i)
    c[:, C_NMA:C_NMA + 128] = np.where(j >= i, NEG, 0.0)
    c[:, C_MB:C_MB + 128] = np.where((j >= 64) & (i < 64), NEG, 0.0)
    c[:, C_CK0:C_CK0 + 128] = np.where((i >= 64) & (j < 64), NEG, 0.0)
    c[:, C_CK4:C_CK4 + 128] = np.where((i < 64) & (j >= 64), NEG, 0.0)
    for e in range(8):
        c[e, C_SEL + e * 128:C_SEL + (e + 1) * 128] = 1.0
    return c


G_QN, G_KVN, G_QQK, G_KQK, G_CQ, G_CK, G_OUT = 0, 256, 384, 480, 576, 640, 704
NGAIN = 704 + 1024


def build(S, nlayers=2, debug=False, TS=1024, stop_after=None):
    NT = S // 128
    NB = S // 512
    TS = min(TS, S)
    nc = bass.Bass("TRN2", target_bir_lowering=False)
    P = Prog(nc)

    def din(name, shape, dt=F32):
        return nc.dram_tensor(name, list(shape), dt, kind="ExternalInput").ap()

    def dscr(name, shape, dt):
        return nc.dram_tensor(name, list(shape), dt, kind=("ExternalOutput" if debug else "Internal")).ap()

    xT_in = din("xT", [D, S])
    cT_d = din("cT", [128, 8])
    pos_d = din("pos", [128, NT], I32)
    consts_d = din("consts", [128, NCONST])
    vec_d = din("vec", [2, 128, 64])
    gains_d = din("gains", [2, 128, NGAIN])
    ckb_d = din("ckbias", [2, 128, 20, 128])
    ada_w = din("ada_w", [2, D, 6 * D])
    w_in = din("w_in", [2, D, 1952])
    w_q_up = din("w_q_up", [2, 256, 768])
    w_kv_up = din("w_kv_up", [2, 128, 1024])
    w_out = din("w_out", [2, D, D])
    ffn_g = din("ffn_w_gate", [1, D, DFF])
    ffn_u = din("ffn_w_up", [1, D, DFF])
    ffn_d = din("ffn_w_down", [1, DFF, D])
    router = din("moe_router", [1, D, NE])
    moe_g = din("moe_w_gate", [1, NE, D, DFF])
    moe_u = din("moe_w_up", [1, NE, D, DFF])
    moe_d = din("moe_w_down", [1, NE, DFF, D])
    yT = nc.dram_tensor("yT", [D, S], F32, kind="ExternalOutput").ap()

    xT_mid = dscr("xT_mid", [D, S], F32)
    sbQZ = dscr("sbQZ", [4, 128, S], BF16)
    sbKT = dscr("sbKT", [2, 128, S], BF16)
    sbV = dscr("sbV", [S, 256], BF16)
    mlQT = dscr("mlQT", [8, 96, S], BF16)
    mlKT = dscr("mlKT", [8, 96, S], BF16)
    mlV = dscr("mlV", [S, 512], BF16)
    ckQZ = dscr("ckQZ", [4, 128, S], BF16)
    ckKT = dscr("ckKT", [2, 128, S], BF16)
    ckV = dscr("ckV", [S, 256], BF16)
    mgT = dscr("mgT", [D, S], BF16)
    rope_d = dscr("rope_d", [2, 128, NT, 16], F32)
    dbgG = dscr("dbgG", [8, S], F32) if debug else None

    cst = P.sb([128, NCONST], F32)
    cbf = P.sb([128, 5 * 128], BF16)
    ones_bf = P.sb([128, 128], BF16)
    negones_bf = P.sb([128, 128], BF16)
    negrow = P.sb([1, 128], F32)
    zeros_bf = P.sb([128, 512], BF16)
    negid_bf = P.sb([128, 128], BF16)
    cact = P.sb([128, 8], F32)
    vec = P.sb([128, 2, 64], F32)
    mod = P.sb([128, 2, 48], F32)
    A1 = P.sb([128, 2, 8], F32)
    A2 = P.sb([128, 2, 8], F32)
    PS = [P.ps([128, 512], F32) for _ in range(8)]

    def psr(k):
        return ("ps", k)

    ident_bf = cbf[:, 0:128]
    negut_bf = cbf[:, 128:256]
    strict_bf = cbf[:, 256:384]
    nma_bf = cbf[:, 384:512]
    mb_bf = cbf[:, 512:640]
    ident_f = cst[:, C_ID:C_ID + 128]

    P.dma(cst[:], consts_d[:, :], ["consts_d"], ["cst"])
    P.dma(cact[:], cT_d[:, :], ["cT_d"], ["cact"])
    P.dma(vec[:], vec_d.rearrange("l p c -> p l c"), ["vec_d"], ["vec"])
    P.op("dve", lambda e: e.tensor_copy(out=cbf[:], in_=cst[:, 0:640]), ["cst"], ["cbf"])
    P.op("pool", lambda e: e.memset(ones_bf[:], 1.0), [], ["ones_bf"])
    P.op("pool", lambda e: e.memset(negones_bf[:], -1.0), [], ["negones_bf"])
    P.op("pool", lambda e: e.memset(negrow[:], -1.0), [], ["negrow"])
    P.op("pool", lambda e: e.memset(zeros_bf[:], 0.0), [], ["zeros_bf"])
    P.op("dve", lambda e: e.tensor_scalar(out=negid_bf[:], in0=cst[:, C_ID:C_ID + 128], scalar1=-1.0, scalar2=None, op0=ALU.mult), ["cst"], ["negid_bf"])
    P.op("act", lambda e: e.activation(out=cact[:], in_=cact[:], func=AF.Silu), ["cact"], ["cact"])
    for zt, zres in ((sbQZ, "sbQZ"), (ckQZ, "ckQZ")):
        for h in range(4):
            lo = 64 if h % 2 == 0 else 0
            for tbz in range(NB):
                P.dma(zt[h, lo:lo + 64, tbz * 512:(tbz + 1) * 512], zeros_bf[lo:lo + 64, :], ["zeros_bf"], [zres])

    P.push()
    awb = [P.sb([128, 8, 512], F32) for _ in range(2)]
    for l in range(nlayers):
        awv = ada_w[l].rearrange("(kc p) n -> p kc n", p=128)
        for g in range(12):
            b = g % 2
            P.dma(awb[b][:], awv[:, :, g * 512:(g + 1) * 512], ["ada_w"], [("awb", b)])
            for m in range(4):
                col = g * 4 + m
                for kc in range(8):
                    P.op("pe", lambda e, b=b, m=m, kc=kc, col=col: e.matmul(
                        PS[0][:, col:col + 1], lhsT=awb[b][:, kc, m * 128:(m + 1) * 128], rhs=cact[:, kc:kc + 1],
                        start=(kc == 0), stop=(kc == 7)), [("awb", b), "cact"], [psr(0)])
        P.op("dve", lambda e, l=l: e.tensor_tensor(out=mod[:, l, :], in0=PS[0][:, 0:48], in1=vec[:, l, 0:48], op=ALU.add),
             [psr(0), "vec"], ["mod"])
        P.op("dve", lambda e, l=l: e.scalar_tensor_tensor(out=A1[:, l, :], in0=mod[:, l, 8:16], scalar=1.0, in1=vec[:, l, 48:56],
                                                          op0=ALU.add, op1=ALU.mult), ["mod", "vec"], ["A1"])
        P.op("dve", lambda e, l=l: e.scalar_tensor_tensor(out=A2[:, l, :], in0=mod[:, l, 32:40], scalar=1.0, in1=vec[:, l, 56:64],
                                                          op0=ALU.add, op1=ALU.mult), ["mod", "vec"], ["A2"])

    cos_t = P.sb([128, NT, 16], F32)
    sin_t = P.sb([128, NT, 16], F32)
    posi = P.sb([128, NT], I32)
    posf = P.sb([128, NT], F32)
    ang = P.sb([128, NT, 16], F32)
    u = P.sb([128, NT, 16], F32)
    ki = P.sb([128, NT, 16], I32)
    kf = P.sb([128, NT, 16], F32)
    r = P.sb([128, NT, 16], F32)
    fx = P.sb([128, NT, 16], F32)
    P.dma(posi[:], pos_d[:, :], ["pos_d"], ["posi"])
    P.op("dve", lambda e: e.tensor_copy(out=posf[:], in_=posi[:]), ["posi"], ["posf"])
    for i in range(16):
        P.op("dve", lambda e, i=i: e.tensor_scalar(out=ang[:, :, i], in0=posf[:], scalar1=float(INV_FREQ[i]), scalar2=None,
                                                   op0=ALU.mult), ["posf"], ["ang"])
    TWO_PI = 2.0 * math.pi
    C1 = 6.28125
    C2 = TWO_PI - C1
    for which, dst in ((0, sin_t), (1, cos_t)):
        src = ang
        if which == 1:
            P.op("dve", lambda e: e.tensor_scalar(out=r[:], in0=ang[:], scalar1=math.pi / 2, scalar2=None, op0=ALU.add),
                 ["ang"], ["r"])
            src = r
        P.op("dve", lambda e, src=src: e.tensor_scalar(out=u[:], in0=src[:], scalar1=1.0 / TWO_PI, scalar2=None, op0=ALU.mult),
             ["ang", "r"], ["u"])
        P.op("dve", lambda e: e.tensor_copy(out=ki[:], in_=u[:]), ["u"], ["ki"])
        P.op("dve", lambda e: e.tensor_copy(out=kf[:], in_=ki[:]), ["ki"], ["kf"])
        P.op("dve", lambda e, src=src: e.scalar_tensor_tensor(out=u[:], in0=kf[:], scalar=-C1, in1=src[:], op0=ALU.mult, op1=ALU.add),
             ["kf", "ang", "r"], ["u"])
        P.op("dve", lambda e: e.scalar_tensor_tensor(out=r[:], in0=kf[:], scalar=-C2, in1=u[:], op0=ALU.mult, op1=ALU.add),
             ["kf", "u"], ["r"])
        P.op("dve", lambda e: e.tensor_scalar(out=fx[:], in0=r[:], scalar1=math.pi, scalar2=-TWO_PI, op0=ALU.is_gt, op1=ALU.mult),
             ["r"], ["fx"])
        P.op("dve", lambda e: e.tensor_tensor(out=r[:], in0=r[:], in1=fx[:], op=ALU.add), ["r", "fx"], ["r"])
        P.op("dve", lambda e: e.tensor_scalar(out=fx[:], in0=r[:], scalar1=-math.pi, scalar2=TWO_PI, op0=ALU.is_lt, op1=ALU.mult),
             ["r"], ["fx"])
        P.op("dve", lambda e: e.tensor_tensor(out=r[:], in0=r[:], in1=fx[:], op=ALU.add), ["r", "fx"], ["r"])
        P.op("act", lambda e, dst=dst: e.activation(out=dst[:], in_=r[:], func=AF.Sin), ["r"], ["rope_t0"])
    P.dma(rope_d[0], cos_t[:], ["rope_t0"], ["rope_d"])
    P.dma(rope_d[1], sin_t[:], ["rope_t0"], ["rope_d"])
    P.barrier()
    P.pop()

    def do_layer(l):
        x_src = xT_in if l == 0 else xT_mid
        x_dst = yT if l == nlayers - 1 else xT_mid
        XIN = ["xT_in"] if l == 0 else [("xT_mid", 0), ("xT_mid", 1)]
        xres_out = "yT" if l == nlayers - 1 else "xT_mid"
        xsv = x_src.rearrange("(c p) s -> p c s", p=128)
        xdv = x_dst.rearrange("(c p) s -> p c s", p=128)

        P.push()
        cos_t = P.sb([128, NT, 16], F32)
        sin_t = P.sb([128, NT, 16], F32)
        gains = P.sb([128, NGAIN], F32)
        ckbm = P.sb([128, 20, 128], F32)
        P.dma(cos_t[:], rope_d[0], ["rope_d"], ["rope_t"])
        P.dma(sin_t[:], rope_d[1], ["rope_d"], ["rope_t"])
        P.dma(gains[:], gains_d[l], ["gains_d"], ["gains"])
        P.dma(ckbm[:], ckb_d[l], ["ckb_d"], ["ckbm"])
        for h in range(4):
            P.op("dve", lambda e, h=h: e.tensor_tensor(out=ckbm[:, h * 5 + 0, :], in0=ckbm[:, h * 5 + 0, :],
                                                       in1=cst[:, C_CK0:C_CK0 + 128], op=ALU.add), ["ckbm", "cst"], ["ckbm"])
            P.op("dve", lambda e, h=h: e.tensor_tensor(out=ckbm[:, h * 5 + 4, :], in0=ckbm[:, h * 5 + 4, :],
                                                       in1=cst[:, C_CK4:C_CK4 + 128], op=ALU.add), ["ckbm", "cst"], ["ckbm"])

        P.push()
        win_bf = P.sb([128, 8, 1952], BF16)
        wq_bf = P.sb([128, 2, 768], BF16)
        wkv_bf = P.sb([128, 1024], BF16)
        wiv = w_in[l].rearrange("(kc p) n -> p kc n", p=128)
        for kc in range(8):
            P.dma(win_bf[:, kc, :], wiv[:, kc, :], ["w_in"], ["win_bf"], q="pool")
        P.dma(wq_bf[:], w_q_up[l].rearrange("(kc p) n -> p kc n", p=128), ["w_q_up"], ["wq_bf"], q="pool")
        P.dma(wkv_bf[:], w_kv_up[l], ["w_kv_up"], ["wkv_bf"], q="pool")

        xb = P.sb([128, 8, 512], F32)
        sq = P.sb([128, 8, 512], BF16)
        rstd = P.sb([128, 512], F32)
        hT = P.sb([128, 8, 512], BF16)
        fmo = [P.sb([128, 512], BF16) for _ in range(4)]
        LOCAL = {"proj", "vst", "junk", "ss", "ss2", "lat", "latT", "qf", "kvf", "kf32", "kper", "t16", "ssh", "sqj", "qn", "kn",
                 "cqn", "ckn", "trq", "trk", "trc", "mlv"}

        def alloc_tile_set():
            return dict(
                proj=P.sb([128, 1440], F32), vst=[P.sb([128, 256], BF16) for _ in range(2)], junk=P.sb([128, 256], F32),
                ss=P.sb([128, 4], F32), lat=P.sb([128, 384], BF16), latT=P.sb([128, 3, 128], BF16), qf=P.sb([128, 8, 96], F32),
                kvf=P.sb([128, 8, 128], F32), kf32=P.sb([128, 8, 96], F32), kper=P.sb([128, 32], F32),
                t16=[P.sb([128, 8, 16], F32) for _ in range(4)], ssh=P.sb([128, 8], F32), sqj=P.sb([128, 8, 96], F32),
                qn=P.sb([128, 8, 96], BF16), kn=P.sb([128, 8, 96], BF16), cqn=P.sb([128, 4, 64], BF16), ckn=P.sb([128, 4, 64], BF16),
                trq=P.sb([128, 1024], BF16), trk=P.sb([128, 1024], BF16), trc=P.sb([128, 512], BF16), mlv=P.sb([128, 8, 64], BF16))
        tsets = [alloc_tile_set() for _ in range(2)]

        def tt_s1(tb, i, proj, vst, junk, ss, lat, latT, qf, kvf, kf32, kper, t16, ssh, sqj, qn, kn, cqn, ckn, trq, trk, trc, mlv):
            j = tb * 4 + i
            tk = slice(i * 128, (i + 1) * 128)
            rows = slice(j * 128, (j + 1) * 128)
            for gi, (c0, c1, pb) in enumerate(((512, 1024, 3), (1024, 1440, 4), (1440, 1952, 5))):
                for kc in range(8):
                    P.op("pe", lambda e, kc=kc, c0=c0, c1=c1, pb=pb, tk=tk: e.matmul(
                        PS[pb][:, 0:c1 - c0], lhsT=hT[:, kc, tk], rhs=win_bf[:, kc, c0:c1], start=(kc == 0), stop=(kc == 7)),
                        ["hT", "win_bf"], [psr(pb)])
            P.op("act", lambda e: e.activation(out=proj[:, 0:512], in_=PS[3][:, :], func=AF.Copy), [psr(3)], [("proj", 0)])
            P.op("dve", lambda e: e.tensor_copy(out=proj[:, 512:928], in_=PS[4][:, 0:416]), [psr(4)], [("proj", 1)])
            P.op("act", lambda e: e.activation(out=proj[:, 928:1440], in_=PS[5][:, :], func=AF.Copy), [psr(5)], [("proj", 2)])
            P.op("pool", lambda e: e.tensor_copy(out=vst[0][:], in_=proj[:, 0:256]), [("proj", 0)], [("vst", 0)])
            P.dma(sbV[rows, :], vst[0][:], [("vst", 0)], ["sbV"])
            P.op("pool", lambda e: e.tensor_copy(out=vst[1][:], in_=proj[:, 1184:1440]), [("proj", 2)], [("vst", 1)])
            P.dma(ckV[rows, :], vst[1][:], [("vst", 1)], ["ckV"])
            P.op("act", lambda e: e.activation(out=junk[:, 0:256], in_=proj[:, 256:512], func=AF.Square, accum_out=ss[:, 0:1]),
                 [("proj", 0)], ["junk", "ss"])
            P.op("act", lambda e: e.activation(out=junk[:, 0:128], in_=proj[:, 512:640], func=AF.Square, accum_out=ss[:, 1:2]),
                 [("proj", 1), "ss"], ["junk", "ss"])
            P.op("act", lambda e: e.activation(out=ss[:, 2:3], in_=ss[:, 0:1], func=AF.Sqrt, scale=1.0 / 256, bias=EPS), ["ss"], ["ss"])
            P.op("act", lambda e: e.activation(out=ss[:, 3:4], in_=ss[:, 1:2], func=AF.Sqrt, scale=1.0 / 128, bias=EPS), ["ss"], ["ss"])
            P.op("dve", lambda e: e.reciprocal(out=ss[:, 2:4], in_=ss[:, 2:4]), ["ss"], ["ss"])
            P.op("dve", lambda e: e.scalar_tensor_tensor(out=lat[:, 0:256], in0=proj[:, 256:512], scalar=ss[:, 2:3],
                                                         in1=gains[:, G_QN:G_QN + 256], op0=ALU.mult, op1=ALU.mult),
                 [("proj", 0), "ss", "gains"], ["lat"])
            P.op("dve", lambda e: e.scalar_tensor_tensor(out=lat[:, 256:384], in0=proj[:, 512:640], scalar=ss[:, 3:4],
                                                         in1=gains[:, G_KVN:G_KVN + 128], op0=ALU.mult, op1=ALU.mult),
                 [("proj", 1), "ss", "gains"], ["lat"])
            pbt = PS[2][:].bitcast(BF16)
            for k in range(3):
                P.op("pe", lambda e, k=k, pbt=pbt: e.transpose(pbt[:, k * 128:(k + 1) * 128], lat[:, k * 128:(k + 1) * 128], ident_bf),
                     ["lat", "cbf"], [psr(2)])
            P.op("dve", lambda e, pbt=pbt: e.tensor_copy(out=latT[:], in_=pbt[:, 0:384]), [psr(2)], ["latT"])
            for half, pb in ((0, 3), (1, 4)):
                for kc in range(2):
                    P.op("pe", lambda e, half=half, pb=pb, kc=kc: e.matmul(
                        PS[pb][:, 0:384], lhsT=latT[:, kc, :], rhs=wq_bf[:, kc, half * 384:(half + 1) * 384],
                        start=(kc == 0), stop=(kc == 1)), ["latT", "wq_bf"], [psr(pb)])
            for half, pb in ((0, 5), (1, 7)):
                P.op("pe", lambda e, half=half, pb=pb: e.matmul(
                    PS[pb][:, :], lhsT=latT[:, 2, :], rhs=wkv_bf[:, half * 512:(half + 1) * 512], start=True, stop=True),
                    ["latT", "wkv_bf"], [psr(pb)])
            P.op("act", lambda e: e.activation(out=qf[:, 0:4, :], in_=PS[3][:, 0:384], func=AF.Copy), [psr(3)], [("qf", 0)])
            P.op("act", lambda e: e.activation(out=qf[:, 4:8, :], in_=PS[4][:, 0:384], func=AF.Copy), [psr(4)], [("qf", 1)])
            P.op("dve", lambda e: e.tensor_copy(out=kvf[:, 0:4, :], in_=PS[5][:, :]), [psr(5)], [("kvf", 0)])
            P.op("dve", lambda e: e.tensor_copy(out=kvf[:, 4:8, :], in_=PS[7][:, :]), [psr(7)], [("kvf", 1)])

        def tt_s2(tb, i, proj, vst, junk, ss, lat, latT, qf, kvf, kf32, kper, t16, ssh, sqj, qn, kn, cqn, ckn, trq, trk, trc, mlv):
            j = tb * 4 + i
            rows = slice(j * 128, (j + 1) * 128)
            pbt = PS[6][:].bitcast(BF16)
            cosb = cos_t[:, j, :]
            sinb = sin_t[:, j, :]
            cos8 = cosb.unsqueeze(1).broadcast_to([128, 8, 16])
            sin8 = sinb.unsqueeze(1).broadcast_to([128, 8, 16])
            QF = [("qf", 0), ("qf", 1)]
            P.op("pool", lambda e, cos8=cos8: e.tensor_tensor(out=t16[0][:], in0=qf[:, :, 64:80], in1=cos8, op=ALU.mult), QF + ["rope_t"], [("t16", 0)])
            P.op("pool", lambda e, sin8=sin8: e.tensor_tensor(out=t16[1][:], in0=qf[:, :, 80:96], in1=sin8, op=ALU.mult), QF + ["rope_t"], [("t16", 1)])
            P.op("pool", lambda e, cos8=cos8: e.tensor_tensor(out=t16[2][:], in0=qf[:, :, 80:96], in1=cos8, op=ALU.mult), QF + ["rope_t"], [("t16", 2)])
            P.op("pool", lambda e, sin8=sin8: e.tensor_tensor(out=t16[3][:], in0=qf[:, :, 64:80], in1=sin8, op=ALU.mult), QF + ["rope_t"], [("t16", 3)])
            P.op("dve", lambda e: e.tensor_tensor(out=qf[:, :, 64:80], in0=t16[0][:], in1=t16[1][:], op=ALU.subtract),
                 [("t16", 0), ("t16", 1)], QF)
            P.op("dve", lambda e: e.tensor_tensor(out=qf[:, :, 80:96], in0=t16[2][:], in1=t16[3][:], op=ALU.add),
                 [("t16", 2), ("t16", 3)], QF)
            P.op("dve", lambda e, cosb=cosb: e.tensor_tensor(out=t16[0][:, 0, :], in0=proj[:, 640:656], in1=cosb, op=ALU.mult), [("proj", 1), "rope_t"], [("t16", 0)])
            P.op("dve", lambda e, sinb=sinb: e.tensor_tensor(out=t16[1][:, 0, :], in0=proj[:, 656:672], in1=sinb, op=ALU.mult), [("proj", 1), "rope_t"], [("t16", 1)])
            P.op("dve", lambda e, cosb=cosb: e.tensor_tensor(out=t16[2][:, 0, :], in0=proj[:, 656:672], in1=cosb, op=ALU.mult), [("proj", 1), "rope_t"], [("t16", 2)])
            P.op("dve", lambda e, sinb=sinb: e.tensor_tensor(out=t16[3][:, 0, :], in0=proj[:, 640:656], in1=sinb, op=ALU.mult), [("proj", 1), "rope_t"], [("t16", 3)])
            P.op("dve", lambda e: e.tensor_tensor(out=kper[:, 0:16], in0=t16[0][:, 0, :], in1=t16[1][:, 0, :], op=ALU.subtract),
                 [("t16", 0), ("t16", 1)], ["kper"])
            P.op("dve", lambda e: e.tensor_tensor(out=kper[:, 16:32], in0=t16[2][:, 0, :], in1=t16[3][:, 0, :], op=ALU.add),
                 [("t16", 2), ("t16", 3)], ["kper"])
            KV = [("kvf", 0), ("kvf", 1)]
            P.op("pool", lambda e: e.tensor_copy(out=kf32[:, :, 0:64], in_=kvf[:, :, 0:64]), KV, ["kf32"])
            P.op("pool", lambda e: e.tensor_copy(out=kf32[:, :, 64:96], in_=kper[:].unsqueeze(1).broadcast_to([128, 8, 32])), ["kper", "kf32"], ["kf32"])
            P.op("pool", lambda e: e.tensor_copy(out=mlv[:], in_=kvf[:, :, 64:128]), KV, ["mlv"])
            P.dma(mlV[rows, :], mlv[:].rearrange("p h d -> p (h d)"), ["mlv"], ["mlV"])
            for (src, srcres, gofs, dstn, dres, sc) in ((qf, QF, G_QQK, qn, "qn", 96 ** -0.5), (kf32, ["kf32"], G_KQK, kn, "kn", 1.0)):
                P.op("pool", lambda e, src=src: e.tensor_tensor(out=sqj[:], in0=src[:], in1=src[:], op=ALU.mult), srcres, ["sqj"])
                P.op("dve", lambda e: e.tensor_reduce(out=ssh[:], in_=sqj[:], axis=AX.X, op=ALU.add), ["sqj"], ["ssh"])
                P.op("act", lambda e: e.activation(out=ssh[:], in_=ssh[:], func=AF.Sqrt, scale=1.0 / 96, bias=EPS), ["ssh"], ["ssh"])
                P.op("dve", lambda e: e.reciprocal(out=ssh[:], in_=ssh[:]), ["ssh"], ["ssh"])
                P.op("dve", lambda e, src=src: e.tensor_tensor(out=sqj[:], in0=src[:], in1=ssh[:].unsqueeze(2).broadcast_to([128, 8, 96]),
                                                               op=ALU.mult), srcres + ["ssh"], ["sqj"])
                P.op("dve", lambda e, gofs=gofs, dstn=dstn, sc=sc: e.scalar_tensor_tensor(
                    out=dstn[:], in0=sqj[:], scalar=float(sc), in1=gains[:, gofs:gofs + 96].unsqueeze(1).broadcast_to([128, 8, 96]),
                    op0=ALU.mult, op1=ALU.mult), ["sqj", "gains"], [dres])
            for (c0, pres, gofs, dstn, dres, sc) in ((672, ("proj", 1), G_CQ, cqn, "cqn", 0.125), (928, ("proj", 2), G_CK, ckn, "ckn", 1.0)):
                srcv = proj[:, c0:c0 + 256].rearrange("p (h d) -> p h d", h=4)
                sq4 = sqj[:, 0:4, 0:64]
                P.op("dve", lambda e, srcv=srcv, sq4=sq4: e.tensor_tensor(out=sq4, in0=srcv, in1=srcv, op=ALU.mult), [pres], ["sqj"])
                P.op("dve", lambda e, sq4=sq4: e.tensor_reduce(out=ssh[:, 0:4], in_=sq4, axis=AX.X, op=ALU.add), ["sqj"], ["ssh"])
                P.op("act", lambda e: e.activation(out=ssh[:, 0:4], in_=ssh[:, 0:4], func=AF.Sqrt, scale=1.0 / 64, bias=EPS), ["ssh"], ["ssh"])
                P.op("dve", lambda e: e.reciprocal(out=ssh[:, 0:4], in_=ssh[:, 0:4]), ["ssh"], ["ssh"])
                P.op("dve", lambda e, srcv=srcv, sq4=sq4: e.tensor_tensor(out=sq4, in0=srcv, in1=ssh[:, 0:4].unsqueeze(2).broadcast_to([128, 4, 64]),
                                                                          op=ALU.mult), [pres, "ssh"], ["sqj"])
                P.op("dve", lambda e, gofs=gofs, dstn=dstn, sc=sc, sq4=sq4: e.scalar_tensor_tensor(
                    out=dstn[:], in0=sq4, scalar=float(sc), in1=gains[:, gofs:gofs + 64].unsqueeze(1).broadcast_to([128, 4, 64]),
                    op0=ALU.mult, op1=ALU.mult), ["sqj", "gains"], [dres])
            for (srcn, sres, dstd, dres, trx, tres, pbk) in ((qn, "qn", mlQT, "mlQT", trq, "trq", 6), (kn, "kn", mlKT, "mlKT", trk, "trk", 0)):
                pbx = PS[pbk][:].bitcast(BF16)
                for h in range(8):
                    P.op("pe", lambda e, h=h, srcn=srcn, pbx=pbx: e.transpose(pbx[0:96, h * 128:(h + 1) * 128], srcn[:, h, :], ident_bf),
                         [sres, "cbf"], [psr(pbk)])
                P.op("act", lambda e, pbx=pbx, trx=trx: e.activation(out=trx[0:96, :], in_=pbx[0:96, :], func=AF.Copy), [psr(pbk)], [tres])
                P.dma(dstd[:, :, rows].rearrange("h d t -> d h t"), trx[0:96, :].rearrange("d (h t) -> d h t", h=8), [tres], [dres])
            for pr in range(2):
                P.op("pe", lambda e, pr=pr, pbt=pbt: e.transpose(pbt[:, pr * 128:(pr + 1) * 128], cqn[:, 2 * pr:2 * pr + 2, :].rearrange("p h d -> p (h d)"), ident_bf),
                     ["cqn", "cbf"], [psr(6)])
                P.op("pe", lambda e, pr=pr, pbt=pbt: e.transpose(pbt[:, (2 + pr) * 128:(3 + pr) * 128], ckn[:, 2 * pr:2 * pr + 2, :].rearrange("p h d -> p (h d)"), ident_bf),
                     ["ckn", "cbf"], [psr(6)])
            P.op("act", lambda e, pbt=pbt: e.activation(out=trc[:, 0:512], in_=pbt[:, 0:512], func=AF.Copy), [psr(6)], ["trc"])
            for pr in range(2):
                P.dma(ckQZ[2 * pr, 0:64, rows], trc[0:64, pr * 128:(pr + 1) * 128], ["trc"], ["ckQZ"])
                P.dma(ckQZ[2 * pr + 1, 64:128, rows], trc[64:128, pr * 128:(pr + 1) * 128], ["trc"], ["ckQZ"])
                P.dma(ckKT[pr, :, rows], trc[:, (2 + pr) * 128:(3 + pr) * 128], ["trc"], ["ckKT"])

        for tb in range(NB):
            t0 = tb * 512
            P.dma(xb[:], xsv[:, :, t0:t0 + 512], XIN, ["xb"])
            P.op("act", lambda e: e.activation(out=sq[:], in_=xb[:], func=AF.Square), ["xb"], ["sq"])
            for c in range(8):
                P.op("pe", lambda e, c=c: e.matmul(PS[0][:, :], lhsT=ones_bf[:], rhs=sq[:, c, :], start=(c == 0), stop=(c == 7)),
                     ["sq", "ones_bf"], [psr(0)])
            P.op("act", lambda e: e.activation(out=rstd[:], in_=PS[0][:, :], func=AF.Sqrt, scale=1.0 / D, bias=EPS), [psr(0)], ["rstd"])
            P.op("dve", lambda e: e.reciprocal(out=rstd[:], in_=rstd[:]), ["rstd"], ["rstd"])
            for c in range(8):
                P.op("dve", lambda e, c=c: e.scalar_tensor_tensor(out=xb[:, c, :], in0=xb[:, c, :], scalar=A1[:, l, c:c + 1], in1=rstd[:],
                                                                  op0=ALU.mult, op1=ALU.mult), ["xb", "A1", "rstd"], ["xb"])
                P.op("act", lambda e, c=c: e.activation(out=hT[:, c, :], in_=xb[:, c, :], func=AF.Identity, bias=mod[:, l, c:c + 1], scale=1.0),
                     ["xb", "mod"], ["hT"])
            for m in range(4):
                pb = 1 + (m % 2)
                for kc in range(8):
                    P.op("pe", lambda e, m=m, kc=kc, pb=pb: e.matmul(PS[pb][:, :], lhsT=win_bf[:, kc, m * 128:(m + 1) * 128], rhs=hT[:, kc, :],
                                                                    start=(kc == 0), stop=(kc == 7)), ["win_bf", "hT"], [psr(pb)])
                fb = m
                if m < 2:
                    P.op("act", lambda e, pb=pb, fb=fb: e.activation(out=fmo[fb][:], in_=PS[pb][:, :], func=AF.Copy, scale=0.125),
                         [psr(pb)], [("fmo", fb)])
                    P.dma(sbQZ[2 * m, 0:64, t0:t0 + 512], fmo[fb][0:64, :], [("fmo", fb)], ["sbQZ"])
                    P.dma(sbQZ[2 * m + 1, 64:128, t0:t0 + 512], fmo[fb][64:128, :], [("fmo", fb)], ["sbQZ"])
                else:
                    P.op("act", lambda e, pb=pb, fb=fb: e.activation(out=fmo[fb][:], in_=PS[pb][:, :], func=AF.Copy),
                         [psr(pb)], [("fmo", fb)])
                    P.dma(sbKT[m - 2, :, t0:t0 + 512], fmo[fb][:, :], [("fmo", fb)], ["sbKT"])
            for i in range(4):
                jj = tb * 4 + i
                caps = []
                P.capture = []
                P.local = (LOCAL, jj % 2)
                tt_s1(tb, i, **tsets[jj % 2])
                caps.append(P.capture)
                if i >= 1:
                    P.capture = []
                    P.local = (LOCAL, (jj - 1) % 2)
                    tt_s2(tb, i - 1, **tsets[(jj - 1) % 2])
                    caps.append(P.capture)
                P.capture = None
                P.local = None
                P.replay_interleaved(caps)
            P.local = (LOCAL, (tb * 4 + 3) % 2)
            tt_s2(tb, 3, **tsets[(tb * 4 + 3) % 2])
            P.local = None
        P.barrier()
        P.pop()
        if stop_after == ("P1", l):
            return True

        def norm_pass(o_t, Dg, gofs, row0):
            nch = Dg // 128
            onb = [P.sb([128, Dg], BF16) for _ in range(4)]
            stg = [P.sb([128, nch, 128], BF16) for _ in range(4)]
            gss = P.sb([128, 2], F32)
            gj = P.sb([128, Dg], F32)
            for m in range(NT):
                b = m % 4
                P.op("act", lambda e, m=m: e.activation(out=gj[:], in_=o_t[:, m, :], func=AF.Square, accum_out=gss[:, 0:1]), ["o_t"], ["gj", "gss"])
                P.op("act", lambda e: e.activation(out=gss[:, 1:2], in_=gss[:, 0:1], func=AF.Sqrt, scale=1.0 / Dg, bias=EPS), ["gss"], ["gss"])
                P.op("dve", lambda e: e.reciprocal(out=gss[:, 1:2], in_=gss[:, 1:2]), ["gss"], ["gss"])
                P.op("dve", lambda e, m=m, b=b: e.scalar_tensor_tensor(out=onb[b][:], in0=o_t[:, m, :], scalar=gss[:, 1:2],
                                                                       in1=gains[:, gofs:gofs + Dg], op0=ALU.mult, op1=ALU.mult),
                     ["o_t", "gss", "gains"], [("onb", b)])
                pbt = PS[7][:].bitcast(BF16)
                for k in range(nch):
                    P.op("pe", lambda e, k=k, b=b, pbt=pbt: e.transpose(pbt[:, k * 128:(k + 1) * 128], onb[b][:, k * 128:(k + 1) * 128], ident_bf),
                         [("onb", b), "cbf"], [psr(7)])
                P.op("act", lambda e, b=b, pbt=pbt: e.activation(out=stg[b][:].rearrange("p c t -> p (c t)"), in_=pbt[:, 0:nch * 128], func=AF.Copy),
                     [psr(7)], [("stg", b)])
                P.dma(mgT[row0:row0 + Dg, m * 128:(m + 1) * 128].rearrange("(c p) t -> p c t", p=128), stg[b][:], [("stg", b)], ["mgT"])

        P.push()
        QZ = P.sb([128, 4, S], BF16)
        KT = P.sb([128, 2, S], BF16)
        VA = P.sb([128, NT, 256], BF16)
        o_t = P.sb([128, NT, 256], F32)
        P.dma(QZ[:], sbQZ.rearrange("h p s -> p h s"), ["sbQZ"], ["QZ"])
        P.dma(KT[:], sbKT.rearrange("h p s -> p h s"), ["sbKT"], ["KT"])
        P.dma(VA[:], sbV.rearrange("(n p) c -> p n c", p=128), ["sbV"], ["VA"])
        Eb = [P.sb([128, 512], F32) for _ in range(2)]
        SPb = [P.sb([128, 512], BF16) for _ in range(2)]
        Wb = [P.sb([128, 512], BF16) for _ in range(2)]
        chi = [P.sb([128, 128], BF16) for _ in range(2)]
        clo = [P.sb([128, 128], BF16) for _ in range(2)]
        items = []
        for m in range(NT):
            for h in range(4):
                blocks = list(range(m, -1, -1))
                ng = (len(blocks) + 3) // 4
                for g in range(ng):
                    items.append((m, h, g, blocks[g * 4:(g + 1) * 4], g == ng - 1))
        Zp = [0, 1]
        LWp = [2, 3]
        CP = 4
        OP = [5, 6]

        def a_st1(it, i):
            m, h, g, blks, last = it
            par = i % 2
            n = len(blks)
            qs = slice(m * 128, (m + 1) * 128)
            for c, kb in enumerate(blks):
                P.op("pe", lambda e, c=c, kb=kb, h=h, par=par, qs=qs: e.matmul(
                    PS[Zp[par]][:, c * 128:(c + 1) * 128], lhsT=KT[:, h // 2, kb * 128:(kb + 1) * 128], rhs=QZ[:, h, qs], start=True, stop=True),
                    ["KT", "QZ"], [psr(Zp[par])])
            P.op("act", lambda e, par=par, n=n: e.activation(out=Eb[par][:, 0:n * 128], in_=PS[Zp[par]][:, 0:n * 128], func=AF.Exp),
                 [psr(Zp[par])], [("Eb", par)])
            P.op("act", lambda e, par=par, n=n: e.activation(out=SPb[par][:, 0:n * 128], in_=Eb[par][:, 0:n * 128], func=AF.Ln, bias=1.0),
                 [("Eb", par)], [("SPb", par)])
            if g == 0:
                P.op("dve", lambda e, par=par: e.tensor_tensor(out=SPb[par][:, 0:128], in0=SPb[par][:, 0:128], in1=strict_bf, op=ALU.mult),
                     [("SPb", par), "cbf"], [("SPb", par)])

        def a_st2(it, i):
            m, h, g, blks, last = it
            par = i % 2
            n = len(blks)
            qs = slice(m * 128, (m + 1) * 128)
            cpar = g % 2
            for c in range(n):
                P.op("pe", lambda e, c=c, par=par, g=g, n=n, last=last: e.matmul(
                    PS[CP][:, 0:128], lhsT=ones_bf[:], rhs=SPb[par][:, c * 128:(c + 1) * 128],
                    start=(g == 0 and c == 0), stop=(last and c == n - 1), skip_group_check=True), [("SPb", par), "ones_bf"], [psr(CP)])
            if not last:
                P.op("dve", lambda e, cpar=cpar: e.tensor_copy(out=chi[1 - cpar][:], in_=PS[CP][:, 0:128]),
                     [psr(CP)], [("chi", 1 - cpar)])
                P.op("dve", lambda e, cpar=cpar: e.tensor_tensor(out=clo[1 - cpar][:], in0=PS[CP][:, 0:128], in1=chi[1 - cpar][:], op=ALU.subtract),
                     [psr(CP), ("chi", 1 - cpar)], [("clo", 1 - cpar)])
            mms = []
            for c, kb in enumerate(blks):
                cs = slice(c * 128, (c + 1) * 128)
                mms.append((PS[LWp[par]][:, cs], KT[:, h // 2, kb * 128:(kb + 1) * 128], QZ[:, h, qs], ["KT", "QZ"]))
            mms.append((PS[LWp[par]][:, 0:n * 128], negut_bf, SPb[par][:, 0:n * 128], ["cbf", ("SPb", par)]))
            for c2 in range(n - 1):
                k = n - 1 - c2
                mms.append((PS[LWp[par]][:, (c2 + 1) * 128:n * 128].rearrange("p (k i) -> p k i", k=k), negones_bf[:],
                            SPb[par][:, c2 * 128:(c2 + 1) * 128].unsqueeze(1).broadcast_to([128, k, 128]), ["negones_bf", ("SPb", par)]))
            if g > 0:
                for ct, cres in ((chi, "chi"), (clo, "clo")):
                    mms.append((PS[LWp[par]][:, 0:n * 128].rearrange("p (k i) -> p k i", k=n), negid_bf[:],
                                ct[cpar][:].unsqueeze(1).broadcast_to([128, n, 128]), ["negid_bf", (cres, cpar)]))
            if g == 0:
                mms.append((PS[LWp[par]][:, 0:128], ident_bf, nma_bf, ["cbf"]))
            for k, (ot, lt, rh, rd) in enumerate(mms):
                P.op("pe", lambda e, ot=ot, lt=lt, rh=rh, k=k, nm=len(mms): e.matmul(
                    ot, lhsT=lt, rhs=rh, start=(k == 0), stop=(k == nm - 1), skip_group_check=True), rd, [psr(LWp[par])])
            P.op("act", lambda e, par=par, n=n: e.activation(out=Wb[par][:, 0:n * 128], in_=PS[LWp[par]][:, 0:n * 128], func=AF.Exp),
                 [psr(LWp[par])], [("Wb", par)])

        def a_st3(it, i):
            m, h, g, blks, last = it
            par = i % 2
            opar = (m * 4 + h) % 2
            n = len(blks)
            for c, kb in enumerate(blks):
                P.op("pe", lambda e, c=c, kb=kb, par=par, opar=opar, h=h: e.matmul(
                    PS[OP[opar]][:, 0:64], lhsT=Wb[par][:, c * 128:(c + 1) * 128], rhs=VA[:, kb, h * 64:(h + 1) * 64],
                    start=(g == 0 and c == 0), stop=(last and c == n - 1)), [("Wb", par), "VA"], [psr(OP[opar])])
            if last:
                P.op("dve", lambda e, opar=opar, m=m, h=h: e.tensor_copy(out=o_t[:, m, h * 64:(h + 1) * 64], in_=PS[OP[opar]][:, 0:64]),
                     [psr(OP[opar])], ["o_t"])

        NI = len(items)
        for i in range(NI + 2):
            if i < NI:
                a_st1(items[i], i)
            if 1 <= i <= NI:
                a_st2(items[i - 1], i - 1)
            if 2 <= i:
                a_st3(items[i - 2], i - 2)
        norm_pass(o_t, 256, G_OUT, 0)
        P.barrier()
        P.pop()

        def softmax_attn(nheads, Dg, load_head, blocks_of, add_of, gofs, row0):
            P.push()
            o_t = P.sb([128, NT, Dg], F32)
            Wb = [P.sb([128, 512], BF16) for _ in range(2)]
            rc = [P.sb([128, 1], F32) for _ in range(2)]
            hb = [load_head(b) for b in range(2)]
            for b in range(2):
                P.op("pool", lambda e, b=b: e.memset(hb[b][2][:, :, 64:65], 1.0), [], [("hv", b)])
            Sp = [0, 1]
            Op = [2, 3]
            items = []
            for h in range(nheads):
                for m in range(NT):
                    blocks = blocks_of(m)
                    ng = (len(blocks) + 3) // 4
                    for g in range(ng):
                        items.append((h, m, g, blocks[g * 4:(g + 1) * 4], g == ng - 1))

            def st1(it, i):
                h, m, g, blks, last = it
                par = i % 2
                b = h % 2
                qt, kt, vt, fill = hb[b]
                if m == 0 and g == 0:
                    fill(h, b)
                n = len(blks)
                qs = slice(m * 128, (m + 1) * 128)
                for c, kb in enumerate(blks):
                    cs = slice(c * 128, (c + 1) * 128)
                    ad = add_of(h, m, kb)
                    P.op("pe", lambda e, cs=cs, kb=kb, par=par, qs=qs, kt=kt, qt=qt, ad=ad: e.matmul(
                        PS[Sp[par]][:, cs], lhsT=kt[:, kb * 128:(kb + 1) * 128], rhs=qt[:, qs], start=True, stop=(ad is None or ad[0] != "pe")),
                        [("hk", b), ("hq", b)], [psr(Sp[par])])
                    if ad is not None and ad[0] == "pe":
                        P.op("pe", lambda e, cs=cs, par=par, ad=ad: e.matmul(PS[Sp[par]][:, cs], lhsT=ident_bf, rhs=ad[1], start=False, stop=True),
                             ["cbf"], [psr(Sp[par])])
                    elif ad is not None:
                        P.op("dve", lambda e, cs=cs, par=par, ad=ad: e.tensor_tensor(out=PS[Sp[par]][:, cs], in0=PS[Sp[par]][:, cs], in1=ad[1], op=ALU.add),
                             [psr(Sp[par]), "ckbm"], [psr(Sp[par])])
                P.op("act", lambda e, par=par, n=n: e.activation(out=Wb[par][:, 0:n * 128], in_=PS[Sp[par]][:, 0:n * 128], func=AF.Exp),
                     [psr(Sp[par])], [("Wb", par)])

            def st2(it, i):
                h, m, g, blks, last = it
                par = i % 2
                b = h % 2
                qt, kt, vt, fill = hb[b]
                opar = (h * NT + m) % 2
                n = len(blks)
                for c, kb in enumerate(blks):
                    P.op("pe", lambda e, c=c, kb=kb, par=par, opar=opar, vt=vt: e.matmul(
                        PS[Op[opar]][:, 0:65], lhsT=Wb[par][:, c * 128:(c + 1) * 128], rhs=vt[:, kb, :],
                        start=(g == 0 and c == 0), stop=(last and c == n - 1)), [("Wb", par), ("hv", b)], [psr(Op[opar])])
                if last:
                    P.op("dve", lambda e, opar=opar: e.reciprocal(out=rc[opar][:], in_=PS[Op[opar]][:, 64:65]), [psr(Op[opar])], [("rc", opar)])
                    P.op("dve", lambda e, opar=opar, m=m, h=h: e.tensor_scalar(out=o_t[:, m, h * 64:(h + 1) * 64], in0=PS[Op[opar]][:, 0:64],
                                                                              scalar1=rc[opar][:, 0:1], scalar2=None, op0=ALU.mult),
                         [psr(Op[opar]), ("rc", opar)], ["o_t"])

            NI = len(items)
            for i in range(NI + 1):
                if i < NI:
                    st1(items[i], i)
                if i >= 1:
                    st2(items[i - 1], i - 1)
            norm_pass(o_t, Dg, gofs, row0)
            P.barrier()
            P.pop()

        def mla_load(b):
            qt = P.sb([96, S], BF16)
            kt = P.sb([96, S], BF16)
            vt = P.sb([128, NT, 65], BF16)

            def fill(h, b):
                P.dma(qt[:], mlQT[h], ["mlQT"], [("hq", b)])
                P.dma(kt[:], mlKT[h], ["mlKT"], [("hk", b)])
                P.dma(vt[:, :, 0:64], mlV[:, h * 64:(h + 1) * 64].rearrange("(n p) c -> p n c", p=128), ["mlV"], [("hv", b)])
            return (qt, kt, vt, fill)

        softmax_attn(8, 512, mla_load, lambda m: list(range(m, -1, -1)),
                     lambda h, m, kb: (("pe", mb_bf) if kb == m else None), G_OUT + 256, 256)

        def ck_load(b):
            qt = P.sb([128, S], BF16)
            kt = P.sb([128, S], BF16)
            vt = P.sb([128, NT, 65], BF16)

            def fill(h, b):
                P.dma(qt[:], ckQZ[h], ["ckQZ"], [("hq", b)])
                P.dma(kt[:], ckKT[h // 2], ["ckKT"], [("hk", b)])
                P.dma(vt[:, :, 0:64], ckV[:, h * 64:(h + 1) * 64].rearrange("(n p) c -> p n c", p=128), ["ckV"], [("hv", b)])
            return (qt, kt, vt, fill)

        softmax_attn(4, 256, ck_load, lambda m: [kb for kb in range(m, m - 5, -1) if kb >= 0],
                     lambda h, m, kb: ("dve", ckbm[:, h * 5 + (kb - (m - 4)), :]), G_OUT + 768, 768)
        if stop_after == ("P2", l):
            return True

        P.pop()
        P.push()
        moe = (l % 2 == 1)
        li = l // 2
        NSB = TS // 512
        wo_bf = P.sb([128, 8, D], BF16)
        wgb = [P.sb([128, 8, 512], BF16) for _ in range(2)]
        wub = [P.sb([128, 8, 512], BF16) for _ in range(2)]
        wdb = [P.sb([128, 12, 512], BF16) for _ in range(2)]
        P.dma(wo_bf[:], w_out[l].rearrange("(kc p) n -> p kc n", p=128), ["w_out"], ["wo_bf"], q="pool")
        x1 = P.sb([128, 8, TS], F32)
        h2T = P.sb([128, 8, TS], BF16)
        h2f = P.sb([128, 8, 512], F32)
        mtb = P.sb([128, 8, 512], BF16)
        rstd = P.sb([128, 512], F32)
        AT = P.sb([128, 12, TS], BF16)
        sg = [P.sb([128, 512], BF16) for _ in range(2)]
        if moe:
            wr = P.sb([128, 8, NE], F32)
            P.dma(wr[:], router[li].rearrange("(kc p) n -> p kc n", p=128), ["router"], ["wr"])
            lg = P.sb([128, 4, NE], F32)
            lg2 = P.sb([128, 4, NE], F32)
            eq1 = P.sb([128, 4, NE], F32)
            eq2 = P.sb([128, 4, NE], F32)
            mx = P.sb([128, 4, 4], F32)
            gts = P.sb([128, 4, NE], F32)
            gT = P.sb([8, TS], F32)
            Gb = [P.sb([128, 512], F32) for _ in range(NSB)]
            ytmp = [P.sb([128, 512], F32) for _ in range(2)]
        gcount = [0, 0]
        for tsb in range(S // TS):
            for sbi in range(NSB):
                t0 = tsb * TS + sbi * 512
                xs = slice(sbi * 512, (sbi + 1) * 512)
                P.dma(x1[:, :, xs], xsv[:, :, t0:t0 + 512], XIN, [("x1", sbi)])
                P.dma(mtb[:], mgT.rearrange("(c p) s -> p c s", p=128)[:, :, t0:t0 + 512], ["mgT"], ["mtb"])
                for c in range(8):
                    pb = c % 2
                    for kc in range(8):
                        P.op("pe", lambda e, c=c, kc=kc, pb=pb: e.matmul(PS[pb][:, :], lhsT=wo_bf[:, kc, c * 128:(c + 1) * 128], rhs=mtb[:, kc, :],
                                                                        start=(kc == 0), stop=(kc == 7)), ["wo_bf", "mtb"], [psr(pb)])
                    P.op("dve", lambda e, c=c, pb=pb, xs=xs: e.scalar_tensor_tensor(out=x1[:, c, xs], in0=PS[pb][:, :], scalar=mod[:, l, 16 + c:17 + c],
                                                                                    in1=x1[:, c, xs], op0=ALU.mult, op1=ALU.add),
                         [psr(pb), "mod", ("x1", sbi)], [("x1", sbi)])
                P.op("act", lambda e, xs=xs: e.activation(out=h2T[:, :, xs], in_=x1[:, :, xs], func=AF.Square), [("x1", sbi)], [("h2T", sbi)])
                for c in range(8):
                    P.op("pe", lambda e, c=c, xs=xs: e.matmul(PS[2][:, :], lhsT=ones_bf[:], rhs=h2T[:, c, xs], start=(c == 0), stop=(c == 7)),
                         [("h2T", sbi), "ones_bf"], [psr(2)])
                P.op("act", lambda e: e.activation(out=rstd[:], in_=PS[2][:, :], func=AF.Sqrt, scale=1.0 / D, bias=EPS), [psr(2)], ["rstd"])
                P.op("dve", lambda e: e.reciprocal(out=rstd[:], in_=rstd[:]), ["rstd"], ["rstd"])
                for c in range(8):
                    P.op("dve", lambda e, c=c, xs=xs: e.scalar_tensor_tensor(out=h2f[:, c, :], in0=x1[:, c, xs], scalar=A2[:, l, c:c + 1], in1=rstd[:],
                                                                             op0=ALU.mult, op1=ALU.mult), [("x1", sbi), "A2", "rstd"], [("h2f", c)])
                    P.op("act", lambda e, c=c: e.activation(out=h2f[:, c, :], in_=h2f[:, c, :], func=AF.Identity, bias=mod[:, l, 24 + c:25 + c], scale=1.0),
                         [("h2f", c), "mod"], [("h2f", c)])
                    P.op("pool", lambda e, c=c, xs=xs: e.tensor_copy(out=h2T[:, c, xs], in_=h2f[:, c, :]), [("h2f", c)], [("h2T", sbi)])
                if moe:
                    H2F = [("h2f", c) for c in range(8)]
                    for i in range(4):
                        for kc in range(8):
                            P.op("pe", lambda e, i=i, kc=kc: e.matmul(PS[3][:, i * 8:(i + 1) * 8], lhsT=h2f[:, kc, i * 128:(i + 1) * 128], rhs=wr[:, kc, :],
                                                                      start=(kc == 0), stop=(kc == 7)), H2F + ["wr"], [psr(3)])
                    P.op("dve", lambda e: e.tensor_copy(out=lg[:].rearrange("p a b -> p (a b)"), in_=PS[3][:, 0:32]), [psr(3)], ["lg"])
                    P.op("dve", lambda e: e.tensor_reduce(out=mx[:, :, 0], in_=lg[:], axis=AX.X, op=ALU.max), ["lg"], ["mx"])
                    P.op("dve", lambda e: e.tensor_tensor(out=eq1[:], in0=lg[:], in1=mx[:, :, 0:1].broadcast_to([128, 4, NE]), op=ALU.is_equal),
                         ["lg", "mx"], ["eq1"])
                    P.op("dve", lambda e: e.scalar_tensor_tensor(out=lg2[:], in0=eq1[:], scalar=-1e30, in1=lg[:], op0=ALU.mult, op1=ALU.add),
                         ["eq1", "lg"], ["lg2"])
                    P.op("dve", lambda e: e.tensor_reduce(out=mx[:, :, 1], in_=lg2[:], axis=AX.X, op=ALU.max), ["lg2", "mx"], ["mx"])
                    P.op("dve", lambda e: e.tensor_tensor(out=eq2[:], in0=lg2[:], in1=mx[:, :, 1:2].broadcast_to([128, 4, NE]), op=ALU.is_equal),
                         ["lg2", "mx"], ["eq2"])
                    P.op("dve", lambda e: e.tensor_tensor(out=mx[:, :, 2], in0=mx[:, :, 1], in1=mx[:, :, 0], op=ALU.subtract), ["mx"], ["mx"])
                    P.op("act", lambda e: e.activation(out=mx[:, :, 2], in_=mx[:, :, 2], func=AF.Exp), ["mx"], ["mx"])
                    P.op("dve", lambda e: e.tensor_scalar(out=mx[:, :, 2], in0=mx[:, :, 2], scalar1=1.0, scalar2=None, op0=ALU.add), ["mx"], ["mx"])
                    P.op("dve", lambda e: e.reciprocal(out=mx[:, :, 2], in_=mx[:, :, 2]), ["mx"], ["mx"])
                    P.op("dve", lambda e: e.tensor_scalar(out=mx[:, :, 3], in0=mx[:, :, 2], scalar1=-1.0, scalar2=1.0, op0=ALU.mult, op1=ALU.add), ["mx"], ["mx"])
                    P.op("dve", lambda e: e.tensor_tensor(out=eq1[:], in0=eq1[:], in1=mx[:, :, 2:3].broadcast_to([128, 4, NE]), op=ALU.mult), ["eq1", "mx"], ["eq1"])
                    P.op("dve", lambda e: e.tensor_tensor(out=eq2[:], in0=eq2[:], in1=mx[:, :, 3:4].broadcast_to([128, 4, NE]), op=ALU.mult), ["eq2", "mx"], ["eq2"])
                    P.op("dve", lambda e: e.tensor_tensor(out=gts[:], in0=eq1[:], in1=eq2[:], op=ALU.add), ["eq1", "eq2"], ["gts"])
                    for i in range(4):
                        P.op("pe", lambda e, i=i: e.transpose(PS[2][0:8, i * 128:(i + 1) * 128], gts[:, i, :], ident_f), ["gts", "cst"], [psr(2)])
                    P.op("act", lambda e, xs=xs: e.activation(out=gT[:, xs], in_=PS[2][0:8, :], func=AF.Copy), [psr(2)], [("gT", sbi)])
                    if debug:
                        P.dma(dbgG[:, t0:t0 + 512], gT[:, xs], [("gT", sbi)], ["dbgG"])
            nexp = NE if moe else 1
            for ex in range(nexp):
                if moe:
                    wg_d, wu_d, wd_d = moe_g[li, ex], moe_u[li, ex], moe_d[li, ex]
                else:
                    wg_d, wu_d, wd_d = ffn_g[li], ffn_u[li], ffn_d[li]
                wgv = wg_d.rearrange("(kc p) n -> p kc n", p=128)
                wuv = wu_d.rearrange("(kc p) n -> p kc n", p=128)
                wdv = wd_d.rearrange("(j p) n -> p j n", p=128)
                for pas, (j0, njp, groups) in enumerate(((0, 12, ((0, 4), (4, 4), (8, 4))), (12, 10, ((12, 4), (16, 4), (20, 2))))):
                    for (js, nj) in groups:
                        wbuf = gcount[0] % 2
                        gcount[0] += 1
                        P.dma(wgb[wbuf][:, :, 0:nj * 128], wgv[:, :, js * 128:(js + nj) * 128], ["wg_d"], [("wgb", wbuf)], q="pool")
                        P.dma(wub[wbuf][:, :, 0:nj * 128], wuv[:, :, js * 128:(js + nj) * 128], ["wu_d"], [("wub", wbuf)], q="pool")
                        for jj in range(nj):
                            jl = js + jj - j0
                            for sbi in range(NSB):
                                xs = slice(sbi * 512, (sbi + 1) * 512)
                                par = (jj * NSB + sbi) % 2
                                gp, up = 4 + par, 6 + par
                                for kc in range(8):
                                    P.op("pe", lambda e, kc=kc, jj=jj, wbuf=wbuf, xs=xs, gp=gp: e.matmul(
                                        PS[gp][:, :], lhsT=wgb[wbuf][:, kc, jj * 128:(jj + 1) * 128], rhs=h2T[:, kc, xs], start=(kc == 0), stop=(kc == 7)),
                                        [("wgb", wbuf), ("h2T", sbi)], [psr(gp)])
                                for kc in range(8):
                                    P.op("pe", lambda e, kc=kc, jj=jj, wbuf=wbuf, xs=xs, up=up: e.matmul(
                                        PS[up][:, :], lhsT=wub[wbuf][:, kc, jj * 128:(jj + 1) * 128], rhs=h2T[:, kc, xs], start=(kc == 0), stop=(kc == 7)),
                                        [("wub", wbuf), ("h2T", sbi)], [psr(up)])
                                P.op("act", lambda e, par=par, gp=gp: e.activation(out=sg[par][:], in_=PS[gp][:, :], func=AF.Silu), [psr(gp)], [("sg", par)])
                                P.op("dve", lambda e, par=par, up=up, jl=jl, xs=xs: e.tensor_tensor(out=AT[:, jl, xs], in0=PS[up][:, :], in1=sg[par][:], op=ALU.mult),
                                     [psr(up), ("sg", par)], [("AT", sbi)])
                    for dh in range(2):
                        wdbuf = gcount[1] % 2
                        gcount[1] += 1
                        P.dma(wdb[wdbuf][:, 0:njp, :], wdv[:, j0:j0 + njp, dh * 512:(dh + 1) * 512], ["wd_d"], [("wdb", wdbuf)], q="pool")
                        for cc in range(4):
                            c = dh * 4 + cc
                            for sbi in range(NSB):
                                xs = slice(sbi * 512, (sbi + 1) * 512)
                                par = (cc * NSB + sbi) % 2
                                dp = par
                                for jl in range(njp):
                                    P.op("pe", lambda e, jl=jl, wdbuf=wdbuf, xs=xs, dp=dp, cc=cc, njp=njp: e.matmul(
                                        PS[dp][:, :], lhsT=wdb[wdbuf][:, jl, cc * 128:(cc + 1) * 128], rhs=AT[:, jl, xs], start=(jl == 0), stop=(jl == njp - 1)),
                                        [("wdb", wdbuf), ("AT", sbi)], [psr(dp)])
                                if not moe:
                                    P.op("dve", lambda e, c=c, xs=xs, dp=dp: e.scalar_tensor_tensor(
                                        out=x1[:, c, xs], in0=PS[dp][:, :], scalar=mod[:, l, 40 + c:41 + c], in1=x1[:, c, xs], op0=ALU.mult, op1=ALU.add),
                                        [psr(dp), "mod", ("x1", sbi)], [("x1", sbi)])
                                else:
                                    if pas == 0 and dh == 0 and cc == 0:
                                        P.op("pe", lambda e, xs=xs, sbi=sbi, ex=ex: e.matmul(
                                            PS[2 + sbi % 2][:, :], lhsT=cst[0:8, C_SEL + ex * 128:C_SEL + (ex + 1) * 128], rhs=gT[:, xs], start=True, stop=True),
                                            ["cst", ("gT", sbi)], [psr(2 + sbi % 2)])
                                        P.op("act", lambda e, sbi=sbi: e.activation(out=Gb[sbi][:], in_=PS[2 + sbi % 2][:, :], func=AF.Copy),
                                             [psr(2 + sbi % 2)], [("Gb", sbi)])
                                    P.op("dve", lambda e, c=c, dp=dp, sbi=sbi, par=par: e.scalar_tensor_tensor(
                                        out=ytmp[par][:], in0=PS[dp][:, :], scalar=mod[:, l, 40 + c:41 + c], in1=Gb[sbi][:], op0=ALU.mult, op1=ALU.mult),
                                        [psr(dp), "mod", ("Gb", sbi)], [("ytmp", par)])
                                    P.op("dve", lambda e, c=c, xs=xs, par=par: e.tensor_tensor(out=x1[:, c, xs], in0=x1[:, c, xs], in1=ytmp[par][:], op=ALU.add),
                                         [("ytmp", par), ("x1", sbi)], [("x1", sbi)])
            for sbi in range(NSB):
                t0 = tsb * TS + sbi * 512
                xs = slice(sbi * 512, (sbi + 1) * 512)
                P.dma(xdv[:, :, t0:t0 + 512], x1[:, :, xs], [("x1", sbi)], [(xres_out, sbi % 2)])
        P.barrier()
        P.pop()

    for l in range(nlayers):
        if do_layer(l):
            break

    P.emit(final_waits=[("yT", 0), ("yT", 1)])
    return nc


def host_inputs(inp, S):
    NT = S // 128
    B = inp["x"].shape[0]
    f = lambda a: np.ascontiguousarray(np.asarray(a, dtype=np.float32))
    consts = make_consts()
    L = inp["w_in"].shape[0]
    vec = np.zeros((L, 128, 64), np.float32)
    gains = np.zeros((L, 128, NGAIN), np.float32)
    ckb = np.zeros((L, 128, 20, 128), np.float32)
    jj = np.arange(128)[:, None]
    ii = np.arange(128)[None, :]
    for l in range(L):
        vec[l, :, 0:48] = np.asarray(inp["ada_b"][l]).reshape(48, 128).T
        vec[l, :, 48:56] = np.asarray(inp["norm_mix"][l]).reshape(8, 128).T
        vec[l, :, 56:64] = np.asarray(inp["norm_ffn"][l]).reshape(8, 128).T
        g = np.concatenate([np.asarray(inp[k][l]) for k in ("mla_q_norm", "mla_kv_norm", "mla_q_qknorm", "mla_k_qknorm",
                                                            "ck_q_qknorm", "ck_k_qknorm", "group_out_norm")])
        gains[l] = np.broadcast_to(g[None, :], (128, NGAIN))
        rb = np.asarray(inp["ck_rel_bias"][l])
        for h in range(4):
            for o in range(5):
                idx = np.clip(128 * (4 - o) + ii - jj, -128, 128) + 128
                ckb[l, :, h * 5 + o, :] = rb[h][idx]
    shared = dict(consts=consts, vec=vec, gains=gains, ckbias=ckb)
    for k in ("ada_w", "w_in", "w_q_up", "w_kv_up", "w_out", "ffn_w_gate", "ffn_w_up", "ffn_w_down", "moe_router",
              "moe_w_gate", "moe_w_up", "moe_w_down"):
        shared[k] = f(inp[k])
    maps = []
    for b in range(B):
        m = dict(shared)
        m["xT"] = np.ascontiguousarray(np.asarray(inp["x"][b], np.float32).T)
        m["cT"] = np.ascontiguousarray(np.asarray(inp["c"][b], np.float32).reshape(8, 128).T)
        m["pos"] = np.ascontiguousarray(np.asarray(inp["positions"][b], np.int32).reshape(NT, 128).T)
        maps.append(m)
    return maps


_NC_CACHE = {}


def kernel(**inputs):
    S = inputs["x"].shape[1]
    B = inputs["x"].shape[0]
    key = (S,)
    if key not in _NC_CACHE:
        _NC_CACHE[key] = build(S)
    nc = _NC_CACHE[key]
    maps = host_inputs(inputs, S)
    res = run_bass_kernel_spmd(nc, maps, core_ids=list(range(B)))
    out = np.stack([np.ascontiguousarray(res.results[b]["yT"].T) for b in range(B)], axis=0)
    return out.astype(np.float32)
```

```python
import math
import numpy as np
from contextlib import ExitStack
import concourse.bass as bass
import concourse.mybir as mybir
from concourse.bass_utils import run_bass_kernel_spmd

F32 = mybir.dt.float32
BF16 = mybir.dt.bfloat16
I32 = mybir.dt.int32
AF = mybir.ActivationFunctionType
ALU = mybir.AluOpType
AX = mybir.AxisListType

D = 1024
DFF = 2816
NJ = DFF // 128
NE = 8
EPS = 1e-6
NEG = -30000.0
COMPUTE = ("pe", "act", "dve", "pool")


class Prog:
    def __init__(self, nc):
        self.nc = nc
        self.stacks = [ExitStack()]
        self.ins = {e: [] for e in COMPUTE + ("sp",)}
        self.res_w = {}
        self.res_r = {}
        self.dma_cnt = {}
        self.sems = {}
        self.ntile = 0
        self.pending = {}
        self.local = None
        self.capture = None

    def replay_interleaved(self, lists):
        self.local = None
        pos = [0] * len(lists)
        tot = max(len(x) for x in lists) if lists else 0
        for step in range(tot):
            for li, lst in enumerate(lists):
                upto = ((step + 1) * len(lst) + tot - 1) // tot
                while pos[li] < min(upto, len(lst)):
                    rec = lst[pos[li]]
                    pos[li] += 1
                    if rec[0] == "op":
                        self.op(rec[1], rec[2], rec[3], rec[4])
                    else:
                        self.dma(rec[1], rec[2], rec[3], rec[4], q=rec[5])

    def _rn(self, rs):
        if self.local is None:
            return list(rs)
        names, sfx = self.local
        out = []
        for r in rs:
            base = r[0] if isinstance(r, tuple) else r
            out.append((r, sfx) if base in names else r)
        return out

    def push(self):
        self.stacks.append(ExitStack())

    def pop(self):
        self.stacks.pop().close()

    def sb(self, shape, dt, name=None):
        self.ntile += 1
        return self.stacks[-1].enter_context(self.nc.sbuf_tensor(f"t{self.ntile}", list(shape), dt))

    def ps(self, shape, dt=F32):
        self.ntile += 1
        return self.stacks[0].enter_context(self.nc.psum_tensor(f"p{self.ntile}", list(shape), dt))

    def _deps(self, eng, reads, writes):
        deps = {}

        def add(d):
            for k, v in d.items():
                if deps.get(k, -1) < v:
                    deps[k] = v
        for r in reads:
            add(self.res_w.get(r, {}))
        for w in writes:
            add(self.res_w.get(w, {}))
            add(self.res_r.get(w, {}))
        if eng in self.pending:
            add(self.pending.pop(eng))
        return deps

    def barrier(self, engines=COMPUTE + ("sp",)):
        d = {}
        for e in COMPUTE:
            if self.ins[e]:
                for i in range(len(self.ins[e]) - 1, -1, -1):
                    if self.ins[e][i]["kind"] == "op":
                        d[("E", e)] = i
                        break
        for res, cnt in self.dma_cnt.items():
            d[("D", res)] = cnt
        for e in engines:
            cur = self.pending.setdefault(e, {})
            for k, v in d.items():
                if cur.get(k, -1) < v:
                    cur[k] = v

    def op(self, eng, fn, reads=(), writes=()):
        reads = self._rn(reads)
        writes = self._rn(writes)
        if self.capture is not None:
            self.capture.append(("op", eng, fn, reads, writes))
            return None
        lst = self.ins[eng]
        idx = len(lst)
        deps = self._deps(eng, reads, writes)
        if eng == "pe":
            deps.pop(("E", "pe"), None)
        rec = dict(kind="op", fn=fn, deps=deps, signal=False)
        lst.append(rec)
        key = ("E", eng)
        for r in reads:
            self.res_r.setdefault(r, {})[key] = idx
        for w in writes:
            self.res_w[w] = {key: idx}
            self.res_r[w] = {}
        return rec

    def dma(self, out_ap, in_ap, reads, writes, q="sp"):
        reads = self._rn(reads)
        writes = self._rn(writes)
        if self.capture is not None:
            self.capture.append(("dma", out_ap, in_ap, reads, writes, q))
            return None
        lst = self.ins[q]
        dst = writes[0]
        deps = self._deps(q, reads, writes)
        skey = ("D", dst)
        deps.pop(skey, None)
        cnt = self.dma_cnt.get(dst, 0) + 16
        self.dma_cnt[dst] = cnt
        rec = dict(kind="dma", out=out_ap, in_=in_ap, deps=deps, skey=skey)
        lst.append(rec)
        for r in reads:
            self.res_r.setdefault(r, {})[skey] = cnt
        for w in writes:
            d = self.res_w.setdefault(w, {})
            if set(d.keys()) - {skey}:
                d = {}
                self.res_w[w] = d
            d[skey] = cnt
            self.res_r[w] = {}
        return rec

    def emit(self, final_waits=()):
        nc = self.nc
        es = self.stacks[0]
        for e in self.ins:
            for rec in self.ins[e]:
                for (kind, k), v in rec["deps"].items():
                    if kind == "E":
                        self.ins[k][v]["signal"] = True
        for e in COMPUTE:
            c = 0
            for rec in self.ins[e]:
                if rec.get("signal"):
                    c += 1
                    rec["sigval"] = c
        keys = [("E", e) for e in COMPUTE] + sorted({rec["skey"] for e in self.ins for rec in self.ins[e]
                                                     if rec["kind"] == "dma"}, key=str)
        for i, k in enumerate(keys):
            self.sems[k] = es.enter_context(nc.semaphore(f"s{i}"))
        final = dict(self.dma_cnt)
        with nc.Block() as block:
            def mk(ename, lists):
                def body(eng):
                    waited = {}
                    for rec in lists:
                        for (kind, k), v in rec["deps"].items():
                            val = self.ins[k][v]["sigval"] if kind == "E" else v
                            key = (kind, k)
                            if waited.get(key, -1) >= val:
                                continue
                            waited[key] = val
                            eng.wait_ge(self.sems[key], val)
                        if rec["kind"] == "op":
                            inst = rec["fn"](eng)
                            if rec["signal"]:
                                inst.then_inc(self.sems[("E", ename)], 1)
                        else:
                            inst = eng.dma_start(out=rec["out"], in_=rec["in_"])
                            inst.then_inc(self.sems[rec["skey"]], 16)
                    if ename == "sp":
                        for res in final_waits:
                            if ("D", res) in self.sems:
                                eng.wait_ge(self.sems[("D", res)], final[res])
                return body
            block.tensor(mk("pe", self.ins["pe"]))
            block.scalar(mk("act", self.ins["act"]))
            block.vector(mk("dve", self.ins["dve"]))
            block.gpsimd(mk("pool", self.ins["pool"]))
            block.sync(mk("sp", self.ins["sp"]))
        while self.stacks:
            self.stacks.pop().close()


C_ID, C_NUT, C_STRICT, C_NMA, C_MB, C_CK0, C_CK4, C_SEL = [i * 128 for i in range(8)]
NCONST = 7 * 128 + 8 * 128
INV_FREQ = (np.float32(10000.0) ** (-np.arange(0, 32, 2, dtype=np.float32) / np.float32(32))).astype(np.float32)


def make_consts():
    j = np.arange(128)[:, None]
    i = np.arange(128)[None, :]
    c = np.zeros((128, NCONST), np.float32)
    c[:, C_ID:C_ID + 128] = (j == i)
    c[:, C_NUT:C_NUT + 128] = -(j >= i).astype(np.float32)
    c[:, C_STRICT:C_STRICT + 128] = (j < i)
    c[:, C_NMA:C_NMA + 128] = np.where(j >= i, NEG, 0.0)
    c[:, C_MB:C_MB + 128] = np.where((j >= 64) & (i < 64), NEG, 0.0)
    c[:, C_CK0:C_CK0 + 128] = np.where((i >= 64) & (j < 64), NEG, 0.0)
    c[:, C_CK4:C_CK4 + 128] = np.where((i < 64) & (j >= 64), NEG, 0.0)
    for e in range(8):
        c[e, C_SEL + e * 128:C_SEL + (e + 1) * 128] = 1.0
    return c


G_QN, G_KVN, G_QQK, G_KQK, G_CQ, G_CK, G_OUT = 0, 256, 384, 480, 576, 640, 704
NGAIN = 704 + 1024


def build(S, nlayers=2, debug=False, TS=1024, stop_after=None):
    NT = S // 128
    NB = S // 512
    TS = min(TS, S)
    nc = bass.Bass("TRN2", target_bir_lowering=False)
    P = Prog(nc)

    def din(name, shape, dt=F32):
        return nc.dram_tensor(name, list(shape), dt, kind="ExternalInput").ap()

    def dscr(name, shape, dt):
        return nc.dram_tensor(name, list(shape), dt, kind=("ExternalOutput" if debug else "Internal")).ap()

    xT_in = din("xT", [D, S])
    cT_d = din("cT", [128, 8])
    pos_d = din("pos", [128, NT], I32)
    consts_d = din("consts", [128, NCONST])
    vec_d = din("vec", [2, 128, 64])
    gains_d = din("gains", [2, 128, NGAIN])
    ckb_d = din("ckbias", [2, 128, 20, 128])
    ada_w = din("ada_w", [2, D, 6 * D])
    w_in = din("w_in", [2, D, 1952])
    w_q_up = din("w_q_up", [2, 256, 768])
    w_kv_up = din("w_kv_up", [2, 128, 1024])
    w_out = din("w_out", [2, D, D])
    ffn_g = din("ffn_w_gate", [1, D, DFF])
    ffn_u = din("ffn_w_up", [1, D, DFF])
    ffn_d = din("ffn_w_down", [1, DFF, D])
    router = din("moe_router", [1, D, NE])
    moe_g = din("moe_w_gate", [1, NE, D, DFF])
    moe_u = din("moe_w_up", [1, NE, D, DFF])
    moe_d = din("moe_w_down", [1, NE, DFF, D])
    yT = nc.dram_tensor("yT", [D, S], F32, kind="ExternalOutput").ap()

    xT_mid = dscr("xT_mid", [D, S], F32)
    sbQZ = dscr("sbQZ", [4, 128, S], BF16)
    sbKT = dscr("sbKT", [2, 128, S], BF16)
    sbV = dscr("sbV", [S, 256], BF16)
    mlQT = dscr("mlQT", [8, 96, S], BF16)
    mlKT = dscr("mlKT", [8, 96, S], BF16)
    mlV = dscr("mlV", [S, 512], BF16)
    ckQZ = dscr("ckQZ", [4, 128, S], BF16)
    ckKT = dscr("ckKT", [2, 128, S], BF16)
    ckV = dscr("ckV", [S, 256], BF16)
    mgT = dscr("mgT", [D, S], BF16)
    rope_d = dscr("rope_d", [2, 128, NT, 16], F32)
    dbgG = dscr("dbgG", [8, S], F32) if debug else None

    cst = P.sb([128, NCONST], F32)
    cbf = P.sb([128, 5 * 128], BF16)
    ones_bf = P.sb([128, 128], BF16)
    negones_bf = P.sb([128, 128], BF16)
    negrow = P.sb([1, 128], F32)
    zeros_bf = P.sb([128, 512], BF16)
    negid_bf = P.sb([128, 128], BF16)
    cact = P.sb([128, 8], F32)
    vec = P.sb([128, 2, 64], F32)
    mod = P.sb([128, 2, 48], F32)
    A1 = P.sb([128, 2, 8], F32)
    A2 = P.sb([128, 2, 8], F32)
    PS = [P.ps([128, 512], F32) for _ in range(8)]

    def psr(k):
        return ("ps", k)

    ident_bf = cbf[:, 0:128]
    negut_bf = cbf[:, 128:256]
    strict_bf = cbf[:, 256:384]
    nma_bf = cbf[:, 384:512]
    mb_bf = cbf[:, 512:640]
    ident_f = cst[:, C_ID:C_ID + 128]

    P.dma(cst[:], consts_d[:, :], ["consts_d"], ["cst"])
    P.dma(cact[:], cT_d[:, :], ["cT_d"], ["cact"])
    P.dma(vec[:], vec_d.rearrange("l p c -> p l c"), ["vec_d"], ["vec"])
    P.op("dve", lambda e: e.tensor_copy(out=cbf[:], in_=cst[:, 0:640]), ["cst"], ["cbf"])
    P.op("pool", lambda e: e.memset(ones_bf[:], 1.0), [], ["ones_bf"])
    P.op("pool", lambda e: e.memset(negones_bf[:], -1.0), [], ["negones_bf"])
    P.op("pool", lambda e: e.memset(negrow[:], -1.0), [], ["negrow"])
    P.op("pool", lambda e: e.memset(zeros_bf[:], 0.0), [], ["zeros_bf"])
    P.op("dve", lambda e: e.tensor_scalar(out=negid_bf[:], in0=cst[:, C_ID:C_ID + 128], scalar1=-1.0, scalar2=None, op0=ALU.mult), ["cst"], ["negid_bf"])
    P.op("act", lambda e: e.activation(out=cact[:], in_=cact[:], func=AF.Silu), ["cact"], ["cact"])
    for zt, zres in ((sbQZ, "sbQZ"), (ckQZ, "ckQZ")):
        for h in range(4):
            lo = 64 if h % 2 == 0 else 0
            for tbz in range(NB):
                P.dma(zt[h, lo:lo + 64, tbz * 512:(tbz + 1) * 512], zeros_bf[lo:lo + 64, :], ["zeros_bf"], [zres])

    P.push()
    awb = [P.sb([128, 8, 512], F32) for _ in range(2)]
    for l in range(nlayers):
        awv = ada_w[l].rearrange("(kc p) n -> p kc n", p=128)
        for g in range(12):
            b = g % 2
            P.dma(awb[b][:], awv[:, :, g * 512:(g + 1) * 512], ["ada_w"], [("awb", b)])
            for m in range(4):
                col = g * 4 + m
                for kc in range(8):
                    P.op("pe", lambda e, b=b, m=m, kc=kc, col=col: e.matmul(
                        PS[0][:, col:col + 1], lhsT=awb[b][:, kc, m * 128:(m + 1) * 128], rhs=cact[:, kc:kc + 1],
                        start=(kc == 0), stop=(kc == 7)), [("awb", b), "cact"], [psr(0)])
        P.op("dve", lambda e, l=l: e.tensor_tensor(out=mod[:, l, :], in0=PS[0][:, 0:48], in1=vec[:, l, 0:48], op=ALU.add),
             [psr(0), "vec"], ["mod"])
        P.op("dve", lambda e, l=l: e.scalar_tensor_tensor(out=A1[:, l, :], in0=mod[:, l, 8:16], scalar=1.0, in1=vec[:, l, 48:56],
                                                          op0=ALU.add, op1=ALU.mult), ["mod", "vec"], ["A1"])
        P.op("dve", lambda e, l=l: e.scalar_tensor_tensor(out=A2[:, l, :], in0=mod[:, l, 32:40], scalar=1.0, in1=vec[:, l, 56:64],
                                                          op0=ALU.add, op1=ALU.mult), ["mod", "vec"], ["A2"])

    cos_t = P.sb([128, NT, 16], F32)
    sin_t = P.sb([128, NT, 16], F32)
    posi = P.sb([128, NT], I32)
    posf = P.sb([128, NT], F32)
    ang = P.sb([128, NT, 16], F32)
    u = P.sb([128, NT, 16], F32)
    ki = P.sb([128, NT, 16], I32)
    kf = P.sb([128, NT, 16], F32)
    r = P.sb([128, NT, 16], F32)
    fx = P.sb([128, NT, 16], F32)
    P.dma(posi[:], pos_d[:, :], ["pos_d"], ["posi"])
    P.op("dve", lambda e: e.tensor_copy(out=posf[:], in_=posi[:]), ["posi"], ["posf"])
    for i in range(16):
        P.op("dve", lambda e, i=i: e.tensor_scalar(out=ang[:, :, i], in0=posf[:], scalar1=float(INV_FREQ[i]), scalar2=None,
                                                   op0=ALU.mult), ["posf"], ["ang"])
    TWO_PI = 2.0 * math.pi
    C1 = 6.28125
    C2 = TWO_PI - C1
    for which, dst in ((0, sin_t), (1, cos_t)):
        src = ang
        if which == 1:
            P.op("dve", lambda e: e.tensor_scalar(out=r[:], in0=ang[:], scalar1=math.pi / 2, scalar2=None, op0=ALU.add),
                 ["ang"], ["r"])
            src = r
        P.op("dve", lambda e, src=src: e.tensor_scalar(out=u[:], in0=src[:], scalar1=1.0 / TWO_PI, scalar2=None, op0=ALU.mult),
             ["ang", "r"], ["u"])
        P.op("dve", lambda e: e.tensor_copy(out=ki[:], in_=u[:]), ["u"], ["ki"])
        P.op("dve", lambda e: e.tensor_copy(out=kf[:], in_=ki[:]), ["ki"], ["kf"])
        P.op("dve", lambda e, src=src: e.scalar_tensor_tensor(out=u[:], in0=kf[:], scalar=-C1, in1=src[:], op0=ALU.mult, op1=ALU.add),
             ["kf", "ang", "r"], ["u"])
        P.op("dve", lambda e: e.scalar_tensor_tensor(out=r[:], in0=kf[:], scalar=-C2, in1=u[:], op0=ALU.mult, op1=ALU.add),
             ["kf", "u"], ["r"])
        P.op("dve", lambda e: e.tensor_scalar(out=fx[:], in0=r[:], scalar1=math.pi, scalar2=-TWO_PI, op0=ALU.is_gt, op1=ALU.mult),
             ["r"], ["fx"])
        P.op("dve", lambda e: e.tensor_tensor(out=r[:], in0=r[:], in1=fx[:], op=ALU.add), ["r", "fx"], ["r"])
        P.op("dve", lambda e: e.tensor_scalar(out=fx[:], in0=r[:], scalar1=-math.pi, scalar2=TWO_PI, op0=ALU.is_lt, op1=ALU.mult),
             ["r"], ["fx"])
        P.op("dve", lambda e: e.tensor_tensor(out=r[:], in0=r[:], in1=fx[:], op=ALU.add), ["r", "fx"], ["r"])
        P.op("act", lambda e, dst=dst: e.activation(out=dst[:], in_=r[:], func=AF.Sin), ["r"], ["rope_t0"])
    P.dma(rope_d[0], cos_t[:], ["rope_t0"], ["rope_d"])
    P.dma(rope_d[1], sin_t[:], ["rope_t0"], ["rope_d"])
    P.barrier()
    P.pop()

    def do_layer(l):
        x_src = xT_in if l == 0 else xT_mid
        x_dst = yT if l == nlayers - 1 else xT_mid
        XIN = ["xT_in"] if l == 0 else [("xT_mid", 0), ("xT_mid", 1)]
        xres_out = "yT" if l == nlayers - 1 else "xT_mid"
        xsv = x_src.rearrange("(c p) s -> p c s", p=128)
        xdv = x_dst.rearrange("(c p) s -> p c s", p=128)

        P.push()
        cos_t = P.sb([128, NT, 16], F32)
        sin_t = P.sb([128, NT, 16], F32)
        gains = P.sb([128, NGAIN], F32)
        ckbm = P.sb([128, 20, 128], F32)
        P.dma(cos_t[:], rope_d[0], ["rope_d"], ["rope_t"])
        P.dma(sin_t[:], rope_d[1], ["rope_d"], ["rope_t"])
        P.dma(gains[:], gains_d[l], ["gains_d"], ["gains"])
        P.dma(ckbm[:], ckb_d[l], ["ckb_d"], ["ckbm"])
        for h in range(4):
            P.op("dve", lambda e, h=h: e.tensor_tensor(out=ckbm[:, h * 5 + 0, :], in0=ckbm[:, h * 5 + 0, :],
                                                       in1=cst[:, C_CK0:C_CK0 + 128], op=ALU.add), ["ckbm", "cst"], ["ckbm"])
            P.op("dve", lambda e, h=h: e.tensor_tensor(out=ckbm[:, h * 5 + 4, :], in0=ckbm[:, h * 5 + 4, :],
                                                       in1=cst[:, C_CK4:C_CK4 + 128], op=ALU.add), ["ckbm", "cst"], ["ckbm"])

        P.push()
        win_bf = P.sb([128, 8, 1952], BF16)
        wq_bf = P.sb([128, 2, 768], BF16)
        wkv_bf = P.sb([128, 1024], BF16)
        wiv = w_in[l].rearrange("(kc p) n -> p kc n", p=128)
        for kc in range(8):
            P.dma(win_bf[:, kc, :], wiv[:, kc, :], ["w_in"], ["win_bf"], q="pool")
        P.dma(wq_bf[:], w_q_up[l].rearrange("(kc p) n -> p kc n", p=128), ["w_q_up"], ["wq_bf"], q="pool")
        P.dma(wkv_bf[:], w_kv_up[l], ["w_kv_up"], ["wkv_bf"], q="pool")

        xb = P.sb([128, 8, 512], F32)
        sq = P.sb([128, 8, 512], BF16)
        rstd = P.sb([128, 512], F32)
        hT = P.sb([128, 8, 512], BF16)
        fmo = [P.sb([128, 512], BF16) for _ in range(4)]
        LOCAL = {"proj", "vst", "junk", "ss", "ss2", "lat", "latT", "qf", "kvf", "kf32", "kper", "t16", "ssh", "sqj", "qn", "kn",
                 "cqn", "ckn", "trq", "trk", "trc", "mlv"}

        def alloc_tile_set():
            return dict(
                proj=P.sb([128, 1440], F32), vst=[P.sb([128, 256], BF16) for _ in range(2)], junk=P.sb([128, 256], F32),
                ss=P.sb([128, 4], F32), lat=P.sb([128, 384], BF16), latT=P.sb([128, 3, 128], BF16), qf=P.sb([128, 8, 96], F32),
                kvf=P.sb([128, 8, 128], F32), kf32=P.sb([128, 8, 96], F32), kper=P.sb([128, 32], F32),
                t16=[P.sb([128, 8, 16], F32) for _ in range(4)], ssh=P.sb([128, 8], F32), sqj=P.sb([128, 8, 96], F32),
                qn=P.sb([128, 8, 96], BF16), kn=P.sb([128, 8, 96], BF16), cqn=P.sb([128, 4, 64], BF16), ckn=P.sb([128, 4, 64], BF16),
                trq=P.sb([128, 1024], BF16), trk=P.sb([128, 1024], BF16), trc=P.sb([128, 512], BF16), mlv=P.sb([128, 8, 64], BF16))
        tsets = [alloc_tile_set() for _ in range(2)]

        def tt_s1(tb, i, proj, vst, junk, ss, lat, latT, qf, kvf, kf32, kper, t16, ssh, sqj, qn, kn, cqn, ckn, trq, trk, trc, mlv):
            j = tb * 4 + i
            tk = slice(i * 128, (i + 1) * 128)
            rows = slice(j * 128, (j + 1) * 128)
            for gi, (c0, c1, pb) in enumerate(((512, 1024, 3), (1024, 1440, 4), (1440, 1952, 5))):
                for kc in range(8):
                    P.op("pe", lambda e, kc=kc, c0=c0, c1=c1, pb=pb, tk=tk: e.matmul(
                        PS[pb][:, 0:c1 - c0], lhsT=hT[:, kc, tk], rhs=win_bf[:, kc, c0:c1], start=(kc == 0), stop=(kc == 7)),
                        ["hT", "win_bf"], [psr(pb)])
            P.op("act", lambda e: e.activation(out=proj[:, 0:512], in_=PS[3][:, :], func=AF.Copy), [psr(3)], [("proj", 0)])
            P.op("dve", lambda e: e.tensor_copy(out=proj[:, 512:928], in_=PS[4][:, 0:416]), [psr(4)], [("proj", 1)])
            P.op("act", lambda e: e.activation(out=proj[:, 928:1440], in_=PS[5][:, :], func=AF.Copy), [psr(5)], [("proj", 2)])
            P.op("pool", lambda e: e.tensor_copy(out=vst[0][:], in_=proj[:, 0:256]), [("proj", 0)], [("vst", 0)])
            P.dma(sbV[rows, :], vst[0][:], [("vst", 0)], ["sbV"])
            P.op("pool", lambda e: e.tensor_copy(out=vst[1][:], in_=proj[:, 1184:1440]), [("proj", 2)], [("vst", 1)])
            P.dma(ckV[rows, :], vst[1][:], [("vst", 1)], ["ckV"])
            P.op("act", lambda e: e.activation(out=junk[:, 0:256], in_=proj[:, 256:512], func=AF.Square, accum_out=ss[:, 0:1]),
                 [("proj", 0)], ["junk", "ss"])
            P.op("act", lambda e: e.activation(out=junk[:, 0:128], in_=proj[:, 512:640], func=AF.Square, accum_out=ss[:, 1:2]),
                 [("proj", 1), "ss"], ["junk", "ss"])
            P.op("act", lambda e: e.activation(out=ss[:, 2:3], in_=ss[:, 0:1], func=AF.Sqrt, scale=1.0 / 256, bias=EPS), ["ss"], ["ss"])
            P.op("act", lambda e: e.activation(out=ss[:, 3:4], in_=ss[:, 1:2], func=AF.Sqrt, scale=1.0 / 128, bias=EPS), ["ss"], ["ss"])
            P.op("dve", lambda e: e.reciprocal(out=ss[:, 2:4], in_=ss[:, 2:4]), ["ss"], ["ss"])
            P.op("dve", lambda e: e.scalar_tensor_tensor(out=lat[:, 0:256], in0=proj[:, 256:512], scalar=ss[:, 2:3],
                                                         in1=gains[:, G_QN:G_QN + 256], op0=ALU.mult, op1=ALU.mult),
                 [("proj", 0), "ss", "gains"], ["lat"])
            P.op("dve", lambda e: e.scalar_tensor_tensor(out=lat[:, 256:384], in0=proj[:, 512:640], scalar=ss[:, 3:4],
                                                         in1=gains[:, G_KVN:G_KVN + 128], op0=ALU.mult, op1=ALU.mult),
                 [("proj", 1), "ss", "gains"], ["lat"])
            pbt = PS[2][:].bitcast(BF16)
            for k in range(3):
                P.op("pe", lambda e, k=k, pbt=pbt: e.transpose(pbt[:, k * 128:(k + 1) * 128], lat[:, k * 128:(k + 1) * 128], ident_bf),
                     ["lat", "cbf"], [psr(2)])
            P.op("dve", lambda e, pbt=pbt: e.tensor_copy(out=latT[:], in_=pbt[:, 0:384]), [psr(2)], ["latT"])
            for half, pb in ((0, 3), (1, 4)):
                for kc in range(2):
                    P.op("pe", lambda e, half=half, pb=pb, kc=kc: e.matmul(
                        PS[pb][:, 0:384], lhsT=latT[:, kc, :], rhs=wq_bf[:, kc, half * 384:(half + 1) * 384],
                        start=(kc == 0), stop=(kc == 1)), ["latT", "wq_bf"], [psr(pb)])
            for half, pb in ((0, 5), (1, 7)):
                P.op("pe", lambda e, half=half, pb=pb: e.matmul(
                    PS[pb][:, :], lhsT=latT[:, 2, :], rhs=wkv_bf[:, half * 512:(half + 1) * 512], start=True, stop=True),
                    ["latT", "wkv_bf"], [psr(pb)])
            P.op("act", lambda e: e.activation(out=qf[:, 0:4, :], in_=PS[3][:, 0:384], func=AF.Copy), [psr(3)], [("qf", 0)])
            P.op("act", lambda e: e.activation(out=qf[:, 4:8, :], in_=PS[4][:, 0:384], func=AF.Copy), [psr(4)], [("qf", 1)])
            P.op("dve", lambda e: e.tensor_copy(out=kvf[:, 0:4, :], in_=PS[5][:, :]), [psr(5)], [("kvf", 0)])
            P.op("dve", lambda e: e.tensor_copy(out=kvf[:, 4:8, :], in_=PS[7][:, :]), [psr(7)], [("kvf", 1)])

        def tt_s2(tb, i, proj, vst, junk, ss, lat, latT, qf, kvf, kf32, kper, t16, ssh, sqj, qn, kn, cqn, ckn, trq, trk, trc, mlv):
            j = tb * 4 + i
            rows = slice(j * 128, (j + 1) * 128)
            pbt = PS[6][:].bitcast(BF16)
            cosb = cos_t[:, j, :]
            sinb = sin_t[:, j, :]
            cos8 = cosb.unsqueeze(1).broadcast_to([128, 8, 16])
            sin8 = sinb.unsqueeze(1).broadcast_to([128, 8, 16])
            QF = [("qf", 0), ("qf", 1)]
            P.op("pool", lambda e, cos8=cos8: e.tensor_tensor(out=t16[0][:], in0=qf[:, :, 64:80], in1=cos8, op=ALU.mult), QF + ["rope_t"], [("t16", 0)])
            P.op("pool", lambda e, sin8=sin8: e.tensor_tensor(out=t16[1][:], in0=qf[:, :, 80:96], in1=sin8, op=ALU.mult), QF + ["rope_t"], [("t16", 1)])
            P.op("pool", lambda e, cos8=cos8: e.tensor_tensor(out=t16[2][:], in0=qf[:, :, 80:96], in1=cos8, op=ALU.mult), QF + ["rope_t"], [("t16", 2)])
            P.op("pool", lambda e, sin8=sin8: e.tensor_tensor(out=t16[3][:], in0=qf[:, :, 64:80], in1=sin8, op=ALU.mult), QF + ["rope_t"], [("t16", 3)])
            P.op("dve", lambda e: e.tensor_tensor(out=qf[:, :, 64:80], in0=t16[0][:], in1=t16[1][:], op=ALU.subtract),
                 [("t16", 0), ("t16", 1)], QF)
            P.op("dve", lambda e: e.tensor_tensor(out=qf[:, :, 80:96], in0=t16[2][:], in1=t16[3][:], op=ALU.add),
                 [("t16", 2), ("t16", 3)], QF)
            P.op("dve", lambda e, cosb=cosb: e.tensor_tensor(out=t16[0][:, 0, :], in0=proj[:, 640:656], in1=cosb, op=ALU.mult), [("proj", 1), "rope_t"], [("t16", 0)])
            P.op("dve", lambda e, sinb=sinb: e.tensor_tensor(out=t16[1][:, 0, :], in0=proj[:, 656:672], in1=sinb, op=ALU.mult), [("proj", 1), "rope_t"], [("t16", 1)])
            P.op("dve", lambda e, cosb=cosb: e.tensor_tensor(out=t16[2][:, 0, :], in0=proj[:, 656:672], in1=cosb, op=ALU.mult), [("proj", 1), "rope_t"], [("t16", 2)])
            P.op("dve", lambda e, sinb=sinb: e.tensor_tensor(out=t16[3][:, 0, :], in0=proj[:, 640:656], in1=sinb, op=ALU.mult), [("proj", 1), "rope_t"], [("t16", 3)])
            P.op("dve", lambda e: e.tensor_tensor(out=kper[:, 0:16], in0=t16[0][:, 0, :], in1=t16[1][:, 0, :], op=ALU.subtract),
                 [("t16", 0), ("t16", 1)], ["kper"])
            P.op("dve", lambda e: e.tensor_tensor(out=kper[:, 16:32], in0=t16[2][:, 0, :], in1=t16[3][:, 0, :], op=ALU.add),
                 [("t16", 2), ("t16", 3)], ["kper"])
            KV = [("kvf", 0), ("kvf", 1)]
            P.op("pool", lambda e: e.tensor_copy(out=kf32[:, :, 0:64], in_=kvf[:, :, 0:64]), KV, ["kf32"])
            P.op("pool", lambda e: e.tensor_copy(out=kf32[:, :, 64:96], in_=kper[:].unsqueeze(1).broadcast_to([128, 8, 32])), ["kper", "kf32"], ["kf32"])
            P.op("pool", lambda e: e.tensor_copy(out=mlv[:], in_=kvf[:, :, 64:128]), KV, ["mlv"])
            P.dma(mlV[rows, :], mlv[:].rearrange("p h d -> p (h d)"), ["mlv"], ["mlV"])
            for (src, srcres, gofs, dstn, dres, sc) in ((qf, QF, G_QQK, qn, "qn", 96 ** -0.5), (kf32, ["kf32"], G_KQK, kn, "kn", 1.0)):
                P.op("pool", lambda e, src=src: e.tensor_tensor(out=sqj[:], in0=src[:], in1=src[:], op=ALU.mult), srcres, ["sqj"])
                P.op("dve", lambda e: e.tensor_reduce(out=ssh[:], in_=sqj[:], axis=AX.X, op=ALU.add), ["sqj"], ["ssh"])
                P.op("act", lambda e: e.activation(out=ssh[:], in_=ssh[:], func=AF.Sqrt, scale=1.0 / 96, bias=EPS), ["ssh"], ["ssh"])
                P.op("dve", lambda e: e.reciprocal(out=ssh[:], in_=ssh[:]), ["ssh"], ["ssh"])
                P.op("dve", lambda e, src=src: e.tensor_tensor(out=sqj[:], in0=src[:], in1=ssh[:].unsqueeze(2).broadcast_to([128, 8, 96]),
                                                               op=ALU.mult), srcres + ["ssh"], ["sqj"])
                P.op("dve", lambda e, gofs=gofs, dstn=dstn, sc=sc: e.scalar_tensor_tensor(
                    out=dstn[:], in0=sqj[:], scalar=float(sc), in1=gains[:, gofs:gofs + 96].unsqueeze(1).broadcast_to([128, 8, 96]),
                    op0=ALU.mult, op1=ALU.mult), ["sqj", "gains"], [dres])
            for (c0, pres, gofs, dstn, dres, sc) in ((672, ("proj", 1), G_CQ, cqn, "cqn", 0.125), (928, ("proj", 2), G_CK, ckn, "ckn", 1.0)):
                srcv = proj[:, c0:c0 + 256].rearrange("p (h d) -> p h d", h=4)
                sq4 = sqj[:, 0:4, 0:64]
                P.op("dve", lambda e, srcv=srcv, sq4=sq4: e.tensor_tensor(out=sq4, in0=srcv, in1=srcv, op=ALU.mult), [pres], ["sqj"])
                P.op("dve", lambda e, sq4=sq4: e.tensor_reduce(out=ssh[:, 0:4], in_=sq4, axis=AX.X, op=ALU.add), ["sqj"], ["ssh"])
                P.op("act", lambda e: e.activation(out=ssh[:, 0:4], in_=ssh[:, 0:4], func=AF.Sqrt, scale=1.0 / 64, bias=EPS), ["ssh"], ["ssh"])
                P.op("dve", lambda e: e.reciprocal(out=ssh[:, 0:4], in_=ssh[:, 0:4]), ["ssh"], ["ssh"])
                P.op("dve", lambda e, srcv=srcv, sq4=sq4: e.tensor_tensor(out=sq4, in0=srcv, in1=ssh[:, 0:4].unsqueeze(2).broadcast_to([128, 4, 64]),
                                                                          op=ALU.mult), [pres, "ssh"], ["sqj"])
                P.op("dve", lambda e, gofs=gofs, dstn=dstn, sc=sc, sq4=sq4: e.scalar_tensor_tensor(
                    out=dstn[:], in0=sq4, scalar=float(sc), in1=gains[:, gofs:gofs + 64].unsqueeze(1).broadcast_to([128, 4, 64]),
                    op0=ALU.mult, op1=ALU.mult), ["sqj", "gains"], [dres])
            for (srcn, sres, dstd, dres, trx, tres, pbk) in ((qn, "qn", mlQT, "mlQT", trq, "trq", 6), (kn, "kn", mlKT, "mlKT", trk, "trk", 0)):
                pbx = PS[pbk][:].bitcast(BF16)
                for h in range(8):
                    P.op("pe", lambda e, h=h, srcn=srcn, pbx=pbx: e.transpose(pbx[0:96, h * 128:(h + 1) * 128], srcn[:, h, :], ident_bf),
                         [sres, "cbf"], [psr(pbk)])
                P.op("act", lambda e, pbx=pbx, trx=trx: e.activation(out=trx[0:96, :], in_=pbx[0:96, :], func=AF.Copy), [psr(pbk)], [tres])
                P.dma(dstd[:, :, rows].rearrange("h d t -> d h t"), trx[0:96, :].rearrange("d (h t) -> d h t", h=8), [tres], [dres])
            for pr in range(2):
                P.op("pe", lambda e, pr=pr, pbt=pbt: e.transpose(pbt[:, pr * 128:(pr + 1) * 128], cqn[:, 2 * pr:2 * pr + 2, :].rearrange("p h d -> p (h d)"), ident_bf),
                     ["cqn", "cbf"], [psr(6)])
                P.op("pe", lambda e, pr=pr, pbt=pbt: e.transpose(pbt[:, (2 + pr) * 128:(3 + pr) * 128], ckn[:, 2 * pr:2 * pr + 2, :].rearrange("p h d -> p (h d)"), ident_bf),
                     ["ckn", "cbf"], [psr(6)])
            P.op("act", lambda e, pbt=pbt: e.activation(out=trc[:, 0:512], in_=pbt[:, 0:512], func=AF.Copy), [psr(6)], ["trc"])
            for pr in range(2):
                P.dma(ckQZ[2 * pr, 0:64, rows], trc[0:64, pr * 128:(pr + 1) * 128], ["trc"], ["ckQZ"])
                P.dma(ckQZ[2 * pr + 1, 64:128, rows], trc[64:128, pr * 128:(pr + 1) * 128], ["trc"], ["ckQZ"])
                P.dma(ckKT[pr, :, rows], trc[:, (2 + pr) * 128:(3 + pr) * 128], ["trc"], ["ckKT"])

        for tb in range(NB):
            t0 = tb * 512
            P.dma(xb[:], xsv[:, :, t0:t0 + 512], XIN, ["xb"])
            P.op("act", lambda e: e.activation(out=sq[:], in_=xb[:], func=AF.Square), ["xb"], ["sq"])
            for c in range(8):
                P.op("pe", lambda e, c=c: e.matmul(PS[0][:, :], lhsT=ones_bf[:], rhs=sq[:, c, :], start=(c == 0), stop=(c == 7)),
                     ["sq", "ones_bf"], [psr(0)])
            P.op("act", lambda e: e.activation(out=rstd[:], in_=PS[0][:, :], func=AF.Sqrt, scale=1.0 / D, bias=EPS), [psr(0)], ["rstd"])
            P.op("dve", lambda e: e.reciprocal(out=rstd[:], in_=rstd[:]), ["rstd"], ["rstd"])
            for c in range(8):
                P.op("dve", lambda e, c=c: e.scalar_tensor_tensor(out=xb[:, c, :], in0=xb[:, c, :], scalar=A1[:, l, c:c + 1], in1=rstd[:],
                                                                  op0=ALU.mult, op1=ALU.mult), ["xb", "A1", "rstd"], ["xb"])
                P.op("act", lambda e, c=c: e.activation(out=hT[:, c, :], in_=xb[:, c, :], func=AF.Identity, bias=mod[:, l, c:c + 1], scale=1.0),
                     ["xb", "mod"], ["hT"])
            for m in range(4):
                pb = 1 + (m % 2)
                for kc in range(8):
                    P.op("pe", lambda e, m=m, kc=kc, pb=pb: e.matmul(PS[pb][:, :], lhsT=win_bf[:, kc, m * 128:(m + 1) * 128], rhs=hT[:, kc, :],
                                                                    start=(kc == 0), stop=(kc == 7)), ["win_bf", "hT"], [psr(pb)])
                fb = m
                if m < 2:
                    P.op("act", lambda e, pb=pb, fb=fb: e.activation(out=fmo[fb][:], in_=PS[pb][:, :], func=AF.Copy, scale=0.125),
                         [psr(pb)], [("fmo", fb)])
                    P.dma(sbQZ[2 * m, 0:64, t0:t0 + 512], fmo[fb][0:64, :], [("fmo", fb)], ["sbQZ"])
                    P.dma(sbQZ[2 * m + 1, 64:128, t0:t0 + 512], fmo[fb][64:128, :], [("fmo", fb)], ["sbQZ"])
                else:
                    P.op("act", lambda e, pb=pb, fb=fb: e.activation(out=fmo[fb][:], in_=PS[pb][:, :], func=AF.Copy),
                         [psr(pb)], [("fmo", fb)])
                    P.dma(sbKT[m - 2, :, t0:t0 + 512], fmo[fb][:, :], [("fmo", fb)], ["sbKT"])
            for i in range(4):
                jj = tb * 4 + i
                caps = []
                P.capture = []
                P.local = (LOCAL, jj % 2)
                tt_s1(tb, i, **tsets[jj % 2])
                caps.append(P.capture)
                if i >= 1:
                    P.capture = []
                    P.local = (LOCAL, (jj - 1) % 2)
                    tt_s2(tb, i - 1, **tsets[(jj - 1) % 2])
                    caps.append(P.capture)
                P.capture = None
                P.local = None
                P.replay_interleaved(caps)
            P.local = (LOCAL, (tb * 4 + 3) % 2)
            tt_s2(tb, 3, **tsets[(tb * 4 + 3) % 2])
            P.local = None
        P.barrier()
        P.pop()
        if stop_after == ("P1", l):
            return True

        def norm_pass(o_t, Dg, gofs, row0):
            nch = Dg // 128
            onb = [P.sb([128, Dg], BF16) for _ in range(4)]
            stg = [P.sb([128, nch, 128], BF16) for _ in range(4)]
            gss = P.sb([128, 2], F32)
            gj = P.sb([128, Dg], F32)
            for m in range(NT):
                b = m % 4
                P.op("act", lambda e, m=m: e.activation(out=gj[:], in_=o_t[:, m, :], func=AF.Square, accum_out=gss[:, 0:1]), ["o_t"], ["gj", "gss"])
                P.op("act", lambda e: e.activation(out=gss[:, 1:2], in_=gss[:, 0:1], func=AF.Sqrt, scale=1.0 / Dg, bias=EPS), ["gss"], ["gss"])
                P.op("dve", lambda e: e.reciprocal(out=gss[:, 1:2], in_=gss[:, 1:2]), ["gss"], ["gss"])
                P.op("dve", lambda e, m=m, b=b: e.scalar_tensor_tensor(out=onb[b][:], in0=o_t[:, m, :], scalar=gss[:, 1:2],
                                                                       in1=gains[:, gofs:gofs + Dg], op0=ALU.mult, op1=ALU.mult),
                     ["o_t", "gss", "gains"], [("onb", b)])
                pbt = PS[7][:].bitcast(BF16)
                for k in range(nch):
                    P.op("pe", lambda e, k=k, b=b, pbt=pbt: e.transpose(pbt[:, k * 128:(k + 1) * 128], onb[b][:, k * 128:(k + 1) * 128], ident_bf),
                         [("onb", b), "cbf"], [psr(7)])
                P.op("act", lambda e, b=b, pbt=pbt: e.activation(out=stg[b][:].rearrange("p c t -> p (c t)"), in_=pbt[:, 0:nch * 128], func=AF.Copy),
                     [psr(7)], [("stg", b)])
                P.dma(mgT[row0:row0 + Dg, m * 128:(m + 1) * 128].rearrange("(c p) t -> p c t", p=128), stg[b][:], [("stg", b)], ["mgT"])

        P.push()
        QZ = P.sb([128, 4, S], BF16)
        KT = P.sb([128, 2, S], BF16)
        VA = P.sb([128, NT, 256], BF16)
        o_t = P.sb([128, NT, 256], F32)
        P.dma(QZ[:], sbQZ.rearrange("h p s -> p h s"), ["sbQZ"], ["QZ"])
        P.dma(KT[:], sbKT.rearrange("h p s -> p h s"), ["sbKT"], ["KT"])
        P.dma(VA[:], sbV.rearrange("(n p) c -> p n c", p=128), ["sbV"], ["VA"])
        Eb = [P.sb([128, 512], F32) for _ in range(2)]
        SPb = [P.sb([128, 512], BF16) for _ in range(2)]
        Wb = [P.sb([128, 512], BF16) for _ in range(2)]
        chi = [P.sb([128, 128], BF16) for _ in range(2)]
        clo = [P.sb([128, 128], BF16) for _ in range(2)]
        items = []
        for m in range(NT):
            for h in range(4):
                blocks = list(range(m, -1, -1))
                ng = (len(blocks) + 3) // 4
                for g in range(ng):
                    items.append((m, h, g, blocks[g * 4:(g + 1) * 4], g == ng - 1))
        Zp = [0, 1]
        LWp = [2, 3]
        CP = 4
        OP = [5, 6]

        def a_st1(it, i):
            m, h, g, blks, last = it
            par = i % 2
            n = len(blks)
            qs = slice(m * 128, (m + 1) * 128)
            for c, kb in enumerate(blks):
                P.op("pe", lambda e, c=c, kb=kb, h=h, par=par, qs=qs: e.matmul(
                    PS[Zp[par]][:, c * 128:(c + 1) * 128], lhsT=KT[:, h // 2, kb * 128:(kb + 1) * 128], rhs=QZ[:, h, qs], start=True, stop=True),
                    ["KT", "QZ"], [psr(Zp[par])])
            P.op("act", lambda e, par=par, n=n: e.activation(out=Eb[par][:, 0:n * 128], in_=PS[Zp[par]][:, 0:n * 128], func=AF.Exp),
                 [psr(Zp[par])], [("Eb", par)])
            P.op("act", lambda e, par=par, n=n: e.activation(out=SPb[par][:, 0:n * 128], in_=Eb[par][:, 0:n * 128], func=AF.Ln, bias=1.0),
                 [("Eb", par)], [("SPb", par)])
            if g == 0:
                P.op("dve", lambda e, par=par: e.tensor_tensor(out=SPb[par][:, 0:128], in0=SPb[par][:, 0:128], in1=strict_bf, op=ALU.mult),
                     [("SPb", par), "cbf"], [("SPb", par)])

        def a_st2(it, i):
            m, h, g, blks, last = it
            par = i % 2
            n = len(blks)
            qs = slice(m * 128, (m + 1) * 128)
            cpar = g % 2
            for c in range(n):
                P.op("pe", lambda e, c=c, par=par, g=g, n=n, last=last: e.matmul(
                    PS[CP][:, 0:128], lhsT=ones_bf[:], rhs=SPb[par][:, c * 128:(c + 1) * 128],
                    start=(g == 0 and c == 0), stop=(last and c == n - 1), skip_group_check=True), [("SPb", par), "ones_bf"], [psr(CP)])
            if not last:
                P.op("dve", lambda e, cpar=cpar: e.tensor_copy(out=chi[1 - cpar][:], in_=PS[CP][:, 0:128]),
                     [psr(CP)], [("chi", 1 - cpar)])
                P.op("dve", lambda e, cpar=cpar: e.tensor_tensor(out=clo[1 - cpar][:], in0=PS[CP][:, 0:128], in1=chi[1 - cpar][:], op=ALU.subtract),
                     [psr(CP), ("chi", 1 - cpar)], [("clo", 1 - cpar)])
            mms = []
            for c, kb in enumerate(blks):
                cs = slice(c * 128, (c + 1) * 128)
                mms.append((PS[LWp[par]][:, cs], KT[:, h // 2, kb * 128:(kb + 1) * 128], QZ[:, h, qs], ["KT", "QZ"]))
            mms.append((PS[LWp[par]][:, 0:n * 128], negut_bf, SPb[par][:, 0:n * 128], ["cbf", ("SPb", par)]))
            for c2 in range(n - 1):
                k = n - 1 - c2
                mms.append((PS[LWp[par]][:, (c2 + 1) * 128:n * 128].rearrange("p (k i) -> p k i", k=k), negones_bf[:],
                            SPb[par][:, c2 * 128:(c2 + 1) * 128].unsqueeze(1).broadcast_to([128, k, 128]), ["negones_bf", ("SPb", par)]))
            if g > 0:
                for ct, cres in ((chi, "chi"), (clo, "clo")):
                    mms.append((PS[LWp[par]][:, 0:n * 128].rearrange("p (k i) -> p k i", k=n), negid_bf[:],
                                ct[cpar][:].unsqueeze(1).broadcast_to([128, n, 128]), ["negid_bf", (cres, cpar)]))
            if g == 0:
                mms.append((PS[LWp[par]][:, 0:128], ident_bf, nma_bf, ["cbf"]))
            for k, (ot, lt, rh, rd) in enumerate(mms):
                P.op("pe", lambda e, ot=ot, lt=lt, rh=rh, k=k, nm=len(mms): e.matmul(
                    ot, lhsT=lt, rhs=rh, start=(k == 0), stop=(k == nm - 1), skip_group_check=True), rd, [psr(LWp[par])])
            P.op("act", lambda e, par=par, n=n: e.activation(out=Wb[par][:, 0:n * 128], in_=PS[LWp[par]][:, 0:n * 128], func=AF.Exp),
                 [psr(LWp[par])], [("Wb", par)])

        def a_st3(it, i):
            m, h, g, blks, last = it
            par = i % 2
            opar = (m * 4 + h) % 2
            n = len(blks)
            for c, kb in enumerate(blks):
                P.op("pe", lambda e, c=c, kb=kb, par=par, opar=opar, h=h: e.matmul(
                    PS[OP[opar]][:, 0:64], lhsT=Wb[par][:, c * 128:(c + 1) * 128], rhs=VA[:, kb, h * 64:(h + 1) * 64],
                    start=(g == 0 and c == 0), stop=(last and c == n - 1)), [("Wb", par), "VA"], [psr(OP[opar])])
            if last:
                P.op("dve", lambda e, opar=opar, m=m, h=h: e.tensor_copy(out=o_t[:, m, h * 64:(h + 1) * 64], in_=PS[OP[opar]][:, 0:64]),
                     [psr(OP[opar])], ["o_t"])

        NI = len(items)
        for i in range(NI + 2):
            if i < NI:
                a_st1(items[i], i)
            if 1 <= i <= NI:
                a_st2(items[i - 1], i - 1)
            if 2 <= i:
                a_st3(items[i - 2], i - 2)
        norm_pass(o_t, 256, G_OUT, 0)
        P.barrier()
        P.pop()

        def softmax_attn(nheads, Dg, load_head, blocks_of, add_of, gofs, row0):
            P.push()
            o_t = P.sb([128, NT, Dg], F32)
            Wb = [P.sb([128, 512], BF16) for _ in range(2)]
            rc = [P.sb([128, 1], F32) for _ in range(2)]
            hb = [load_head(b) for b in range(2)]
            for b in range(2):
                P.op("pool", lambda e, b=b: e.memset(hb[b][2][:, :, 64:65], 1.0), [], [("hv", b)])
            Sp = [0, 1]
            Op = [2, 3]
            items = []
            for h in range(nheads):
                for m in range(NT):
                    blocks = blocks_of(m)
                    ng = (len(blocks) + 3) // 4
                    for g in range(ng):
                        items.append((h, m, g, blocks[g * 4:(g + 1) * 4], g == ng - 1))

            def st1(it, i):
                h, m, g, blks, last = it
                par = i % 2
                b = h % 2
                qt, kt, vt, fill = hb[b]
                if m == 0 and g == 0:
                    fill(h, b)
                n = len(blks)
                qs = slice(m * 128, (m + 1) * 128)
                for c, kb in enumerate(blks):
                    cs = slice(c * 128, (c + 1) * 128)
                    ad = add_of(h, m, kb)
                    P.op("pe", lambda e, cs=cs, kb=kb, par=par, qs=qs, kt=kt, qt=qt, ad=ad: e.matmul(
                        PS[Sp[par]][:, cs], lhsT=kt[:, kb * 128:(kb + 1) * 128], rhs=qt[:, qs], start=True, stop=(ad is None or ad[0] != "pe")),
                        [("hk", b), ("hq", b)], [psr(Sp[par])])
                    if ad is not None and ad[0] == "pe":
                        P.op("pe", lambda e, cs=cs, par=par, ad=ad: e.matmul(PS[Sp[par]][:, cs], lhsT=ident_bf, rhs=ad[1], start=False, stop=True),
                             ["cbf"], [psr(Sp[par])])
                    elif ad is not None:
                        P.op("dve", lambda e, cs=cs, par=par, ad=ad: e.tensor_tensor(out=PS[Sp[par]][:, cs], in0=PS[Sp[par]][:, cs], in1=ad[1], op=ALU.add),
                             [psr(Sp[par]), "ckbm"], [psr(Sp[par])])
                P.op("act", lambda e, par=par, n=n: e.activation(out=Wb[par][:, 0:n * 128], in_=PS[Sp[par]][:, 0:n * 128], func=AF.Exp),
                     [psr(Sp[par])], [("Wb", par)])

            def st2(it, i):
                h, m, g, blks, last = it
                par = i % 2
                b = h % 2
                qt, kt, vt, fill = hb[b]
                opar = (h * NT + m) % 2
                n = len(blks)
                for c, kb in enumerate(blks):
                    P.op("pe", lambda e, c=c, kb=kb, par=par, opar=opar, vt=vt: e.matmul(
                        PS[Op[opar]][:, 0:65], lhsT=Wb[par][:, c * 128:(c + 1) * 128], rhs=vt[:, kb, :],
                        start=(g == 0 and c == 0), stop=(last and c == n - 1)), [("Wb", par), ("hv", b)], [psr(Op[opar])])
                if last:
                    P.op("dve", lambda e, opar=opar: e.reciprocal(out=rc[opar][:], in_=PS[Op[opar]][:, 64:65]), [psr(Op[opar])], [("rc", opar)])
                    P.op("dve", lambda e, opar=opar, m=m, h=h: e.tensor_scalar(out=o_t[:, m, h * 64:(h + 1) * 64], in0=PS[Op[opar]][:, 0:64],
                                                                              scalar1=rc[opar][:, 0:1], scalar2=None, op0=ALU.mult),
                         [psr(Op[opar]), ("rc", opar)], ["o_t"])

            NI = len(items)
            for i in range(NI + 1):
                if i < NI:
                    st1(items[i], i)
                if i >= 1:
                    st2(items[i - 1], i - 1)
            norm_pass(o_t, Dg, gofs, row0)
            P.barrier()
            P.pop()

        def mla_load(b):
            qt = P.sb([96, S], BF16)
            kt = P.sb([96, S], BF16)
            vt = P.sb([128, NT, 65], BF16)

            def fill(h, b):
                P.dma(qt[:], mlQT[h], ["mlQT"], [("hq", b)])
                P.dma(kt[:], mlKT[h], ["mlKT"], [("hk", b)])
                P.dma(vt[:, :, 0:64], mlV[:, h * 64:(h + 1) * 64].rearrange("(n p) c -> p n c", p=128), ["mlV"], [("hv", b)])
            return (qt, kt, vt, fill)

        softmax_attn(8, 512, mla_load, lambda m: list(range(m, -1, -1)),
                     lambda h, m, kb: (("pe", mb_bf) if kb == m else None), G_OUT + 256, 256)

        def ck_load(b):
            qt = P.sb([128, S], BF16)
            kt = P.sb([128, S], BF16)
            vt = P.sb([128, NT, 65], BF16)

            def fill(h, b):
                P.dma(qt[:], ckQZ[h], ["ckQZ"], [("hq", b)])
                P.dma(kt[:], ckKT[h // 2], ["ckKT"], [("hk", b)])
                P.dma(vt[:, :, 0:64], ckV[:, h * 64:(h + 1) * 64].rearrange("(n p) c -> p n c", p=128), ["ckV"], [("hv", b)])
            return (qt, kt, vt, fill)

        softmax_attn(4, 256, ck_load, lambda m: [kb for kb in range(m, m - 5, -1) if kb >= 0],
                     lambda h, m, kb: ("dve", ckbm[:, h * 5 + (kb - (m - 4)), :]), G_OUT + 768, 768)
        if stop_after == ("P2", l):
            return True

        P.pop()
        P.push()
        moe = (l % 2 == 1)
        li = l // 2
        NSB = TS // 512
        wo_bf = P.sb([128, 8, D], BF16)
        wgb = [P.sb([128, 8, 512], BF16) for _ in range(2)]
        wub = [P.sb([128, 8, 512], BF16) for _ in range(2)]
        wdb = [P.sb([128, 12, 512], BF16) for _ in range(2)]
        P.dma(wo_bf[:], w_out[l].rearrange("(kc p) n -> p kc n", p=128), ["w_out"], ["wo_bf"], q="pool")
        x1 = P.sb([128, 8, TS], F32)
        h2T = P.sb([128, 8, TS], BF16)
        h2f = P.sb([128, 8, 512], F32)
        mtb = P.sb([128, 8, 512], BF16)
        rstd = P.sb([128, 512], F32)
        AT = P.sb([128, 12, TS], BF16)
        sg = [P.sb([128, 512], BF16) for _ in range(2)]
        if moe:
            wr = P.sb([128, 8, NE], F32)
            P.dma(wr[:], router[li].rearrange("(kc p) n -> p kc n", p=128), ["router"], ["wr"])
            lg = P.sb([128, 4, NE], F32)
            lg2 = P.sb([128, 4, NE], F32)
            eq1 = P.sb([128, 4, NE], F32)
            eq2 = P.sb([128, 4, NE], F32)
            mx = P.sb([128, 4, 4], F32)
            gts = P.sb([128, 4, NE], F32)
            gT = P.sb([8, TS], F32)
            Gb = [P.sb([128, 512], F32) for _ in range(NSB)]
            ytmp = [P.sb([128, 512], F32) for _ in range(2)]
        gcount = [0, 0]
        for tsb in range(S // TS):
            for sbi in range(NSB):
                t0 = tsb * TS + sbi * 512
                xs = slice(sbi * 512, (sbi + 1) * 512)
                P.dma(x1[:, :, xs], xsv[:, :, t0:t0 + 512], XIN, [("x1", sbi)])
                P.dma(mtb[:], mgT.rearrange("(c p) s -> p c s", p=128)[:, :, t0:t0 + 512], ["mgT"], ["mtb"])
                for c in range(8):
                    pb = c % 2
                    for kc in range(8):
                        P.op("pe", lambda e, c=c, kc=kc, pb=pb: e.matmul(PS[pb][:, :], lhsT=wo_bf[:, kc, c * 128:(c + 1) * 128], rhs=mtb[:, kc, :],
                                                                        start=(kc == 0), stop=(kc == 7)), ["wo_bf", "mtb"], [psr(pb)])
                    P.op("dve", lambda e, c=c, pb=pb, xs=xs: e.scalar_tensor_tensor(out=x1[:, c, xs], in0=PS[pb][:, :], scalar=mod[:, l, 16 + c:17 + c],
                                                                                    in1=x1[:, c, xs], op0=ALU.mult, op1=ALU.add),
                         [psr(pb), "mod", ("x1", sbi)], [("x1", sbi)])
                P.op("act", lambda e, xs=xs: e.activation(out=h2T[:, :, xs], in_=x1[:, :, xs], func=AF.Square), [("x1", sbi)], [("h2T", sbi)])
                for c in range(8):
                    P.op("pe", lambda e, c=c, xs=xs: e.matmul(PS[2][:, :], lhsT=ones_bf[:], rhs=h2T[:, c, xs], start=(c == 0), stop=(c == 7)),
                         [("h2T", sbi), "ones_bf"], [psr(2)])
                P.op("act", lambda e: e.activation(out=rstd[:], in_=PS[2][:, :], func=AF.Sqrt, scale=1.0 / D, bias=EPS), [psr(2)], ["rstd"])
                P.op("dve", lambda e: e.reciprocal(out=rstd[:], in_=rstd[:]), ["rstd"], ["rstd"])
                for c in range(8):
                    P.op("dve", lambda e, c=c, xs=xs: e.scalar_tensor_tensor(out=h2f[:, c, :], in0=x1[:, c, xs], scalar=A2[:, l, c:c + 1], in1=rstd[:],
                                                                             op0=ALU.mult, op1=ALU.mult), [("x1", sbi), "A2", "rstd"], [("h2f", c)])
                    P.op("act", lambda e, c=c, xs=xs: e.activation(out=h2T[:, c, xs], in_=h2f[:, c, :], func=AF.Identity, bias=mod[:, l, 24 + c:25 + c], scale=1.0),
                         [("h2f", c), "mod"], [("h2T", sbi)])
                    if moe:
                        P.op("dve", lambda e, c=c: e.tensor_scalar(out=h2f[:, c, :], in0=h2f[:, c, :], scalar1=mod[:, l, 24 + c:25 + c], scalar2=None, op0=ALU.add),
                             [("h2f", c), "mod"], [("h2f", c)])
                if moe:
                    H2F = [("h2f", c) for c in range(8)]
                    for i in range(4):
                        for kc in range(8):
                            P.op("pe", lambda e, i=i, kc=kc: e.matmul(PS[3][:, i * 8:(i + 1) * 8], lhsT=h2f[:, kc, i * 128:(i + 1) * 128], rhs=wr[:, kc, :],
                                                                      start=(kc == 0), stop=(kc == 7)), H2F + ["wr"], [psr(3)])
                    P.op("dve", lambda e: e.tensor_copy(out=lg[:].rearrange("p a b -> p (a b)"), in_=PS[3][:, 0:32]), [psr(3)], ["lg"])
                    P.op("dve", lambda e: e.tensor_reduce(out=mx[:, :, 0], in_=lg[:], axis=AX.X, op=ALU.max), ["lg"], ["mx"])
                    P.op("dve", lambda e: e.tensor_tensor(out=eq1[:], in0=lg[:], in1=mx[:, :, 0:1].broadcast_to([128, 4, NE]), op=ALU.is_equal),
                         ["lg", "mx"], ["eq1"])
                    P.op("dve", lambda e: e.scalar_tensor_tensor(out=lg2[:], in0=eq1[:], scalar=-1e30, in1=lg[:], op0=ALU.mult, op1=ALU.add),
                         ["eq1", "lg"], ["lg2"])
                    P.op("dve", lambda e: e.tensor_reduce(out=mx[:, :, 1], in_=lg2[:], axis=AX.X, op=ALU.max), ["lg2", "mx"], ["mx"])
                    P.op("dve", lambda e: e.tensor_tensor(out=eq2[:], in0=lg2[:], in1=mx[:, :, 1:2].broadcast_to([128, 4, NE]), op=ALU.is_equal),
                         ["lg2", "mx"], ["eq2"])
                    P.op("dve", lambda e: e.tensor_tensor(out=mx[:, :, 2], in0=mx[:, :, 1], in1=mx[:, :, 0], op=ALU.subtract), ["mx"], ["mx"])
                    P.op("act", lambda e: e.activation(out=mx[:, :, 2], in_=mx[:, :, 2], func=AF.Exp), ["mx"], ["mx"])
                    P.op("dve", lambda e: e.tensor_scalar(out=mx[:, :, 2], in0=mx[:, :, 2], scalar1=1.0, scalar2=None, op0=ALU.add), ["mx"], ["mx"])
                    P.op("dve", lambda e: e.reciprocal(out=mx[:, :, 2], in_=mx[:, :, 2]), ["mx"], ["mx"])
                    P.op("dve", lambda e: e.tensor_scalar(out=mx[:, :, 3], in0=mx[:, :, 2], scalar1=-1.0, scalar2=1.0, op0=ALU.mult, op1=ALU.add), ["mx"], ["mx"])
                    P.op("dve", lambda e: e.tensor_tensor(out=eq1[:], in0=eq1[:], in1=mx[:, :, 2:3].broadcast_to([128, 4, NE]), op=ALU.mult), ["eq1", "mx"], ["eq1"])
                    P.op("dve", lambda e: e.tensor_tensor(out=eq2[:], in0=eq2[:], in1=mx[:, :, 3:4].broadcast_to([128, 4, NE]), op=ALU.mult), ["eq2", "mx"], ["eq2"])
                    P.op("dve", lambda e: e.tensor_tensor(out=gts[:], in0=eq1[:], in1=eq2[:], op=ALU.add), ["eq1", "eq2"], ["gts"])
                    for i in range(4):
                        P.op("pe", lambda e, i=i: e.transpose(PS[2][0:8, i * 128:(i + 1) * 128], gts[:, i, :], ident_f), ["gts", "cst"], [psr(2)])
                    P.op("act", lambda e, xs=xs: e.activation(out=gT[:, xs], in_=PS[2][0:8, :], func=AF.Copy), [psr(2)], [("gT", sbi)])
                    if debug:
                        P.dma(dbgG[:, t0:t0 + 512], gT[:, xs], [("gT", sbi)], ["dbgG"])
            nexp = NE if moe else 1
            for ex in range(nexp):
                if moe:
                    wg_d, wu_d, wd_d = moe_g[li, ex], moe_u[li, ex], moe_d[li, ex]
                else:
                    wg_d, wu_d, wd_d = ffn_g[li], ffn_u[li], ffn_d[li]
                wgv = wg_d.rearrange("(kc p) n -> p kc n", p=128)
                wuv = wu_d.rearrange("(kc p) n -> p kc n", p=128)
                wdv = wd_d.rearrange("(j p) n -> p j n", p=128)
                for pas, (j0, njp, groups) in enumerate(((0, 12, ((0, 4), (4, 4), (8, 4))), (12, 10, ((12, 4), (16, 4), (20, 2))))):
                    for (js, nj) in groups:
                        wbuf = gcount[0] % 2
                        gcount[0] += 1
                        P.dma(wgb[wbuf][:, :, 0:nj * 128], wgv[:, :, js * 128:(js + nj) * 128], ["wg_d"], [("wgb", wbuf)], q="pool")
                        P.dma(wub[wbuf][:, :, 0:nj * 128], wuv[:, :, js * 128:(js + nj) * 128], ["wu_d"], [("wub", wbuf)], q="pool")
                        for jj in range(nj):
                            jl = js + jj - j0
                            for sbi in range(NSB):
                                xs = slice(sbi * 512, (sbi + 1) * 512)
                                par = (jj * NSB + sbi) % 2
                                gp, up = 4 + par, 6 + par
                                for kc in range(8):
                                    P.op("pe", lambda e, kc=kc, jj=jj, wbuf=wbuf, xs=xs, gp=gp: e.matmul(
                                        PS[gp][:, :], lhsT=wgb[wbuf][:, kc, jj * 128:(jj + 1) * 128], rhs=h2T[:, kc, xs], start=(kc == 0), stop=(kc == 7)),
                                        [("wgb", wbuf), ("h2T", sbi)], [psr(gp)])
                                for kc in range(8):
                                    P.op("pe", lambda e, kc=kc, jj=jj, wbuf=wbuf, xs=xs, up=up: e.matmul(
                                        PS[up][:, :], lhsT=wub[wbuf][:, kc, jj * 128:(jj + 1) * 128], rhs=h2T[:, kc, xs], start=(kc == 0), stop=(kc == 7)),
                                        [("wub", wbuf), ("h2T", sbi)], [psr(up)])
                                P.op("act", lambda e, par=par, gp=gp: e.activation(out=sg[par][:], in_=PS[gp][:, :], func=AF.Silu), [psr(gp)], [("sg", par)])
                                P.op("dve", lambda e, par=par, up=up, jl=jl, xs=xs: e.tensor_tensor(out=AT[:, jl, xs], in0=PS[up][:, :], in1=sg[par][:], op=ALU.mult),
                                     [psr(up), ("sg", par)], [("AT", sbi)])
                    for dh in range(2):
                        wdbuf = gcount[1] % 2
                        gcount[1] += 1
                        P.dma(wdb[wdbuf][:, 0:njp, :], wdv[:, j0:j0 + njp, dh * 512:(dh + 1) * 512], ["wd_d"], [("wdb", wdbuf)], q="pool")
                        for cc in range(4):
                            c = dh * 4 + cc
                            for sbi in range(NSB):
                                xs = slice(sbi * 512, (sbi + 1) * 512)
                                par = (cc * NSB + sbi) % 2
                                dp = par
                                for jl in range(njp):
                                    P.op("pe", lambda e, jl=jl, wdbuf=wdbuf, xs=xs, dp=dp, cc=cc, njp=njp: e.matmul(
                                        PS[dp][:, :], lhsT=wdb[wdbuf][:, jl, cc * 128:(cc + 1) * 128], rhs=AT[:, jl, xs], start=(jl == 0), stop=(jl == njp - 1)),
                                        [("wdb", wdbuf), ("AT", sbi)], [psr(dp)])
                                if not moe:
                                    P.op("dve", lambda e, c=c, xs=xs, dp=dp: e.scalar_tensor_tensor(
                                        out=x1[:, c, xs], in0=PS[dp][:, :], scalar=mod[:, l, 40 + c:41 + c], in1=x1[:, c, xs], op0=ALU.mult, op1=ALU.add),
                                        [psr(dp), "mod", ("x1", sbi)], [("x1", sbi)])
                                else:
                                    if pas == 0 and dh == 0 and cc == 0:
                                        P.op("pe", lambda e, xs=xs, sbi=sbi, ex=ex: e.matmul(
                                            PS[2 + sbi % 2][:, :], lhsT=cst[0:8, C_SEL + ex * 128:C_SEL + (ex + 1) * 128], rhs=gT[:, xs], start=True, stop=True),
                                            ["cst", ("gT", sbi)], [psr(2 + sbi % 2)])
                                        P.op("act", lambda e, sbi=sbi: e.activation(out=Gb[sbi][:], in_=PS[2 + sbi % 2][:, :], func=AF.Copy),
                                             [psr(2 + sbi % 2)], [("Gb", sbi)])
                                    P.op("dve", lambda e, c=c, dp=dp, sbi=sbi, par=par: e.scalar_tensor_tensor(
                                        out=ytmp[par][:], in0=PS[dp][:, :], scalar=mod[:, l, 40 + c:41 + c], in1=Gb[sbi][:], op0=ALU.mult, op1=ALU.mult),
                                        [psr(dp), "mod", ("Gb", sbi)], [("ytmp", par)])
                                    P.op("dve", lambda e, c=c, xs=xs, par=par: e.tensor_tensor(out=x1[:, c, xs], in0=x1[:, c, xs], in1=ytmp[par][:], op=ALU.add),
                                         [("ytmp", par), ("x1", sbi)], [("x1", sbi)])
            for sbi in range(NSB):
                t0 = tsb * TS + sbi * 512
                xs = slice(sbi * 512, (sbi + 1) * 512)
                P.dma(xdv[:, :, t0:t0 + 512], x1[:, :, xs], [("x1", sbi)], [(xres_out, sbi % 2)])
        P.barrier()
        P.pop()

    for l in range(nlayers):
        if do_layer(l):
            break

    P.emit(final_waits=[("yT", 0), ("yT", 1)])
    return nc


def host_inputs(inp, S):
    NT = S // 128
    B = inp["x"].shape[0]
    f = lambda a: np.ascontiguousarray(np.asarray(a, dtype=np.float32))
    consts = make_consts()
    L = inp["w_in"].shape[0]
    vec = np.zeros((L, 128, 64), np.float32)
    gains = np.zeros((L, 128, NGAIN), np.float32)
    ckb = np.zeros((L, 128, 20, 128), np.float32)
    jj = np.arange(128)[:, None]
    ii = np.arange(128)[None, :]
    for l in range(L):
        vec[l, :, 0:48] = np.asarray(inp["ada_b"][l]).reshape(48, 128).T
        vec[l, :, 48:56] = np.asarray(inp["norm_mix"][l]).reshape(8, 128).T
        vec[l, :, 56:64] = np.asarray(inp["norm_ffn"][l]).reshape(8, 128).T
        g = np.concatenate([np.asarray(inp[k][l]) for k in ("mla_q_norm", "mla_kv_norm", "mla_q_qknorm", "mla_k_qknorm",
                                                            "ck_q_qknorm", "ck_k_qknorm", "group_out_norm")])
        gains[l] = np.broadcast_to(g[None, :], (128, NGAIN))
        rb = np.asarray(inp["ck_rel_bias"][l])
        for h in range(4):
            for o in range(5):
                idx = np.clip(128 * (4 - o) + ii - jj, -128, 128) + 128
                ckb[l, :, h * 5 + o, :] = rb[h][idx]
    shared = dict(consts=consts, vec=vec, gains=gains, ckbias=ckb)
    for k in ("ada_w", "w_in", "w_q_up", "w_kv_up", "w_out", "ffn_w_gate", "ffn_w_up", "ffn_w_down", "moe_router",
              "moe_w_gate", "moe_w_up", "moe_w_down"):
        shared[k] = f(inp[k])
    maps = []
    for b in range(B):
        m = dict(shared)
        m["xT"] = np.ascontiguousarray(np.asarray(inp["x"][b], np.float32).T)
        m["cT"] = np.ascontiguousarray(np.asarray(inp["c"][b], np.float32).reshape(8, 128).T)
        m["pos"] = np.ascontiguousarray(np.asarray(inp["positions"][b], np.int32).reshape(NT, 128).T)
        maps.append(m)
    return maps


_NC_CACHE = {}


def kernel(**inputs):
    S = inputs["x"].shape[1]
    B = inputs["x"].shape[0]
    key = (S,)
    if key not in _NC_CACHE:
        _NC_CACHE[key] = build(S)
    nc = _NC_CACHE[key]
    maps = host_inputs(inputs, S)
    res = run_bass_kernel_spmd(nc, maps, core_ids=list(range(B)))
    out = np.stack([np.ascontiguousarray(res.results[b]["yT"].T) for b in range(B)], axis=0)
    return out.astype(np.float32)
```
